# Optimizing a Trainium2 kernel written in Bass

```python
import jax, jax.numpy as jnp
from jax import lax
import numpy as np

D_MODEL = 2048
BATCH = 4
SEQ = 2048
DEPTH = 1
DEC_BATCH = 128
DEC_SEQ = 8
PAST_LEN = 16384
PAGE_SIZE = 128

SSD_EXPAND = 2
SSD_D_INNER = SSD_EXPAND * D_MODEL
SSD_HEAD_DIM = 64
SSD_HEADS = SSD_D_INNER // SSD_HEAD_DIM
SSD_GROUPS = 8
SSD_STATE = 128
SSD_CONV_W = 4
SSD_CONV_DIM = SSD_D_INNER + 2 * SSD_GROUPS * SSD_STATE
SSD_CHUNK = 128
SSD_NORM_EPS = 1e-5
SC_DIM = D_MODEL
SC_W = 3
MOE_GROUPS = 4
MOE_EXPERTS_PER_GROUP = 8
MOE_EXPERTS = MOE_GROUPS * MOE_EXPERTS_PER_GROUP
MOE_TOP_K = 2
MOE_D_FF = D_MODEL // 2
MOE_BLOCK = 128
NORM_EPS = 1e-6

OFF_GATE_A = 0
OFF_GATE_B = D_MODEL
OFF_Z = 2 * D_MODEL
OFF_XBC = OFF_Z + SSD_D_INNER
OFF_DT = OFF_XBC + SSD_CONV_DIM
OFF_SC = OFF_DT + SSD_HEADS
IN_PROJ_DIM = OFF_SC + 3 * SC_DIM

kernel_name = "hybrid_ssd_shortconv_hmoe_step"


def rmsnorm(x, w, eps=NORM_EPS):
    xf = x.astype(jnp.float32)
    y = xf * lax.rsqrt(jnp.mean(xf * xf, axis=-1, keepdims=True) + eps)
    return (y * w.astype(jnp.float32)).astype(x.dtype)


def causal_dwconv(u, buf, w):
    L = u.shape[1]
    K = w.shape[0]
    up = jnp.concatenate([buf.astype(u.dtype), u], axis=1)
    y = sum(up[:, k:k + L] * w[k] for k in range(K))
    return y, up[:, L:]


def gated_group_rmsnorm(y, z, w):
    g = y.astype(jnp.float32) * jax.nn.silu(z.astype(jnp.float32))
    shp = g.shape
    g = g.reshape(shp[:-1] + (SSD_GROUPS, shp[-1] // SSD_GROUPS))
    g = g * lax.rsqrt(jnp.mean(g * g, axis=-1, keepdims=True) + SSD_NORM_EPS)
    return g.reshape(shp) * w.astype(jnp.float32)


def ssd_chunked(xh, dt, A, Bm, Cm, h0):
    b, L, H, P = xh.shape
    G, N = Bm.shape[2], Bm.shape[3]
    Hg = H // G
    Q = SSD_CHUNK if L % SSD_CHUNK == 0 else L
    nc = L // Q
    f32 = jnp.float32
    x = xh.astype(f32).reshape(b, nc, Q, G, Hg, P)
    dtc = dt.reshape(b, nc, Q, G, Hg)
    a = dtc * A.reshape(G, Hg)
    Bc = Bm.astype(f32).reshape(b, nc, Q, G, N)
    Cc = Cm.astype(f32).reshape(b, nc, Q, G, N)
    cs = jnp.cumsum(a, axis=2)
    causal = jnp.tril(jnp.ones((Q, Q), dtype=bool))[:, :, None, None]
    seg = cs[:, :, :, None] - cs[:, :, None, :]
    decay = jnp.exp(jnp.where(causal, seg, -jnp.inf))
    CB = jnp.einsum('bcqgn,bcsgn->bcqsg', Cc, Bc)
    scores = CB[..., None] * decay
    y_diag = jnp.einsum('bcqsgh,bcsgh,bcsghp->bcqghp', scores, dtc, x)
    decay_end = jnp.exp(cs[:, :, -1:] - cs)
    chunk_states = jnp.einsum('bcsgn,bcsgh,bcsghp->bcghpn', Bc, decay_end * dtc, x)
    chunk_decay = jnp.exp(cs[:, :, -1])

    def step(h, inp):
        st, dec = inp
        return dec[..., None, None] * h + st, h

    h_init = h0.astype(f32).reshape(b, G, Hg, P, N)
    h_final, h_prev = lax.scan(step, h_init, (jnp.moveaxis(chunk_states, 1, 0), jnp.moveaxis(chunk_decay, 1, 0)))
    h_prev = jnp.moveaxis(h_prev, 0, 1)
    y_off = jnp.einsum('bcqgn,bcghpn,bcqgh->bcqghp', Cc, h_prev, jnp.exp(cs))
    y = (y_diag + y_off).reshape(b, L, H, P)
    return y, h_final.reshape(b, H, P, N)


def token_mixers(h, ssm0, conv0, sc0, w_in, conv_w, conv_b, dt_bias, a_log, d_skip, ssd_norm_w, sc_conv_w, w_branch_out, w_out):
    b, L, _ = h.shape
    proj = h @ w_in
    gate_a = jax.nn.sigmoid(proj[..., OFF_GATE_A:OFF_GATE_B])
    gate_b = jax.nn.sigmoid(proj[..., OFF_GATE_B:OFF_Z])
    z = proj[..., OFF_Z:OFF_XBC]
    xbc, conv_new = causal_dwconv(proj[..., OFF_XBC:OFF_DT], conv0, conv_w)
    xbc = jax.nn.silu(xbc + conv_b)
    dt_raw = proj[..., OFF_DT:OFF_SC]
    sc_b = proj[..., OFF_SC:OFF_SC + SC_DIM]
    sc_c = proj[..., OFF_SC + SC_DIM:OFF_SC + 2 * SC_DIM]
    sc_h = proj[..., OFF_SC + 2 * SC_DIM:]
    GN = SSD_GROUPS * SSD_STATE
    xs = xbc[..., :SSD_D_INNER].reshape(b, L, SSD_HEADS, SSD_HEAD_DIM)
    Bm = xbc[..., SSD_D_INNER:SSD_D_INNER + GN].reshape(b, L, SSD_GROUPS, SSD_STATE)
    Cm = xbc[..., SSD_D_INNER + GN:].reshape(b, L, SSD_GROUPS, SSD_STATE)
    dt = jax.nn.softplus(dt_raw.astype(jnp.float32) + dt_bias.astype(jnp.float32))
    A = -jnp.exp(a_log.astype(jnp.float32))
    y, ssm_new = ssd_chunked(xs, dt, A, Bm, Cm, ssm0)
    y = y + xs.astype(jnp.float32) * d_skip.astype(jnp.float32)[:, None]
    y_a = gated_group_rmsnorm(y.reshape(b, L, SSD_D_INNER), z, ssd_norm_w).astype(h.dtype)
    v, sc_new = causal_dwconv(sc_c * sc_h, sc0, sc_conv_w)
    y_b = sc_b * v
    merged = gate_a * (y_a @ w_branch_out[:SSD_D_INNER]) + gate_b * (y_b @ w_branch_out[SSD_D_INNER:])
    return merged @ w_out, ssm_new.astype(ssm0.dtype), conv_new, sc_new


def hier_moe(h, w_rc, w_rf, w_gate, w_up, w_down):
    b, L, D = h.shape
    T = b * L
    xt = h.reshape(T, D)
    lc = (xt @ w_rc).astype(jnp.float32)
    pc = jax.nn.softmax(lc, axis=-1)
    grp = jnp.argmax(lc, axis=-1)
    p_grp = jnp.take_along_axis(pc, grp[:, None], axis=1)[:, 0]
    lf = (xt @ w_rf).astype(jnp.float32).reshape(T, MOE_GROUPS, MOE_EXPERTS_PER_GROUP)
    lf_sel = jnp.take_along_axis(lf, grp[:, None, None], axis=1)[:, 0]
    topv, topi = lax.top_k(lf_sel, MOE_TOP_K)
    wts = jax.nn.softmax(topv, axis=-1) * p_grp[:, None]
    eid = (grp[:, None] * MOE_EXPERTS_PER_GROUP + topi).astype(jnp.int32)
    A_n = T * MOE_TOP_K
    e_flat = eid.reshape(A_n)
    w_flat = wts.reshape(A_n)
    tok_flat = jnp.broadcast_to(jnp.arange(T, dtype=jnp.int32)[:, None], (T, MOE_TOP_K)).reshape(A_n)
    order = jnp.argsort(e_flat)
    e_s, tok_s, w_s = e_flat[order], tok_flat[order], w_flat[order]
    counts = jnp.bincount(e_flat, length=MOE_EXPERTS).astype(jnp.int32)
    start = jnp.cumsum(counts) - counts
    padded = (counts + MOE_BLOCK - 1) // MOE_BLOCK * MOE_BLOCK
    pstart = jnp.cumsum(padded) - padded
    pend = pstart + padded
    dest = pstart[e_s] + (jnp.arange(A_n, dtype=jnp.int32) - start[e_s])
    NB = -(-A_n // MOE_BLOCK) + MOE_EXPERTS
    R = NB * MOE_BLOCK
    row_tok = jnp.full((R,), T, dtype=jnp.int32).at[dest].set(tok_s)
    row_w = jnp.zeros((R,), jnp.float32).at[dest].set(w_s)
    blk_e = jnp.clip(jnp.searchsorted(pend, jnp.arange(NB, dtype=jnp.int32) * MOE_BLOCK, side='right'), 0, MOE_EXPERTS - 1)
    x_pad = jnp.concatenate([xt, jnp.zeros((1, D), xt.dtype)], axis=0)
    xb = x_pad[row_tok].reshape(NB, MOE_BLOCK, D)

    def expert_block(args):
        xblk, e = args
        return (jax.nn.silu(xblk @ w_gate[e]) * (xblk @ w_up[e])) @ w_down[e]

    yb = lax.map(expert_block, (xb, blk_e)).reshape(R, D)
    y = jax.ops.segment_sum(yb.astype(jnp.float32) * row_w[:, None], row_tok, num_segments=T + 1)[:T]
    return y.astype(h.dtype).reshape(b, L, D)


def trunk(x, st_ssm, st_conv, st_sc, weights):
    (norm_mixer, w_in, ssd_conv_w, ssd_conv_b, ssd_dt_bias, ssd_a_log, ssd_d, ssd_norm, sc_conv_w,
     w_branch_out, w_out, norm_ffn, w_router_coarse, w_router_fine, w_expert_gate, w_expert_up,
     w_expert_down, norm_final) = weights
    new_ssm, new_conv, new_sc = [], [], []
    for l in range(DEPTH):
        h = rmsnorm(x, norm_mixer[l])
        mix, s_ssm, s_conv, s_sc = token_mixers(h, st_ssm[l], st_conv[l], st_sc[l], w_in[l], ssd_conv_w[l], ssd_conv_b[l],
                                                 ssd_dt_bias[l], ssd_a_log[l], ssd_d[l], ssd_norm[l], sc_conv_w[l],
                                                 w_branch_out[l], w_out[l])
        x = x + mix
        x = x + hier_moe(rmsnorm(x, norm_ffn[l]), w_router_coarse[l], w_router_fine[l],
                         w_expert_gate[l], w_expert_up[l], w_expert_down[l])
        new_ssm.append(s_ssm)
        new_conv.append(s_conv)
        new_sc.append(s_sc)
    return rmsnorm(x, norm_final), jnp.stack(new_ssm), jnp.stack(new_conv), jnp.stack(new_sc)


def setup_inputs(seed: int = 0) -> dict:
    key = jax.random.key(seed)
    ks = jax.random.split(key, 24)
    f32 = jnp.float32

    def nrm(k, shape, scale):
        return jax.random.normal(k, shape, f32) * scale

    dt0 = jnp.exp(jax.random.uniform(ks[8], (DEPTH, SSD_HEADS), f32, np.log(1e-3), np.log(1e-1)))
    return {
        "x_prompt": nrm(ks[0], (BATCH, SEQ, D_MODEL), 1.0),
        "x_sample": nrm(ks[1], (DEC_BATCH, DEC_SEQ, D_MODEL), 1.0),
        "state_ssm": nrm(ks[2], (DEPTH, DEC_BATCH, SSD_HEADS, SSD_HEAD_DIM, SSD_STATE), 0.1),
        "state_ssd_conv": nrm(ks[3], (DEPTH, DEC_BATCH, SSD_CONV_W - 1, SSD_CONV_DIM), 1.0),
        "state_short_conv": nrm(ks[4], (DEPTH, DEC_BATCH, SC_W - 1, SC_DIM), 0.5),
        "norm_mixer": 1.0 + nrm(ks[5], (DEPTH, D_MODEL), 0.02),
        "w_in": nrm(ks[6], (DEPTH, D_MODEL, IN_PROJ_DIM), D_MODEL ** -0.5),
        "ssd_conv_w": nrm(ks[7], (DEPTH, SSD_CONV_W, SSD_CONV_DIM), SSD_CONV_W ** -0.5),
        "ssd_conv_b": nrm(ks[9], (DEPTH, SSD_CONV_DIM), 0.02),
        "ssd_dt_bias": dt0 + jnp.log(-jnp.expm1(-dt0)),
        "ssd_a_log": jnp.log(jax.random.uniform(ks[10], (DEPTH, SSD_HEADS), f32, 1.0, 16.0)),
        "ssd_d": 1.0 + nrm(ks[11], (DEPTH, SSD_HEADS), 0.1),
        "ssd_norm": 1.0 + nrm(ks[12], (DEPTH, SSD_D_INNER), 0.02),
        "sc_conv_w": nrm(ks[13], (DEPTH, SC_W, SC_DIM), SC_W ** -0.5),
        "w_branch_out": nrm(ks[14], (DEPTH, SSD_D_INNER + SC_DIM, D_MODEL), (SSD_D_INNER + SC_DIM) ** -0.5),
        "w_out": nrm(ks[15], (DEPTH, D_MODEL, D_MODEL), D_MODEL ** -0.5),
        "norm_ffn": 1.0 + nrm(ks[16], (DEPTH, D_MODEL), 0.02),
        "w_router_coarse": nrm(ks[17], (DEPTH, D_MODEL, MOE_GROUPS), D_MODEL ** -0.5),
        "w_router_fine": nrm(ks[18], (DEPTH, D_MODEL, MOE_EXPERTS), D_MODEL ** -0.5),
        "w_expert_gate": nrm(ks[19], (DEPTH, MOE_EXPERTS, D_MODEL, MOE_D_FF), D_MODEL ** -0.5),
        "w_expert_up": nrm(ks[20], (DEPTH, MOE_EXPERTS, D_MODEL, MOE_D_FF), D_MODEL ** -0.5),
        "w_expert_down": nrm(ks[21], (DEPTH, MOE_EXPERTS, MOE_D_FF, D_MODEL), MOE_D_FF ** -0.5),
        "norm_final": 1.0 + nrm(ks[22], (D_MODEL,), 0.02),
    }


def reference(x_prompt, x_sample, state_ssm, state_ssd_conv, state_short_conv, norm_mixer, w_in, ssd_conv_w,
              ssd_conv_b, ssd_dt_bias, ssd_a_log, ssd_d, ssd_norm, sc_conv_w, w_branch_out, w_out, norm_ffn,
              w_router_coarse, w_router_fine, w_expert_gate, w_expert_up, w_expert_down, norm_final):
    weights = (norm_mixer, w_in, ssd_conv_w, ssd_conv_b, ssd_dt_bias, ssd_a_log, ssd_d, ssd_norm, sc_conv_w,
               w_branch_out, w_out, norm_ffn, w_router_coarse, w_router_fine, w_expert_gate, w_expert_up,
               w_expert_down, norm_final)
    bp = x_prompt.shape[0]
    z_ssm = jnp.zeros((DEPTH, bp, SSD_HEADS, SSD_HEAD_DIM, SSD_STATE), state_ssm.dtype)
    z_conv = jnp.zeros((DEPTH, bp, SSD_CONV_W - 1, SSD_CONV_DIM), x_prompt.dtype)
    z_sc = jnp.zeros((DEPTH, bp, SC_W - 1, SC_DIM), x_prompt.dtype)
    y_prompt, p_ssm, p_conv, p_sc = trunk(x_prompt, z_ssm, z_conv, z_sc, weights)
    y_sample, s_ssm, s_conv, s_sc = trunk(x_sample, state_ssm, state_ssd_conv, state_short_conv, weights)
    return (y_prompt, y_sample, p_ssm, p_conv, p_sc, s_ssm, s_conv, s_sc)
```

```python
import numpy as np
import concourse.bass as bass
import concourse.mybir as mybir
from concourse.bass_utils import run_bass_kernel_spmd

F32 = mybir.dt.float32
BF16 = mybir.dt.bfloat16
ALU = mybir.AluOpType
AF = mybir.ActivationFunctionType
AX = mybir.AxisListType
SAME_ENGINE_SYNC = True

D = 2048
TM, NTM = 1152, 9
TP, NTP = 1024, 8
NPROJ = 20544
C_GA, C_GB, C_Z, C_X, C_B, C_C, C_DT, C_SB, C_SC, C_SH = 0, 2048, 4096, 8192, 12288, 13312, 14336, 14400, 16448, 18496
NE, CAP = 32, 128


class Buf:
    __slots__ = ("name", "w", "r", "sem", "cnt")

    def __init__(self, name):
        self.name = name
        self.w = None
        self.r = []
        self.sem = None
        self.cnt = 0


class Eng:
    def __init__(self, name, sem):
        self.name = name
        self.sem = sem
        self.cnt = 0
        self.seen = {}
        self.prog = []
        self.pr = []
        self.pw = []


class FW:
    def __init__(self, nc, es):
        self.nc = nc
        self.es = es
        self.eng = {}
        for n in ("tensor", "vector", "scalar", "gpsimd", "sync"):
            self.eng[n] = Eng(n, self.new_sem("e_" + n))
        self.dma_sems = []
        self.semcnt = {}
        self.nbuf = 0
        self.big = es.enter_context(nc.sbuf_tensor("big", [128, 51200], F32))
        self.big16 = self.big.bitcast(BF16)
        self.off = 0
        self.marks = []
        self.free_sems = []

    def new_sem(self, name):
        return self.es.enter_context(self.nc.semaphore(name))

    def buf(self, name=None):
        self.nbuf += 1
        return Buf(name or f"b{self.nbuf}")

    def push(self):
        self.marks.append((self.off, []))

    def pop(self):
        self.off, bufs = self.marks.pop()
        for b in bufs:
            if b.sem is not None:
                self.free_sems.append(b.sem)
                b.sem = None

    def alloc(self, shape, dt, name=None):
        n = 1
        for s in shape[1:]:
            n *= s
        esz = 4 if dt == F32 else 2
        nbytes = (n * esz + 31) // 32 * 32
        assert self.off + nbytes <= 51200 * 4, f"SBUF overflow {name} {self.off} + {nbytes}"
        if dt == F32:
            o = self.off // 4
            ap = self.big[0:shape[0], o:o + n]
        else:
            o = self.off // 2
            ap = self.big16[0:shape[0], o:o + n]
        self.off += nbytes
        bufobj = self.buf(name)
        if self.marks:
            self.marks[-1][1].append(bufobj)
        if len(shape) == 3:
            ap = ap.rearrange("p (a b) -> p a b", a=shape[1])
        elif len(shape) == 4:
            ap = ap.rearrange("p (a b c) -> p a b c", a=shape[1], b=shape[2])
        return ap, bufobj

    def alias(self, off, shape, dt):
        n = 1
        for x in shape[1:]:
            n *= x
        if dt == F32:
            ap = self.big[0:shape[0], off // 4:off // 4 + n]
        else:
            ap = self.big16[0:shape[0], off // 2:off // 2 + n]
        if len(shape) == 3:
            ap = ap.rearrange("p (a b) -> p a b", a=shape[1])
        return ap

    def ring(self, n, shape, dt, name=None):
        return Ring([self.alloc(shape, dt, f"{name}{i}") for i in range(n)])

    def _need(self, E, tks):
        best = {}
        for (s, v) in tks:
            k = id(s)
            if k not in best or best[k][1] < v:
                best[k] = (s, v)
        for k, (s, v) in best.items():
            if (s is E.sem) and not SAME_ENGINE_SYNC:
                continue
            if E.seen.get(k, 0) >= v:
                continue
            E.seen[k] = v
            E.prog.append(lambda e, s=s, v=v: e.wait_ge(s, v))

    def _chk(self, en, reads, writes):
        for n2, E2 in self.eng.items():
            if n2 == en:
                continue
            for b in writes:
                assert all(b is not p for p in E2.pr) and all(b is not p for p in E2.pw), f"pending hazard {b.name} {n2}"
            for b in reads:
                assert all(b is not p for p in E2.pw), f"pending hazard {b.name} {n2}"

    def op(self, en, build, reads=(), writes=(), signal=True):
        E = self.eng[en]
        self._chk(en, reads, writes)
        tks = []
        for b in reads:
            if b.w:
                tks.append(b.w)
        for b in writes:
            if b.w:
                tks.append(b.w)
            tks.extend(b.r)
        self._need(E, tks)
        if signal:
            E.cnt += 1
            tk = (E.sem, E.cnt)
            sem = E.sem
            E.prog.append(lambda e, build=build, sem=sem: build(e).then_inc(sem, 1))
            rs = list(reads) + E.pr
            ws = list(writes) + E.pw
            E.pr, E.pw = [], []
            for b in ws:
                b.w = tk
                b.r = []
            for b in rs:
                if b.w is not tk:
                    b.r.append(tk)
        else:
            E.prog.append(lambda e, build=build: build(e))
            E.pr.extend(reads)
            E.pw.extend(writes)

    def dma(self, en, out, in_, src=None, dst=None, owner=None):
        E = self.eng[en]
        assert not E.pr and not E.pw
        self._chk(en, [src] if src is not None else [], [dst] if dst is not None else [])
        tks = []
        if src is not None and src.w:
            tks.append(src.w)
        if dst is not None:
            if dst.w:
                tks.append(dst.w)
            tks.extend(dst.r)
        self._need(E, tks)
        if owner is None:
            owner = dst if dst is not None else src
        if owner.sem is None:
            if self.free_sems:
                owner.sem = self.free_sems.pop()
            else:
                owner.sem = self.new_sem(f"d{len(self.semcnt)}")
                self.semcnt[id(owner.sem)] = [owner.sem, 0]
        ent = self.semcnt[id(owner.sem)]
        ent[1] += 16
        tk = (owner.sem, ent[1])
        sem = owner.sem
        E.prog.append(lambda e, out=out, in_=in_, sem=sem: e.dma_start(out=out, in_=in_).then_inc(sem, 16))
        if dst is not None:
            dst.w = tk
            dst.r = []
        if src is not None:
            src.r.append(tk)

    def barrier(self):
        tks = [(E.sem, E.cnt) for E in self.eng.values() if E.cnt > 0]
        tks += [(s_, c_) for (s_, c_) in self.semcnt.values()]
        for E in self.eng.values():
            assert not E.pr and not E.pw, E.name
            self._need(E, tks)

    def emit(self):
        self.barrier()
        with self.nc.Block() as block:
            for en in ("tensor", "vector", "scalar", "gpsimd", "sync"):
                prog = self.eng[en].prog

                def body(e, prog=prog):
                    for f in prog:
                        f(e)
                getattr(block, en)(body)


class Ring:
    def __init__(self, items):
        self.items = items
        self.i = 0

    def next(self):
        r = self.items[self.i % len(self.items)]
        self.i += 1
        return r


def build_program(stage=99, debug=False):
    import contextlib
    nc = bass.Bass("TRN2", target_bir_lowering=False)
    es = contextlib.ExitStack()
    fw = FW(nc, es)

    def din(name, shape, dt=F32):
        return nc.dram_tensor(name, list(shape), dt, kind="ExternalInput").ap()

    def dout(name, shape, dt=F32):
        return nc.dram_tensor(name, list(shape), dt, kind="ExternalOutput").ap()

    def dscr(name, shape, dt):
        return nc.dram_tensor(name, list(shape), dt, kind="Internal").ap()

    xm_d = din("xm", [TM, D]); xp_d = din("xp", [TP, D]); flag_d = din("flag", [128, 1])
    st_ssm_d = din("st_ssm", [16, 64, 64, 128]); st_conv_d = din("st_conv", [48, 6144]); st_sc_d = din("st_sc", [32, 2048])
    w_in_d = din("w_in", [D, NPROJ]); w_bo_d = din("w_bo", [6144, D]); w_out_d = din("w_out", [D, D])
    w_eg_d = din("w_eg", [NE, D, 1024]); w_eu_d = din("w_eu", [NE, D, 1024]); w_ed_d = din("w_ed", [NE, 1024, D])
    w_r_d = din("w_r", [128, 16, 36])
    nm_d = din("nm_bc", [128, D]); nf_d = din("nf_bc", [128, D]); nl_d = din("nl_bc", [128, D])
    ssdn_d = din("ssdn_bc", [128, 4096]); dtb_d = din("dtb_bc", [128, 64]); alog_d = din("alog_bc", [128, 64])
    dsk_d = din("dsk_bc", [128, 4096])
    cw_d = din("cw_fm", [128, 48, 4]); cb_d = din("cb_fm", [128, 48]); scw_d = din("scw_fm", [128, 16, 3])
    ident_d = din("ident", [128, 128]); tri_d = din("tri", [2, 128, 128]); bones_d = din("bones", [2, 128, 128])
    onesj_d = din("onesj", [128, 16, 128]); maskj_d = din("maskj", [128, 16]); iota_d = din("iota", [128, 128])
    stri_d = din("stri", [128, 128])

    y_d = dout("y", [TM, D])
    o_ssm_p = dout("o_ssm_p", [64, 64, 128]); o_conv_p = dout("o_conv_p", [3, 6144]); o_sc_p = dout("o_sc_p", [2, 2048])
    o_ssm_s = dout("o_ssm_s", [16, 64, 64, 128]); o_conv_s = dout("o_conv_s", [48, 6144]); o_sc_s = dout("o_sc_s", [32, 2048])

    xbcT = dscr("xbcT", [6144, TM], F32); scT = dscr("scT", [6144, TM], F32)
    xbcT_p = dscr("xbcT_p", [5120, TP], F32)
    gateT = dscr("gateT", [4096, TM], F32); zs_s = dscr("zs", [TM, 4096], F32)
    x32_tm = dscr("x32_tm", [TM, 4096], F32)
    x_tm = dscr("x_tm", [TM, 4096], BF16); b_tm = dscr("b_tm", [TM, 1024], BF16)
    bT_s = dscr("bT", [1024, TM], BF16); cT_s = dscr("cT", [1024, TM], BF16)
    x_tm_p = dscr("x_tm_p", [TP, 4096], BF16); b_tm_p = dscr("b_tm_p", [TP, 1024], BF16)
    yaT = dscr("yaT", [4096, TM], BF16); ybT = dscr("ybT", [2048, TM], BF16)

    V, S, T, G, SY = "vector", "scalar", "tensor", "gpsimd", "sync"

    idf, b_idf = fw.alloc([128, 128], F32, "idf")
    idb, b_idb = fw.alloc([128, 128], BF16, "idb")
    tri, b_tri = fw.alloc([128, 2, 128], F32, "tri")
    bones, b_bones = fw.alloc([128, 2, 128], F32, "bones")
    flag, b_flag = fw.alloc([128, 1], F32, "flag")
    cw, b_cw = fw.alloc([128, 48, 4], F32, "cw")
    cb, b_cb = fw.alloc([128, 48], F32, "cb")
    scw, b_scw = fw.alloc([128, 16, 3], F32, "scw")
    dtraw, b_dtraw = fw.alloc([128, NTM, 64], F32, "dtraw")
    dtraw_p, b_dtraw_p = fw.alloc([128, NTP, 64], F32, "dtraw_p")
    histx, b_histx = fw.alloc([128, 48, 3], F32, "histx")
    histsc, b_histsc = fw.alloc([128, 32, 3], F32, "histsc")
    xtail, b_xtail = fw.alloc([128, 16, 3], BF16, "xtail")
    dtb, b_dtb = fw.alloc([128, 64], F32, "dtb")
    Abc, b_Abc = fw.alloc([128, 64], F32, "Abc")
    ones64, b_ones64 = fw.alloc([64, 128], F32, "ones64")
    for (ap, b, d) in ((idf, b_idf, ident_d), (flag, b_flag, flag_d), (cw, b_cw, cw_d), (cb, b_cb, cb_d),
                       (scw, b_scw, scw_d), (dtb, b_dtb, dtb_d), (Abc, b_Abc, alog_d)):
        fw.dma(SY, ap, d, dst=b)
    fw.dma(SY, tri, tri_d.rearrange("a p q -> p a q"), dst=b_tri)
    fw.dma(SY, bones, bones_d.rearrange("a p q -> p a q"), dst=b_bones)
    fw.op(V, lambda e: e.tensor_copy(out=idb, in_=idf), reads=[b_idf], writes=[b_idb])
    fw.op(S, lambda e: e.activation(out=Abc, in_=Abc, func=AF.Exp), reads=[b_Abc], writes=[b_Abc])
    fw.op(V, lambda e: e.tensor_scalar(out=Abc, in0=Abc, scalar1=-1.0, scalar2=None, op0=ALU.mult), reads=[b_Abc], writes=[b_Abc])
    fw.op(V, lambda e: e.memset(ones64, 1.0), writes=[b_ones64])

    PS = []
    for i in range(4):
        t = es.enter_context(nc.psum_tensor(f"ps{i}", [128, 1024], F32))
        PS.append((t, fw.buf(f"ps{i}a"), fw.buf(f"ps{i}b")))

    def bank(i):
        t, ba, bb = PS[i // 2]
        return (t[:, 0:512], ba) if i % 2 == 0 else (t[:, 512:1024], bb)

    def bank16(i):
        t, ba, bb = PS[i // 2]
        t16 = t.bitcast(BF16)
        return (t16[:, 0:1024], ba) if i % 2 == 0 else (t16[:, 1024:2048], bb)

    def phase_norm(x_d, ntiles, wbc, b_wbc, xnT, b_xnT, Ttot):
        fw.push()
        xr = fw.ring(2, [128, D], F32, "xr")
        xnr = fw.ring(2, [128, D], BF16, "xnr")
        sc = fw.ring(2, [128, 4], F32, "nsc")
        for i in range(ntiles):
            xt, bx = xr.next(); xn, bxn = xnr.next(); s4, bs = sc.next()
            fw.dma(SY, xt, x_d[i * 128:(i + 1) * 128, :], dst=bx)
            fw.op(V, lambda e, s4=s4: e.memset(s4[:, 0:1], 0.0), writes=[bs])
            fw.op(S, lambda e, xn=xn, xt=xt, s4=s4: e.activation(out=xn, in_=xt, func=AF.Square, accum_out=s4[:, 0:1]),
                  reads=[bx], writes=[bxn, bs])
            fw.op(V, lambda e, s4=s4: e.tensor_scalar(out=s4[:, 1:2], in0=s4[:, 0:1], scalar1=1.0 / D, scalar2=1e-6,
                                                      op0=ALU.mult, op1=ALU.add), reads=[bs], writes=[bs])
            fw.op(S, lambda e, s4=s4: e.activation(out=s4[:, 2:3], in_=s4[:, 1:2], func=AF.Sqrt), reads=[bs], writes=[bs])
            fw.op(V, lambda e, s4=s4: e.reciprocal(out=s4[:, 3:4], in_=s4[:, 2:3]), reads=[bs], writes=[bs])
            fw.op(V, lambda e, xn=xn, xt=xt, s4=s4: e.scalar_tensor_tensor(out=xn, in0=xt, scalar=s4[:, 3:4], in1=wbc,
                                                                         op0=ALU.mult, op1=ALU.mult),
                  reads=[bx, bs, b_wbc], writes=[bxn])
            for half in range(2):
                pt, bp = bank16(2 * (i % 2) + half)
                for k in range(8):
                    kk = half * 8 + k
                    fw.op(T, lambda e, pt=pt, xn=xn, k=k, kk=kk: e.transpose(out=pt[:, k * 128:(k + 1) * 128],
                                                                           in_=xn[:, kk * 128:(kk + 1) * 128], identity=idb),
                          reads=[bxn, b_idb], writes=[bp], signal=(k == 7))
                dst = xnT[:, half * 8:(half + 1) * 8, i * 128:(i + 1) * 128]
                src = pt.rearrange("p (a b) -> p a b", a=8)
                if half == 0:
                    fw.op(S, lambda e, dst=dst, src=src: e.copy(out=dst, in_=src), reads=[bp], writes=[b_xnT])
                else:
                    fw.op(V, lambda e, dst=dst, src=src: e.tensor_copy(out=dst, in_=src), reads=[bp], writes=[b_xnT])
        fw.pop()

    def proj_fm(w_ap, nk, c0, ncols, xT, b_xT, tblocks, evac, wr, extra=None):
        pi = 0
        for cb0 in range(c0, c0 + ncols, 512):
            wt, bw = wr.next()
            fw.dma(G, wt[:, 0:nk, :], w_ap.rearrange("(k p) c -> p k c", p=128)[:, :, cb0:cb0 + 512], dst=bw)
            for j in range(4):
                ch = (cb0 - c0) // 128 + j
                for (t0, tn) in tblocks:
                    pt, bp = bank(pi % 8); pi += 1
                    for k in range(nk):
                        fw.op(T, lambda e, pt=pt, wt=wt, k=k, j=j, t0=t0, tn=tn: e.matmul(
                            pt[:, 0:tn], lhsT=wt[:, k, j * 128:(j + 1) * 128], rhs=xT[:, k, t0:t0 + tn],
                            start=(k == 0), stop=(k == nk - 1)), reads=[bw, b_xT], writes=[bp], signal=(k == nk - 1))
                    evac(ch, t0, tn, pt, bp)
                if extra is not None:
                    extra(ch, wt, bw, j)

    def proj_tm(w_ap, nk, c0, ncols, xT, b_xT, ntiles, evac, wr):
        wt, bw = wr.next()
        fw.dma(G, wt[:, 0:nk, 0:ncols], w_ap.rearrange("(k p) c -> p k c", p=128)[:, :, c0:c0 + ncols], dst=bw)
        for i in range(ntiles):
            pt, bp = bank(i % 8)
            for k in range(nk):
                fw.op(T, lambda e, pt=pt, wt=wt, k=k, i=i: e.matmul(
                    pt[:, 0:ncols], lhsT=xT[:, k, i * 128:(i + 1) * 128], rhs=wt[:, k, 0:ncols],
                    start=(k == 0), stop=(k == nk - 1)), reads=[bw, b_xT], writes=[bp], signal=(k == nk - 1))
            evac(i, pt, bp)

    fw.push()
    wr = fw.ring(2, [128, 16, 512], BF16, "wr")
    nbc, b_nbc = fw.alloc([128, D], F32, "nbc")
    fw.dma(SY, nbc, nm_d, dst=b_nbc)

    def make_store_evac(scr, Ttot, dt_st, func=None):
        stg = fw.ring(3, [128, Ttot], dt_st, "stg")
        cur = {}

        def evac(ch, t0, tn, pt, bp):
            if t0 == 0:
                cur["s"] = stg.next()
            st, bs = cur["s"]
            if func is not None:
                fw.op(S, lambda e: e.activation(out=st[:, t0:t0 + tn], in_=pt[:, 0:tn], func=func), reads=[bp], writes=[bs])
            elif (ch + t0 // 128) % 2 == 0:
                fw.op(V, lambda e: e.tensor_copy(out=st[:, t0:t0 + tn], in_=pt[:, 0:tn]), reads=[bp], writes=[bs])
            else:
                fw.op(S, lambda e: e.copy(out=st[:, t0:t0 + tn], in_=pt[:, 0:tn]), reads=[bp], writes=[bs])
            if t0 + tn == Ttot:
                fw.dma(SY, scr[ch * 128:(ch + 1) * 128, :], st, src=bs)
        return evac

    if stage >= 1:
        fw.push()
        xnTp, b_xnTp = fw.alloc([128, 16, TP], BF16, "xnTp")
        phase_norm(xp_d, NTP, nbc, b_nbc, xnTp, b_xnTp, TP)
        fw.op(V, lambda e: e.tensor_copy(out=xtail, in_=xnTp[:, :, TP - 3:TP]), reads=[b_xnTp], writes=[b_xtail])
        fw.push()
        ev = make_store_evac(xbcT_p, TP, F32)
        proj_fm(w_in_d, 16, C_X, 5120, xnTp, b_xnTp, [(0, 512), (512, 512)], ev, wr)

        def ev_dt_p(i, pt, bp):
            fw.op(V, lambda e: e.tensor_copy(out=dtraw_p[:, i, :], in_=pt[:, 0:64]), reads=[bp], writes=[b_dtraw_p])
        proj_tm(w_in_d, 16, C_DT, 64, xnTp, b_xnTp, NTP, ev_dt_p, wr)
        fw.pop()
        fw.pop()
        fw.barrier()

    def phase_conv(src_scr, nchunks, main):
        fw.push()
        L = 3 + 1024 + (176 if main else 0)
        upr = fw.ring(2, [128, L], F32, "up")
        accr = fw.ring(2, [128, TM if main else TP], F32, "acc")
        xcr = fw.ring(2, [128, TM if main else TP], BF16, "xc")
        ttr = fw.ring(2, [128, NTM, 128], BF16, "tt")
        segb = {}
        if main:
            t32r = fw.ring(2, [128, NTM, 128], F32, "t32")
            stc, b_stc = fw.alloc([48, 6144], F32, "stc")
            fw.dma(SY, stc, st_conv_d, dst=b_stc)
            cvo, b_cvo = fw.alloc([51, 6144], F32, "cvo")
            cst_r = fw.ring(2, [128, 51], F32, "cst")
        ntl = NTM if main else NTP
        xdst = x_tm if main else x_tm_p
        bdst = b_tm if main else b_tm_p
        for c in range(nchunks):
            up, bu = upr.next(); acc, ba = accr.next(); xc, bxc = xcr.next()
            fw.dma(SY, up[:, 3:1027], src_scr[c * 128:(c + 1) * 128, 0:1024], dst=bu)
            if main:
                upS = up[:, 1027:1203].rearrange("p (j t) -> p j t", t=11)
                fw.dma(SY, upS[:, :, 3:11], src_scr[c * 128:(c + 1) * 128, 1024:1152].rearrange("p (j t) -> p j t", t=8), dst=bu)
                fw.op(V, lambda e, up=up, c=c: e.tensor_scalar(out=up[:, 0:3], in0=histx[:, c, :], scalar1=flag[:, 0:1],
                                                              scalar2=None, op0=ALU.mult), reads=[b_histx, b_flag], writes=[bu])
                pt, bp = bank(0)
                fw.op(T, lambda e, pt=pt, c=c: e.transpose(out=pt[:, 0:48], in_=stc[0:48, c * 128:(c + 1) * 128], identity=idf[0:48, 0:48]),
                      reads=[b_stc, b_idf], writes=[bp])
                fw.op(V, lambda e, pt=pt, upS=upS: e.tensor_copy(out=upS[:, :, 0:3], in_=pt[:, 0:48].rearrange("p (j t) -> p j t", t=3)),
                      reads=[bp], writes=[bu])
            else:
                fw.op(V, lambda e, up=up: e.memset(up[:, 0:3], 0.0), writes=[bu])
            if id(ba) not in segb:
                segb[id(ba)] = [fw.buf(f"accseg{q}") for q in range(3)]
            sb3 = segb[id(ba)]
            segs = [(up[:, 0:515], acc[:, 0:512], lambda a, k: a[:, k:k + 512], sb3[0]),
                    (up[:, 512:1027], acc[:, 512:1024], lambda a, k: a[:, k:k + 512], sb3[1])]
            if main:
                segs.append((upS, acc[:, 1024:1152].rearrange("p (j t) -> p j t", t=8), lambda a, k: a[:, :, k:k + 8], sb3[2]))
            for k in range(4):
                for (src, dst, sl, bseg) in segs:
                    if k == 0:
                        fw.op(S, lambda e, src=src, dst=dst, sl=sl, c=c: e.activation(
                            out=dst, in_=sl(src, 0), func=AF.Identity, scale=cw[:, c, 0:1], bias=cb[:, c:c + 1]),
                            reads=[bu, b_cw, b_cb], writes=[bseg])
                    else:
                        fw.op(V, lambda e, src=src, dst=dst, sl=sl, c=c, k=k: e.scalar_tensor_tensor(
                            out=dst, in0=sl(src, k), scalar=cw[:, c, k:k + 1], in1=dst, op0=ALU.mult, op1=ALU.add),
                            reads=[bu, b_cw, bseg], writes=[bseg])
            ba_all = sb3[0:len(segs)]
            if main and c < 32:
                fw.op(S, lambda e, acc=acc: e.activation(out=acc, in_=acc, func=AF.Silu), reads=ba_all, writes=ba_all)
                fw.op(V, lambda e, xc=xc, acc=acc: e.tensor_copy(out=xc, in_=acc), reads=ba_all, writes=[bxc])
            else:
                fw.op(S, lambda e, xc=xc, acc=acc: e.activation(out=xc, in_=acc, func=AF.Silu), reads=ba_all, writes=[bxc])
            if main and c < 32:
                t32, bt32 = t32r.next()
                for (b0, i0, i1) in ((4, 0, 4), (5, 4, 8), (6, 8, 9)):
                    pt, bp = bank(b0)
                    for i in range(i0, i1):
                        fw.op(T, lambda e, pt=pt, acc=acc, i=i, i0=i0: e.transpose(out=pt[:, (i - i0) * 128:(i - i0 + 1) * 128], in_=acc[:, i * 128:(i + 1) * 128], identity=idf),
                              reads=ba_all + [b_idf], writes=[bp], signal=(i == i1 - 1))
                    fw.op(V, lambda e, pt=pt, t32=t32, i0=i0, i1=i1: e.tensor_copy(out=t32[:, i0:i1, :], in_=pt[:, 0:(i1 - i0) * 128].rearrange("p (a b) -> p a b", b=128)),
                          reads=[bp], writes=[bt32])
                fw.dma(G, x32_tm.rearrange("(i p) c -> p i c", p=128)[:, :, c * 128:(c + 1) * 128], t32, src=bt32)
            if main:
                cst, bcs = cst_r.next()
                fw.op(V, lambda e, cst=cst, up=up: e.tensor_copy(out=cst[:, 0:3], in_=up[:, 1024:1027]), reads=[bu], writes=[bcs])
                fw.op(V, lambda e, cst=cst, upS=upS: e.tensor_copy(out=cst[:, 3:51].rearrange("p (j t) -> p j t", t=3), in_=upS[:, :, 8:11]),
                      reads=[bu], writes=[bcs])
                pt, bp = bank(1)
                fw.op(T, lambda e, pt=pt, cst=cst: e.transpose(out=pt[0:51, 0:128], in_=cst, identity=idf), reads=[bcs, b_idf], writes=[bp])
                fw.op(S, lambda e, pt=pt, c=c: e.copy(out=cvo[0:51, c * 128:(c + 1) * 128], in_=pt[0:51, 0:128]), reads=[bp], writes=[b_cvo])
            isx = c < 32
            isb = 32 <= c < 40
            if main and not isx:
                dsc = bT_s if isb else cT_s
                cc = c - 32 if isb else c - 40
                fw.dma(G, dsc[cc * 128:(cc + 1) * 128, :], xc, src=bxc)
            if isx or isb:
                tt, btt = ttr.next()
                for h in range(2):
                    n0 = h * 8
                    n1 = min(ntl, n0 + 8)
                    if n1 <= n0:
                        continue
                    pt, bp = bank16(2 + h)
                    for i in range(n0, n1):
                        fw.op(T, lambda e, pt=pt, xc=xc, i=i, n0=n0: e.transpose(out=pt[:, (i - n0) * 128:(i - n0 + 1) * 128],
                                                                              in_=xc[:, i * 128:(i + 1) * 128], identity=idb),
                              reads=[bxc, b_idb], writes=[bp], signal=(i == n1 - 1))
                    fw.op(S if h == 0 else V, (lambda e, pt=pt, tt=tt, n0=n0, n1=n1: (e.copy if False else e.tensor_copy)(
                        out=tt[:, n0:n1, :], in_=pt[:, 0:(n1 - n0) * 128].rearrange("p (a b) -> p a b", b=128))) if h == 1 else
                        (lambda e, pt=pt, tt=tt, n0=n0, n1=n1: e.copy(out=tt[:, n0:n1, :], in_=pt[:, 0:(n1 - n0) * 128].rearrange("p (a b) -> p a b", b=128))),
                        reads=[bp], writes=[btt])
                if isx:
                    fw.dma(G, xdst.rearrange("(i p) c -> p i c", p=128)[:, :, c * 128:(c + 1) * 128], tt[:, 0:ntl, :], src=btt)
                else:
                    cc = c - 32
                    fw.dma(G, bdst.rearrange("(i p) c -> p i c", p=128)[:, :, cc * 128:(cc + 1) * 128], tt[:, 0:ntl, :], src=btt)
        if main:
            fw.dma(G, o_conv_p, cvo[0:3, :], src=b_cvo)
            fw.dma(G, o_conv_s, cvo[3:51, :], src=b_cvo)
        fw.pop()
        fw.barrier()

    TB = [(0, 384), (384, 384), (768, 384)]
    if stage >= 3:
        xnT, b_xnT = fw.alloc([128, 16, TM], BF16, "xnT")
        phase_norm(xm_d, NTM, nbc, b_nbc, xnT, b_xnT, TM)

        def make_extra(hist, b_hist, ch_off):
            def extra(ch, wt, bw, j):
                pt, bp = bank(7)
                for k in range(16):
                    fw.op(T, lambda e, pt=pt, wt=wt, k=k, j=j: e.matmul(pt[:, 0:3], lhsT=wt[:, k, j * 128:(j + 1) * 128],
                                                                       rhs=xtail[:, k, 0:3], start=(k == 0), stop=(k == 15)),
                          reads=[bw, b_xtail], writes=[bp], signal=(k == 15))
                fw.op(V, lambda e, pt=pt, ch=ch: e.tensor_copy(out=hist[:, ch + ch_off, :], in_=pt[:, 0:3]), reads=[bp], writes=[b_hist])
            return extra

        fw.push()
        ev = make_store_evac(xbcT, TM, F32)
        proj_fm(w_in_d, 16, C_X, 6144, xnT, b_xnT, TB, ev, wr, extra=make_extra(histx, b_histx, 0))
        fw.pop()
        fw.push()
        ev = make_store_evac(scT, TM, F32)
        proj_fm(w_in_d, 16, C_SB, 2048, xnT, b_xnT, TB, ev, wr)
        ev2 = lambda ch, t0, tn, pt, bp: ev(ch + 16, t0, tn, pt, bp)
        proj_fm(w_in_d, 16, C_SC, 4096, xnT, b_xnT, TB, ev2, wr, extra=make_extra(histsc, b_histsc, 0))
        fw.pop()
        fw.push()
        ev = make_store_evac(gateT, TM, F32, func=AF.Sigmoid)
        proj_fm(w_in_d, 16, C_GA, 4096, xnT, b_xnT, TB, ev, wr)
        fw.pop()
        fw.push()
        zst = fw.ring(3, [128, 512], F32, "zst")
        for blk in range(8):
            def ev_z(i, pt, bp, blk=blk):
                st, bs = zst.next()
                fw.op(S, lambda e: e.activation(out=st, in_=pt[:, 0:512], func=AF.Silu), reads=[bp], writes=[bs])
                fw.dma(SY, zs_s[i * 128:(i + 1) * 128, blk * 512:(blk + 1) * 512], st, src=bs)
            proj_tm(w_in_d, 16, C_Z + blk * 512, 512, xnT, b_xnT, NTM, ev_z, wr)

        def ev_dt(i, pt, bp):
            fw.op(V, lambda e: e.tensor_copy(out=dtraw[:, i, :], in_=pt[:, 0:64]), reads=[bp], writes=[b_dtraw])
        proj_tm(w_in_d, 16, C_DT, 64, xnT, b_xnT, NTM, ev_dt, wr)
        fw.pop()
        fw.barrier()

    fw.pop()
    if stage >= 2:
        phase_conv(xbcT_p, 40, False)
    if stage >= 3:
        phase_conv(xbcT, 48, True)

    if stage >= 4:
        fw.push()
        L2 = 2 + 1024 + 160
        sts, b_sts = fw.alloc([32, 2048], F32, "sts")
        fw.dma(SY, sts, st_sc_d, dst=b_sts)
        sco, b_sco = fw.alloc([34, 2048], F32, "sco")
        inr = fw.ring(2, [128, 3, TM], F32, "scin")
        upr = fw.ring(2, [128, L2], F32, "up2")
        vr = fw.ring(2, [128, TM], F32, "scv")
        ybr = fw.ring(2, [128, TM], BF16, "yb")
        cs2r = fw.ring(2, [128, 34], F32, "cs2")
        segb2 = {}
        for c in range(16):
            it, bi = inr.next(); up, bu = upr.next(); v, bv = vr.next(); yb, byb = ybr.next(); cs2, bc2 = cs2r.next()
            for q in range(3):
                fw.dma(SY, it[:, q, :], scT[q * 2048 + c * 128:q * 2048 + (c + 1) * 128, :], dst=bi)
            upS = up[:, 1026:1186].rearrange("p (j t) -> p j t", t=10)
            fw.op(V, lambda e, up=up, it=it: e.tensor_tensor(out=up[:, 2:1026], in0=it[:, 1, 0:1024], in1=it[:, 2, 0:1024], op=ALU.mult),
                  reads=[bi], writes=[bu])
            fw.op(V, lambda e, upS=upS, it=it: e.tensor_tensor(out=upS[:, :, 2:10], in0=it[:, 1, 1024:1152].rearrange("p (j t) -> p j t", t=8),
                                                             in1=it[:, 2, 1024:1152].rearrange("p (j t) -> p j t", t=8), op=ALU.mult),
                  reads=[bi], writes=[bu])
            fw.op(V, lambda e, up=up, c=c: e.scalar_tensor_tensor(out=up[:, 0:2], in0=histsc[:, c, 1:3], scalar=flag[:, 0:1],
                                                                 in1=histsc[:, 16 + c, 1:3], op0=ALU.mult, op1=ALU.mult),
                  reads=[b_histsc, b_flag], writes=[bu])
            pt, bp = bank(c % 2)
            fw.op(T, lambda e, pt=pt, c=c: e.transpose(out=pt[:, 0:32], in_=sts[0:32, c * 128:(c + 1) * 128], identity=idf[0:32, 0:32]),
                  reads=[b_sts, b_idf], writes=[bp])
            fw.op(V, lambda e, pt=pt, upS=upS: e.tensor_copy(out=upS[:, :, 0:2], in_=pt[:, 0:32].rearrange("p (j t) -> p j t", t=2)),
                  reads=[bp], writes=[bu])
            if id(bv) not in segb2:
                segb2[id(bv)] = [fw.buf(f"vseg{q}") for q in range(3)]
            sb3 = segb2[id(bv)]
            segs = [(up[:, 0:514], v[:, 0:512], lambda a, k: a[:, k:k + 512], it[:, 0, 0:512], yb[:, 0:512], sb3[0]),
                    (up[:, 512:1026], v[:, 512:1024], lambda a, k: a[:, k:k + 512], it[:, 0, 512:1024], yb[:, 512:1024], sb3[1]),
                    (upS, v[:, 1024:1152].rearrange("p (j t) -> p j t", t=8), lambda a, k: a[:, :, k:k + 8],
                     it[:, 0, 1024:1152].rearrange("p (j t) -> p j t", t=8), yb[:, 1024:1152].rearrange("p (j t) -> p j t", t=8), sb3[2])]
            for k in range(3):
                for (src, dst, sl, bsrc, ydst, bseg) in segs:
                    if k == 0:
                        fw.op(S, lambda e, src=src, dst=dst, sl=sl, c=c: e.activation(out=dst, in_=sl(src, 0), func=AF.Identity, scale=scw[:, c, 0:1]),
                              reads=[bu, b_scw], writes=[bseg])
                    else:
                        fw.op(V, lambda e, src=src, dst=dst, sl=sl, c=c, k=k: e.scalar_tensor_tensor(
                            out=dst, in0=sl(src, k), scalar=scw[:, c, k:k + 1], in1=dst, op0=ALU.mult, op1=ALU.add),
                            reads=[bu, b_scw, bseg], writes=[bseg])
            for (src, dst, sl, bsrc, ydst, bseg) in segs:
                fw.op(V, lambda e, dst=dst, bsrc=bsrc, ydst=ydst: e.tensor_tensor(out=ydst, in0=dst, in1=bsrc, op=ALU.mult),
                      reads=[bseg, bi], writes=[byb])
            fw.dma(G, ybT[c * 128:(c + 1) * 128, :], yb, src=byb)
            fw.op(V, lambda e, cs2=cs2, up=up: e.tensor_copy(out=cs2[:, 0:2], in_=up[:, 1024:1026]), reads=[bu], writes=[bc2])
            fw.op(V, lambda e, cs2=cs2, upS=upS: e.tensor_copy(out=cs2[:, 2:34].rearrange("p (j t) -> p j t", t=2), in_=upS[:, :, 8:10]),
                  reads=[bu], writes=[bc2])
            pt, bp = bank(2 + c % 2)
            fw.op(T, lambda e, pt=pt, cs2=cs2: e.transpose(out=pt[0:34, 0:128], in_=cs2, identity=idf), reads=[bc2, b_idf], writes=[bp])
            fw.op(S, lambda e, pt=pt, c=c: e.copy(out=sco[0:34, c * 128:(c + 1) * 128], in_=pt[0:34, 0:128]), reads=[bp], writes=[b_sco])
        fw.dma(G, o_sc_p, sco[0:2, :], src=b_sco)
        fw.dma(G, o_sc_s, sco[2:34, :], src=b_sco)
        fw.pop()
        fw.barrier()

    def v3(ap, a):
        return ap.rearrange("p (a b) -> p a b", a=a)

    if stage >= 5:
        fw.push()
        ssdn, b_ssdn = fw.alloc([128, 4096], F32, "ssdn")
        fw.dma(SY, ssdn, ssdn_d, dst=b_ssdn)
        x32r = fw.ring(2, [128, 512], F32, "x32g")
        dskr = fw.ring(2, [128, 512], F32, "dskg")
        ztr = fw.ring(2, [128, 512], F32, "ztg")
        smr = fw.ring(2, [128, 12, 64], F32, "sm")
        csTr = fw.ring(2, [64, 128], F32, "csT")
        R4r = fw.ring(2, [64, 512], F32, "R4")
        t1r = fw.ring(8, [128, 128], F32, "t1")
        Er = fw.ring(8, [128, 128], F32, "E")
        LTr = fw.ring(8, [128, 128], BF16, "LT")
        CBr = fw.ring(2, [128, 128], F32, "CBm")
        ygr = fw.ring(2, [128, 512], F32, "yg")
        jkr = fw.ring(2, [128, 512], BF16, "jk")
        yar = fw.ring(2, [128, 512], BF16, "ya")
        ystr = fw.ring(2, [128, 4, 128], BF16, "yst")
        nscr = fw.ring(2, [128, 4], F32, "nsc2")

        def ssd_tile(i, kind, bufs, HT=None, HTb=None, bHT=None, bHTb=None, samp=None):
            pre = kind == "pre"
            do_y = not pre
            m = 1 if kind == "samp" else 0
            triM = tri[:, m, :]
            bonesM = bones[:, m, :]
            xsrc, bsrc = (x_tm_p, b_tm_p) if pre else (x_tm, b_tm)
            dtr, b_dtr = (dtraw_p, b_dtraw_p) if pre else (dtraw, b_dtraw)
            (xt, bxt), (bt, bbt), (bTt, bbT), (cTt, bcT), (xw, bxw) = bufs
            fw.dma(SY, xt, xsrc[i * 128:(i + 1) * 128, :], dst=bxt)
            fw.dma(SY, bt, bsrc[i * 128:(i + 1) * 128, :], dst=bbt)
            if do_y:
                fw.dma(SY, bTt, bT_s.rearrange("(g n) t -> n g t", n=128)[:, :, i * 128:(i + 1) * 128], dst=bbT)
                fw.dma(SY, cTt, cT_s.rearrange("(g n) t -> n g t", n=128)[:, :, i * 128:(i + 1) * 128], dst=bcT)
            sm, bsm = smr.next()
            v_, ab, l_, dt, a_, negcs, ecs, d1, wdt, dec = [sm[:, q, :] for q in range(10)]
            fw.op(V, lambda e: e.tensor_tensor(out=v_, in0=dtr[:, i, :], in1=dtb, op=ALU.add), reads=[b_dtr, b_dtb], writes=[bsm])
            fw.op(S, lambda e: e.activation(out=ab, in_=v_, func=AF.Abs), reads=[bsm], writes=[bsm])
            fw.op(S, lambda e: e.activation(out=ab, in_=ab, func=AF.Exp, scale=-1.0), reads=[bsm], writes=[bsm])
            fw.op(V, lambda e: e.tensor_scalar(out=ab, in0=ab, scalar1=1.0, scalar2=None, op0=ALU.add), reads=[bsm], writes=[bsm])
            fw.op(S, lambda e: e.activation(out=l_, in_=ab, func=AF.Ln), reads=[bsm], writes=[bsm])
            fw.op(V, lambda e: e.scalar_tensor_tensor(out=dt, in0=v_, scalar=0.0, in1=l_, op0=ALU.max, op1=ALU.add), reads=[bsm], writes=[bsm])
            fw.op(V, lambda e: e.tensor_tensor(out=a_, in0=dt, in1=Abc, op=ALU.mult), reads=[bsm, b_Abc], writes=[bsm])
            pt0, bp0 = bank(0)
            fw.op(T, lambda e: e.matmul(pt0[:, 0:64], lhsT=triM, rhs=a_, start=True, stop=True), reads=[b_tri, bsm], writes=[bp0], signal=False)
            fw.op(T, lambda e: e.matmul(pt0[:, 64:128], lhsT=bonesM, rhs=a_, start=True, stop=True), reads=[b_bones, bsm], writes=[bp0], signal=False)
            fw.op(T, lambda e: e.matmul(pt0[0:64, 128:256], lhsT=a_, rhs=triM, start=True, stop=True), reads=[b_tri, bsm], writes=[bp0])
            fw.op(V, lambda e: e.tensor_scalar(out=negcs, in0=pt0[:, 0:64], scalar1=-1.0, scalar2=None, op0=ALU.mult), reads=[bp0], writes=[bsm])
            fw.op(S, lambda e: e.activation(out=ecs, in_=pt0[:, 0:64], func=AF.Exp), reads=[bp0], writes=[bsm])
            fw.op(V, lambda e: e.tensor_tensor(out=d1, in0=pt0[:, 64:128], in1=negcs, op=ALU.add), reads=[bp0, bsm], writes=[bsm])
            fw.op(S, lambda e: e.activation(out=d1, in_=d1, func=AF.Exp), reads=[bsm], writes=[bsm])
            fw.op(V, lambda e: e.tensor_tensor(out=wdt, in0=d1, in1=dt, op=ALU.mult), reads=[bsm], writes=[bsm])
            fw.op(S, lambda e: e.activation(out=dec, in_=pt0[:, 64:128], func=AF.Exp), reads=[bp0], writes=[bsm])
            csT, bcsT = csTr.next()
            fw.op(S, lambda e: e.copy(out=csT, in_=pt0[0:64, 128:256]), reads=[bp0], writes=[bcsT])
            fw.op(V, lambda e: e.tensor_tensor(out=v3(xw, 64), in0=v3(xt, 64), in1=wdt.unsqueeze(2).broadcast_to([128, 64, 64]), op=ALU.mult),
                  reads=[bxt, bsm], writes=[bxw])
            if samp is not None:
                samp(i, a_, bsm, cTt, bcT, bt, bbt, xw, bxw)
            for g in range(8):
                if do_y:
                    ptb, bpb = bank(1)
                    fw.op(T, lambda e, g=g: e.matmul(ptb[:, 0:128], lhsT=bTt[:, g, :], rhs=cTt[:, g, :], start=True, stop=True),
                          reads=[bbT, bcT], writes=[bpb])
                    CBm, bCB = CBr.next()
                    fw.op(V, lambda e, CBm=CBm: e.tensor_tensor(out=CBm, in0=ptb[:, 0:128], in1=triM, op=ALU.mult), reads=[bpb, b_tri], writes=[bCB])
                    pty, bpy = bank(4)
                    hd = []
                    for half in range(2):
                        h0 = 8 * g + 4 * half
                        R4, bR4 = R4r.next()
                        fw.op(V, lambda e, R4=R4, h0=h0: e.tensor_tensor(out=v3(R4, 4), in0=idf[0:64, h0:h0 + 4].unsqueeze(2).broadcast_to([64, 4, 128]),
                                                                        in1=csT.unsqueeze(1).broadcast_to([64, 4, 128]), op=ALU.mult),
                              reads=[b_idf, bcsT], writes=[bR4])
                        ptc, bpc = bank(2 + half)
                        fw.op(T, lambda e, R4=R4, ptc=ptc: e.matmul(ptc[:, 0:512], lhsT=ones64, rhs=R4, start=True, stop=True),
                              reads=[b_ones64, bR4], writes=[bpc])
                        for hl in range(4):
                            h = h0 + hl
                            hg = 4 * half + hl
                            t1, bt1 = t1r.next(); E_, bE = Er.next()
                            fw.op(V, lambda e, t1=t1, ptc=ptc, hl=hl, h=h: e.tensor_scalar(out=t1, in0=ptc[:, hl * 128:(hl + 1) * 128], scalar1=negcs[:, h:h + 1],
                                                                                     scalar2=0.0, op0=ALU.add, op1=ALU.min), reads=[bpc, bsm], writes=[bt1])
                            fw.op(S, lambda e, t1=t1, E_=E_: e.activation(out=E_, in_=t1, func=AF.Exp), reads=[bt1], writes=[bE])
                            hd.append((h, hg, E_, bE))
                    for (h, hg, E_, bE) in hd:
                        LT, bLT = LTr.next()
                        fw.op(V, lambda e, E_=E_, LT=LT, CBm=CBm, h=h: e.scalar_tensor_tensor(out=LT, in0=E_, scalar=dt[:, h:h + 1], in1=CBm,
                                                                                       op0=ALU.mult, op1=ALU.mult), reads=[bE, bsm, bCB], writes=[bLT])
                        fw.op(T, lambda e, LT=LT, hg=hg, h=h: e.matmul(pty[:, hg * 64:(hg + 1) * 64], lhsT=LT, rhs=xt[:, h * 64:(h + 1) * 64], start=True, stop=True),
                              reads=[bLT, bxt], writes=[bpy])
                    pto, bpo = bank(5)
                    if kind == "main":
                        fw.op(T, lambda e, g=g: e.matmul(pto[:, 0:512], lhsT=cTt[:, g, :], rhs=HTb[:, g * 512:(g + 1) * 512], start=True, stop=True),
                              reads=[bcT, bHTb[g]], writes=[bpo])
                    else:
                        yoffT, byo = samp.yoffT
                        for u in range(4):
                            fw.op(T, lambda e, u=u, g=g: e.transpose(out=pto[:, u * 128:(u + 1) * 128], in_=yoffT[:, 4 * g + u, :], identity=idf),
                                  reads=[byo, b_idf], writes=[bpo], signal=(u == 3))
                    yg, byg = ygr.next()
                    fw.op(V, lambda e, yg=yg, g=g: e.tensor_tensor(out=v3(yg, 8), in0=v3(pto[:, 0:512], 8),
                                                                  in1=ecs[:, 8 * g:8 * g + 8].unsqueeze(2).broadcast_to([128, 8, 64]), op=ALU.mult),
                          reads=[bpo, bsm], writes=[byg])
                    fw.op(V, lambda e, yg=yg: e.tensor_tensor(out=yg, in0=yg, in1=pty[:, 0:512], op=ALU.add), reads=[byg, bpy], writes=[byg])
                    x32g, bx32 = x32r.next(); dskg, bdsk = dskr.next(); ztg, bztg = ztr.next()
                    fw.dma(SY, x32g, x32_tm[i * 128:(i + 1) * 128, g * 512:(g + 1) * 512], dst=bx32)
                    fw.dma(SY, dskg, dsk_d[:, g * 512:(g + 1) * 512], dst=bdsk)
                    fw.dma(SY, ztg, zs_s[i * 128:(i + 1) * 128, g * 512:(g + 1) * 512], dst=bztg)
                    fw.op(V, lambda e, x32g=x32g, dskg=dskg: e.tensor_tensor(out=x32g, in0=x32g, in1=dskg, op=ALU.mult), reads=[bx32, bdsk], writes=[bx32])
                    fw.op(V, lambda e, yg=yg, x32g=x32g: e.tensor_tensor(out=yg, in0=yg, in1=x32g, op=ALU.add), reads=[byg, bx32], writes=[byg])
                    fw.op(V, lambda e, yg=yg, ztg=ztg: e.tensor_tensor(out=yg, in0=yg, in1=ztg, op=ALU.mult), reads=[byg, bztg], writes=[byg])
                    ns, bns = nscr.next(); jk, bjk = jkr.next(); ya, bya = yar.next(); yst, byst = ystr.next()
                    fw.op(V, lambda e, ns=ns: e.memset(ns[:, 0:1], 0.0), writes=[bns])
                    fw.op(S, lambda e, jk=jk, yg=yg, ns=ns: e.activation(out=jk, in_=yg, func=AF.Square, accum_out=ns[:, 0:1]), reads=[byg], writes=[bjk, bns])
                    fw.op(V, lambda e, ns=ns: e.tensor_scalar(out=ns[:, 1:2], in0=ns[:, 0:1], scalar1=1.0 / 512, scalar2=1e-5, op0=ALU.mult, op1=ALU.add),
                          reads=[bns], writes=[bns])
                    fw.op(S, lambda e, ns=ns: e.activation(out=ns[:, 2:3], in_=ns[:, 1:2], func=AF.Sqrt), reads=[bns], writes=[bns])
                    fw.op(V, lambda e, ns=ns: e.reciprocal(out=ns[:, 3:4], in_=ns[:, 2:3]), reads=[bns], writes=[bns])
                    fw.op(V, lambda e, ns=ns, ya=ya, yg=yg, g=g: e.scalar_tensor_tensor(out=ya, in0=yg, scalar=ns[:, 3:4], in1=ssdn[:, g * 512:(g + 1) * 512],
                                                                                 op0=ALU.mult, op1=ALU.mult), reads=[byg, bns, b_ssdn], writes=[bya])
                    pt7, bp7 = bank16(7)
                    for u in range(4):
                        fw.op(T, lambda e, u=u, ya=ya: e.transpose(out=pt7[:, u * 128:(u + 1) * 128], in_=ya[:, u * 128:(u + 1) * 128], identity=idb),
                              reads=[bya, b_idb], writes=[bp7], signal=(u == 3))
                    fw.op(S, lambda e, yst=yst: e.copy(out=yst, in_=v3(pt7[:, 0:512], 4)), reads=[bp7], writes=[byst])
                    fw.dma(G, yaT[g * 512:(g + 1) * 512, i * 128:(i + 1) * 128].rearrange("(j p) t -> p j t", p=128), yst, src=byst)
                if kind != "samp":
                    pts, bps = bank(6)
                    fw.op(T, lambda e, g=g: e.matmul(pts[:, 0:512], lhsT=bt[:, g * 128:(g + 1) * 128], rhs=xw[:, g * 512:(g + 1) * 512], start=True, stop=True),
                          reads=[bbt, bxw], writes=[bps])
                    HTg = HT[:, g * 512:(g + 1) * 512]
                    fw.op(V, lambda e, HTg=HTg, g=g: e.tensor_tensor(out=v3(HTg, 8), in0=v3(HTg, 8),
                                                                    in1=dec[:, 8 * g:8 * g + 8].unsqueeze(2).broadcast_to([128, 8, 64]), op=ALU.mult),
                          reads=[bHT[g], bsm], writes=[bHT[g]])
                    fw.op(V, lambda e, HTg=HTg: e.tensor_tensor(out=HTg, in0=HTg, in1=pts[:, 0:512], op=ALU.add), reads=[bHT[g], bps], writes=[bHT[g]])
                    fw.op(S, lambda e, HTg=HTg, g=g: e.copy(out=HTb[:, g * 512:(g + 1) * 512], in_=HTg), reads=[bHT[g]], writes=[bHTb[g]])

        fw.push()
        HT, _ = fw.alloc([128, 4096], F32, "HT")
        HTb, _ = fw.alloc([128, 4096], BF16, "HTb")
        bHT = [fw.buf(f"HT{g}") for g in range(8)]
        bHTb = [fw.buf(f"HTb{g}") for g in range(8)]
        ldr = [fw.ring(2, sh, BF16, nm) for (sh, nm) in (([128, 4096], "xt"), ([128, 1024], "bt"), ([128, 8, 128], "bTt"), ([128, 8, 128], "cTt"))]
        xw1 = fw.alloc([128, 4096], BF16, "xw")
        hout, b_hout = fw.alloc([128, 32, 128], F32, "houtp")
        for g in range(8):
            fw.op(V, lambda e, g=g: e.memset(HT[:, g * 512:(g + 1) * 512], 0.0), writes=[bHT[g]])
            fw.op(V, lambda e, g=g: e.memset(HTb[:, g * 512:(g + 1) * 512], 0.0), writes=[bHTb[g]])
        for i in range(NTP if stage >= 5.1 else 1):
            ssd_tile(i, "pre", [r.next() for r in ldr] + [xw1], HT, HTb, bHT, bHTb)
        for g in range(8):
            HTg = HT[:, g * 512:(g + 1) * 512]
            fw.op(V, lambda e, HTg=HTg: e.tensor_scalar(out=HTg, in0=HTg, scalar1=flag[:, 0:1], scalar2=None, op0=ALU.mult), reads=[bHT[g], b_flag], writes=[bHT[g]])
            fw.op(S, lambda e, HTg=HTg, g=g: e.copy(out=HTb[:, g * 512:(g + 1) * 512], in_=HTg), reads=[bHT[g]], writes=[bHTb[g]])
        for i in range(8 if stage >= 5.3 else (1 if stage >= 5.2 else 0)):
            ssd_tile(i, "main", [r.next() for r in ldr] + [xw1], HT, HTb, bHT, bHTb)
        for q4 in range(8):
            ptx, bpx = bank(2 + q4 % 2)
            for u in range(4):
                hp = q4 * 4 + u
                fw.op(T, lambda e, ptx=ptx, u=u, hp=hp: e.transpose(out=ptx[:, u * 128:(u + 1) * 128], in_=HT[:, hp * 128:(hp + 1) * 128], identity=idf),
                      reads=[bHT[hp // 4], b_idf], writes=[bpx], signal=(u == 3))
            fw.op(S if q4 % 2 else V, (lambda e, ptx=ptx, q4=q4: e.copy(out=hout[:, q4 * 4:(q4 + 1) * 4, :], in_=v3(ptx[:, 0:512], 4))) if q4 % 2 else
                  (lambda e, ptx=ptx, q4=q4: e.tensor_copy(out=hout[:, q4 * 4:(q4 + 1) * 4, :], in_=v3(ptx[:, 0:512], 4))), reads=[bpx], writes=[b_hout])
        fw.dma(G, o_ssm_p.rearrange("h p n -> (h p) n").rearrange("(hp q) n -> q hp n", q=128), hout, src=b_hout)
        fw.pop()
        fw.barrier()

        fw.push()
        sbufs = [fw.alloc(sh, BF16, nm) for (sh, nm) in (([128, 4096], "sxt"), ([128, 1024], "sbt"), ([128, 8, 128], "sbTt"), ([128, 8, 128], "scTt"),
                                                         ([128, 4096], "sxw"))]
        onesj, b_onesj = fw.alloc([128, 16, 128], F32, "onesj")
        maskj, b_maskj = fw.alloc([128, 16], F32, "maskj")
        fw.dma(SY, onesj, onesj_d, dst=b_onesj)
        fw.dma(SY, maskj, maskj_d, dst=b_maskj)
        decs, b_decs = fw.alloc([128, 1024], F32, "decs")
        h0r = fw.ring(1, [128, 32, 128], F32, "h0f")
        h0T, b_h0T = fw.alloc([128, 4096], F32, "h0T")
        h0Tb, b_h0Tb = fw.alloc([128, 4096], BF16, "h0Tb")
        hnT, b_hnT = fw.alloc([128, 4096], F32, "hnT")
        yoffT, b_yoffT = fw.alloc([128, 32, 128], F32, "yoffT")
        bmjr = fw.ring(2, [128, 1024], BF16, "bmj")

        def samp(i, a_, bsm, cTt, bcT, bt, bbt, xw, bxw):
            if stage < 5.5:
                fw.op(V, lambda e: e.memset(yoffT, 0.0), writes=[b_yoffT])
                return
            PS0, ba0, bb0 = PS[0]
            for j in range(16):
                fw.op(T, lambda e, j=j: e.matmul(PS0[:, j * 64:(j + 1) * 64], lhsT=onesj[:, j, :], rhs=a_, start=True, stop=True),
                      reads=[b_onesj, bsm], writes=[ba0, bb0], signal=(j == 15))
            fw.op(S, lambda e: e.activation(out=decs[:, 0:512], in_=PS0[:, 0:512], func=AF.Exp), reads=[ba0], writes=[b_decs])
            fw.op(S, lambda e: e.activation(out=decs[:, 512:1024], in_=PS0[:, 512:1024], func=AF.Exp), reads=[bb0], writes=[b_decs])
            if stage < 5.52:
                fw.op(V, lambda e: e.memset(yoffT, 0.0), writes=[b_yoffT])
                return
            for j in range(16):
                h0f, bh = h0r.next()
                h0src = st_ssm_d[j].rearrange("h p n -> (h p) n").rearrange("(hp q) n -> q hp n", q=128)
                for q8 in range(4):
                    fw.dma(SY, h0f[:, q8 * 8:(q8 + 1) * 8, :], h0src[:, q8 * 8:(q8 + 1) * 8, :], dst=bh)
                if stage < 5.53:
                    if j == 0:
                        fw.op(V, lambda e: e.memset(yoffT, 0.0), writes=[b_yoffT])
                    continue
                for q4 in range(8):
                    ptx, bpx = bank(q4 % 4)
                    for u in range(4):
                        hp = q4 * 4 + u
                        fw.op(T, lambda e, ptx=ptx, u=u, hp=hp, h0f=h0f: e.transpose(out=ptx[:, u * 128:(u + 1) * 128], in_=h0f[:, hp, :], identity=idf),
                              reads=[bh, b_idf], writes=[bpx], signal=(u == 3))
                    fw.op(V, lambda e, ptx=ptx, q4=q4: e.tensor_copy(out=h0T[:, q4 * 512:(q4 + 1) * 512], in_=ptx[:, 0:512]), reads=[bpx], writes=[b_h0T])
                    fw.op(S, lambda e, q4=q4: e.activation(out=h0Tb[:, q4 * 512:(q4 + 1) * 512], in_=h0T[:, q4 * 512:(q4 + 1) * 512], func=AF.Identity), reads=[b_h0T], writes=[b_h0Tb])
                if stage < 5.6:
                    if j == 0:
                        fw.op(V, lambda e: e.memset(yoffT, 0.0), writes=[b_yoffT])
                    continue
                pto, bpo = bank(5)
                for hp in range(32):
                    fw.op(T, lambda e, hp=hp, j=j: e.matmul(pto[:, hp * 8:(hp + 1) * 8], lhsT=h0Tb[:, hp * 128:(hp + 1) * 128], rhs=cTt[:, hp // 4, 8 * j:8 * j + 8],
                                                          start=True, stop=True), reads=[b_h0Tb, bcT], writes=[bpo], signal=(hp == 31))
                fw.op(V, lambda e, j=j: e.tensor_copy(out=yoffT[:, :, 8 * j:8 * j + 8], in_=v3(pto[:, 0:256], 32)), reads=[bpo], writes=[b_yoffT])
                if stage < 5.7:
                    continue
                bmj, bbm = bmjr.next()
                fw.op(V, lambda e, bmj=bmj, j=j: e.tensor_scalar(out=bmj, in0=bt, scalar1=maskj[:, j:j + 1], scalar2=None, op0=ALU.mult), reads=[bbt, b_maskj], writes=[bbm])
                for g in range(8):
                    pts, bps = bank(6 + g % 2)
                    fw.op(T, lambda e, g=g, bmj=bmj, pts=pts: e.matmul(pts[:, 0:512], lhsT=bmj[:, g * 128:(g + 1) * 128], rhs=xw[:, g * 512:(g + 1) * 512], start=True, stop=True),
                          reads=[bbm, bxw], writes=[bps])
                    hng = hnT[:, g * 512:(g + 1) * 512]
                    fw.op(V, lambda e, hng=hng, g=g, j=j: e.tensor_tensor(out=v3(hng, 8), in0=v3(h0T[:, g * 512:(g + 1) * 512], 8),
                                                                        in1=decs[:, j * 64 + 8 * g:j * 64 + 8 * g + 8].unsqueeze(2).broadcast_to([128, 8, 64]), op=ALU.mult),
                          reads=[b_h0T, b_decs], writes=[b_hnT])
                    fw.op(V, lambda e, hng=hng, pts=pts: e.tensor_tensor(out=hng, in0=hng, in1=pts[:, 0:512], op=ALU.add), reads=[b_hnT, bps], writes=[b_hnT])
                ho, bho = h0r.next()
                for q4 in range(8):
                    ptx, bpx = bank(q4 % 4)
                    for u in range(4):
                        hp = q4 * 4 + u
                        fw.op(T, lambda e, ptx=ptx, u=u, hp=hp: e.transpose(out=ptx[:, u * 128:(u + 1) * 128], in_=hnT[:, hp * 128:(hp + 1) * 128], identity=idf),
                              reads=[b_hnT, b_idf], writes=[bpx], signal=(u == 3))
                    if q4 % 2:
                        fw.op(S, lambda e, ptx=ptx, q4=q4, ho=ho: e.copy(out=ho[:, q4 * 4:(q4 + 1) * 4, :], in_=v3(ptx[:, 0:512], 4)), reads=[bpx], writes=[bho])
                    else:
                        fw.op(V, lambda e, ptx=ptx, q4=q4, ho=ho: e.tensor_copy(out=ho[:, q4 * 4:(q4 + 1) * 4, :], in_=v3(ptx[:, 0:512], 4)), reads=[bpx], writes=[bho])
                fw.dma(G, o_ssm_s[j].rearrange("h p n -> (h p) n").rearrange("(hp q) n -> q hp n", q=128), ho, src=bho)
        samp.yoffT = (yoffT, b_yoffT)
        if stage >= 5.4:
            ssd_tile(8, "samp", sbufs, samp=samp)
        fw.pop()
        fw.pop()
        fw.barrier()

    def rms_stats(src, ns, bns, junk, bjunk, bsrc, eps, n):
        fw.op(V, lambda e: e.memset(ns[:, 0:1], 0.0), writes=[bns])
        fw.op(S, lambda e: e.activation(out=junk, in_=src, func=AF.Square, accum_out=ns[:, 0:1]), reads=(bsrc if isinstance(bsrc, list) else [bsrc]), writes=[bjunk, bns])
        fw.op(V, lambda e: e.tensor_scalar(out=ns[:, 1:2], in0=ns[:, 0:1], scalar1=1.0 / n, scalar2=eps, op0=ALU.mult, op1=ALU.add), reads=[bns], writes=[bns])
        fw.op(S, lambda e: e.activation(out=ns[:, 2:3], in_=ns[:, 1:2], func=AF.Sqrt), reads=[bns], writes=[bns])
        fw.op(V, lambda e: e.reciprocal(out=ns[:, 3:4], in_=ns[:, 2:3]), reads=[bns], writes=[bns])

    if stage >= 6:
        fw.push()
        offR1 = fw.off
        x2, _ = fw.alloc([128, NTM, D], F32, "x2")
        b_x2 = [[fw.buf(f"x2_{i}_{q}") for q in range(4)] for i in range(NTM)]
        yaTs = fw.alias(offR1, [128, 32, TM], BF16); b_yaTs = fw.buf("yaTs")
        offR2 = fw.off
        xn2, _ = fw.alloc([128, NTM, D], BF16, "xn2")
        b_xn2 = [fw.buf(f"xn2_{i}") for i in range(NTM)]
        ybTs = fw.alias(offR2, [128, 16, TM], BF16); b_ybTs = fw.buf("ybTs")
        offR3 = fw.off
        _r3, _ = fw.alloc([128, 20480], BF16, "R3")
        mT = fw.alias(offR3, [128, 16, TM], BF16)
        b_mT = [fw.buf(f"mT{d}") for d in range(16)]
        Mb_all, b_Mb = fw.alloc([128, NTM, 32], BF16, "Mb_all")
        rk_all, b_rk = fw.alloc([128, NTM, 32], F32, "rk_all")
        Wt_all, b_Wt = fw.alloc([128, NTM, 32], F32, "Wt_all")

        fw.push()
        wbr = fw.ring(2, [128, 48, 128], BF16, "wbo")
        gar = fw.ring(2, [128, 384], F32, "ga")
        gbr = fw.ring(2, [128, 384], F32, "gb")
        tmr = fw.ring(2, [128, 384], F32, "tmpm")
        tm2r = fw.ring(2, [128, 384], F32, "tmpm2")
        for k in range(32):
            fw.dma(SY, yaTs[:, k, :], yaT[k * 128:(k + 1) * 128, :], dst=b_yaTs)
        for k in range(16):
            fw.dma(SY, ybTs[:, k, :], ybT[k * 128:(k + 1) * 128, :], dst=b_ybTs)
        wbo_v = w_bo_d.rearrange("(k p) c -> p k c", p=128)
        pi = 0
        for d in range(16):
            wt, bw = wbr.next()
            fw.dma(G, wt, wbo_v[:, :, d * 128:(d + 1) * 128], dst=bw)
            for (t0, tn) in TB:
                ga, bga = gar.next(); gb, bgb = gbr.next()
                fw.dma(SY, ga, gateT[d * 128:(d + 1) * 128, t0:t0 + tn], dst=bga)
                fw.dma(SY, gb, gateT[2048 + d * 128:2048 + (d + 1) * 128, t0:t0 + tn], dst=bgb)
                pa, bpa = bank(pi % 8); pi += 1
                for k in range(32):
                    fw.op(T, lambda e, pa=pa, wt=wt, k=k, t0=t0, tn=tn: e.matmul(pa[:, 0:tn], lhsT=wt[:, k, :], rhs=yaTs[:, k, t0:t0 + tn], start=(k == 0), stop=(k == 31)),
                          reads=[bw, b_yaTs], writes=[bpa], signal=(k == 31))
                pb, bpb = bank(pi % 8); pi += 1
                for k in range(16):
                    fw.op(T, lambda e, pb=pb, wt=wt, k=k, t0=t0, tn=tn: e.matmul(pb[:, 0:tn], lhsT=wt[:, 32 + k, :], rhs=ybTs[:, k, t0:t0 + tn], start=(k == 0), stop=(k == 15)),
                          reads=[bw, b_ybTs], writes=[bpb], signal=(k == 15))
                tm, btm = tmr.next(); tm2, btm2 = tm2r.next()
                fw.op(V, lambda e, pa=pa, ga=ga, tm=tm, t0=t0, tn=tn: e.tensor_tensor(out=tm[:, 0:tn], in0=pa[:, 0:tn], in1=ga[:, 0:tn], op=ALU.mult),
                      reads=[bpa, bga], writes=[btm])
                fw.op(V, lambda e, pb=pb, gb=gb, tm2=tm2, t0=t0, tn=tn: e.tensor_tensor(out=tm2[:, 0:tn], in0=pb[:, 0:tn], in1=gb[:, 0:tn], op=ALU.mult),
                      reads=[bpb, bgb], writes=[btm2])
                fw.op(V, lambda e, tm=tm, tm2=tm2, d=d, t0=t0, tn=tn: e.tensor_tensor(out=mT[:, d, t0:t0 + tn], in0=tm[:, 0:tn], in1=tm2[:, 0:tn], op=ALU.add),
                      reads=[btm, btm2], writes=[b_mT[d]])
        fw.pop()
        fw.barrier()
        fw.push()
        wor = fw.ring(2, [128, 16, 512], BF16, "wo")
        xrr = fw.ring(2, [128, 512], F32, "xres")
        for dq in range(4):
            wo, bwo = wor.next()
            fw.dma(G, wo, w_out_d.rearrange("(k p) c -> p k c", p=128)[:, :, dq * 512:(dq + 1) * 512], dst=bwo)
            for i in range(NTM):
                xr, bxr = xrr.next()
                fw.dma(SY, xr, xm_d[i * 128:(i + 1) * 128, dq * 512:(dq + 1) * 512], dst=bxr)
                pt, bp = bank(pi % 8); pi += 1
                for k in range(16):
                    fw.op(T, lambda e, pt=pt, k=k, i=i, wo=wo: e.matmul(pt[:, 0:512], lhsT=mT[:, k, i * 128:(i + 1) * 128], rhs=wo[:, k, :],
                                                                     start=(k == 0), stop=(k == 15)), reads=[b_mT[k], bwo], writes=[bp], signal=(k == 15))
                c0 = dq * 512
                fw.op(V, lambda e, pt=pt, xr=xr, i=i, c0=c0: e.tensor_tensor(out=x2[:, i, c0:c0 + 512], in0=pt[:, 0:512], in1=xr, op=ALU.add),
                      reads=[bp, bxr], writes=[b_x2[i][dq]])
        fw.pop()
        fw.barrier()
        fw.push()
        nfb, b_nfb = fw.alloc([128, D], F32, "nfb")
        wr_sb, b_wr = fw.alloc([128, 16, 36], F32, "wr_sb")
        strb, b_strb = fw.alloc([128, 128], BF16, "strb")
        onesb, b_onesb = fw.alloc([128, 128], BF16, "onesb")
        fw.dma(SY, nfb, nf_d, dst=b_nfb)
        fw.dma(SY, wr_sb, w_r_d, dst=b_wr)
        fw.dma(G, strb, stri_d, dst=b_strb)
        fw.op(V, lambda e: e.memset(onesb, 1.0), writes=[b_onesb])
        xnf, b_xnf = fw.alloc([128, D], F32, "xnf")
        xfT, b_xfT = fw.alloc([128, 16, 128], F32, "xfT")
        rsr = fw.ring(2, [128, 160], F32, "rs")
        nsr = fw.ring(2, [128, 4], F32, "nsr")
        for i in range(NTM):
            ns, bns = nsr.next()
            rms_stats(x2[:, i, :], ns, bns, xn2[:, i, :], b_xn2[i], b_x2[i], 1e-6, D)
            fw.op(V, lambda e, ns=ns, i=i: e.scalar_tensor_tensor(out=xnf, in0=x2[:, i, :], scalar=ns[:, 3:4], in1=nfb, op0=ALU.mult, op1=ALU.mult),
                  reads=b_x2[i] + [bns, b_nfb], writes=[b_xnf])
            fw.op(V, lambda e, i=i: e.tensor_copy(out=xn2[:, i, :], in_=xnf), reads=[b_xnf], writes=[b_xn2[i]])
            for q in range(4):
                pt, bp = bank(pi % 8); pi += 1
                for u in range(4):
                    k = 4 * q + u
                    fw.op(T, lambda e, pt=pt, u=u, k=k: e.transpose(out=pt[:, u * 128:(u + 1) * 128], in_=xnf[:, k * 128:(k + 1) * 128], identity=idf),
                          reads=[b_xnf, b_idf], writes=[bp], signal=(u == 3))
                fw.op(V, lambda e, pt=pt, q=q: e.tensor_copy(out=xfT[:, 4 * q:4 * q + 4, :], in_=v3(pt[:, 0:512], 4)), reads=[bp], writes=[b_xfT])
            pt, bp = bank(pi % 8); pi += 1
            for k in range(16):
                fw.op(T, lambda e, pt=pt, k=k: e.matmul(pt[:, 0:36], lhsT=xfT[:, k, :], rhs=wr_sb[:, k, :], start=(k == 0), stop=(k == 15)),
                      reads=[b_xfT, b_wr], writes=[bp], signal=(k == 15))
            r, br = rsr.next()
            lg = r[:, 0:36]; mx = r[:, 36:37]; nmx = r[:, 37:38]; sg = r[:, 38:39]; pg = r[:, 39:40]; ohg = r[:, 40:44]; eg = r[:, 44:48]
            pen = r[:, 48:52]; m1 = r[:, 52:53]; m2 = r[:, 53:54]; dd = r[:, 54:55]; w1 = r[:, 55:56]; lfm = r[:, 56:88]; oh1 = r[:, 88:120]
            lf2 = r[:, 120:152]; w1p = r[:, 152:153]; w2p = r[:, 153:154]

            def vop(f, extra_r=(), extra_w=()):
                fw.op(V, f, reads=[br] + list(extra_r), writes=[br] + list(extra_w))
            fw.op(V, lambda e, pt=pt: e.tensor_copy(out=lg, in_=pt[:, 0:36]), reads=[bp], writes=[br])
            vop(lambda e: e.tensor_reduce(out=mx, in_=lg[:, 0:4], axis=AX.X, op=ALU.max))
            vop(lambda e: e.tensor_scalar(out=ohg, in0=lg[:, 0:4], scalar1=mx, scalar2=None, op0=ALU.is_equal))
            vop(lambda e: e.tensor_scalar(out=nmx, in0=mx, scalar1=-1.0, scalar2=None, op0=ALU.mult))
            fw.op(S, lambda e: e.activation(out=eg, in_=lg[:, 0:4], func=AF.Exp, bias=nmx, scale=1.0), reads=[br], writes=[br])
            vop(lambda e: e.reduce_sum(out=sg, in_=eg, axis=AX.X))
            vop(lambda e: e.reciprocal(out=pg, in_=sg))
            vop(lambda e: e.tensor_scalar(out=pen, in0=ohg, scalar1=-1.0, scalar2=1e30, op0=ALU.add, op1=ALU.mult))
            vop(lambda e: e.tensor_tensor(out=v3(lfm, 4), in0=v3(lg[:, 4:36], 4), in1=pen.unsqueeze(2).broadcast_to([128, 4, 8]), op=ALU.add))
            vop(lambda e: e.tensor_reduce(out=m1, in_=lfm, axis=AX.X, op=ALU.max))
            vop(lambda e: e.tensor_scalar(out=oh1, in0=lfm, scalar1=m1, scalar2=None, op0=ALU.is_equal))
            vop(lambda e: e.scalar_tensor_tensor(out=lf2, in0=oh1, scalar=-1e30, in1=lfm, op0=ALU.mult, op1=ALU.add))
            vop(lambda e: e.tensor_reduce(out=m2, in_=lf2, axis=AX.X, op=ALU.max))
            vop(lambda e: e.tensor_scalar(out=lfm, in0=lf2, scalar1=m2, scalar2=None, op0=ALU.is_equal))
            vop(lambda e: e.tensor_tensor(out=dd, in0=m2, in1=m1, op=ALU.subtract))
            fw.op(S, lambda e: e.activation(out=dd, in_=dd, func=AF.Exp), reads=[br], writes=[br])
            vop(lambda e: e.tensor_scalar(out=dd, in0=dd, scalar1=1.0, scalar2=None, op0=ALU.add))
            vop(lambda e: e.reciprocal(out=w1, in_=dd))
            vop(lambda e: e.tensor_tensor(out=w1p, in0=w1, in1=pg, op=ALU.mult))
            vop(lambda e: e.tensor_tensor(out=w2p, in0=pg, in1=w1p, op=ALU.subtract))
            vop(lambda e, i=i: e.tensor_scalar(out=Wt_all[:, i, :], in0=oh1, scalar1=w1p, scalar2=None, op0=ALU.mult), extra_w=[b_Wt])
            vop(lambda e, i=i: e.scalar_tensor_tensor(out=Wt_all[:, i, :], in0=lfm, scalar=w2p, in1=Wt_all[:, i, :], op0=ALU.mult, op1=ALU.add), extra_r=[b_Wt], extra_w=[b_Wt])
            vop(lambda e, i=i: e.tensor_tensor(out=Mb_all[:, i, :], in0=oh1, in1=lfm, op=ALU.add), extra_w=[b_Mb])
            pt2, bp2 = bank(pi % 8); pi += 1
            for ip in range(i):
                fw.op(T, lambda e, pt2=pt2, ip=ip: e.matmul(pt2[:, 0:32], lhsT=onesb, rhs=Mb_all[:, ip, :], start=(ip == 0), stop=False),
                      reads=[b_onesb, b_Mb], writes=[bp2], signal=False)
            fw.op(T, lambda e, pt2=pt2, i=i: e.matmul(pt2[:, 0:32], lhsT=strb, rhs=Mb_all[:, i, :], start=(i == 0), stop=True), reads=[b_strb, b_Mb], writes=[bp2])
            fw.op(V, lambda e, pt2=pt2, i=i: e.scalar_tensor_tensor(out=rk_all[:, i, :], in0=pt2[:, 0:32], scalar=1.0, in1=Mb_all[:, i, :], op0=ALU.add, op1=ALU.mult),
                  reads=[bp2, b_Mb], writes=[b_rk])
            fw.op(V, lambda e, i=i: e.tensor_scalar(out=rk_all[:, i, :], in0=rk_all[:, i, :], scalar1=-1.0, scalar2=None, op0=ALU.add), reads=[b_rk], writes=[b_rk])
        if debug:
            dbg_rk = dout("dbg_rk", [128, NTM * 32]); dbg_wt = dout("dbg_wt", [128, NTM * 32])
            fw.dma(SY, dbg_rk, rk_all.rearrange("p a b -> p (a b)"), src=b_rk)
            fw.dma(SY, dbg_wt, Wt_all.rearrange("p a b -> p (a b)"), src=b_Wt)
        fw.pop()
        fw.barrier()

    if stage >= 7:
        fw.push()
        iot, b_iot = fw.alloc([128, 128], F32, "iot")
        fw.dma(SY, iot, iota_d, dst=b_iot)
        Sr = fw.ring(2, [128, NTM, 128], BF16, "Ssel")
        SWr = fw.ring(1, [128, NTM, 128], BF16, "SWsel")
        SWTr = fw.ring(2, [128, NTM, 128], BF16, "SWT")
        xgr = fw.ring(1, [128, 16, 128], BF16, "xgT")
        xgmr = fw.ring(1, [128, 2048], BF16, "xgm")
        hsr = fw.ring(1, [128, 1024], F32, "hs")
        hbr = fw.ring(1, [128, 1024], BF16, "hb")
        hTr = fw.ring(2, [128, 8, 128], BF16, "hT")
        yer = fw.ring(1, [128, 2048], BF16, "yexp")
        wslots = [(fw.alias(offR3 + q * 8192, [128, 4096], BF16), fw.buf(f"wslot{q}")) for q in range(5)]
        wring = Ring(wslots)
        NEXP = NE if stage >= 7.5 else 2
        ci = 0
        for ex in range(NEXP):
            S_, bS = Sr.next(); SW, bSW = SWr.next(); SWT, bSWT = SWTr.next()
            for i in range(NTM):
                fw.op(V, lambda e, S_=S_, i=i, ex=ex: e.tensor_scalar(out=S_[:, i, :], in0=iot, scalar1=rk_all[:, i, ex:ex + 1], scalar2=None, op0=ALU.is_equal),
                      reads=[b_iot, b_rk], writes=[bS])
                fw.op(V, lambda e, SW=SW, i=i, ex=ex: e.tensor_scalar(out=SW[:, i, :], in0=iot, scalar1=rk_all[:, i, ex:ex + 1], scalar2=Wt_all[:, i, ex:ex + 1],
                                                                     op0=ALU.is_equal, op1=ALU.mult), reads=[b_iot, b_rk, b_Wt], writes=[bSW])
            for (i0, i1, bk) in ((0, 8, 0), (8, 9, 1)):
                pt, bp = bank16(bk)
                for i in range(i0, i1):
                    fw.op(T, lambda e, pt=pt, i=i, i0=i0, SW=SW: e.transpose(out=pt[:, (i - i0) * 128:(i - i0 + 1) * 128], in_=SW[:, i, :], identity=idb),
                          reads=[bSW, b_idb], writes=[bp], signal=(i == i1 - 1))
                fw.op(V, lambda e, pt=pt, i0=i0, i1=i1, SWT=SWT: e.tensor_copy(out=SWT[:, i0:i1, :], in_=v3(pt[:, 0:(i1 - i0) * 128], i1 - i0)), reads=[bp], writes=[bSWT])
            xgT, bxg = xgr.next()
            xg, bxgm = xgmr.next()
            for db in range(4):
                pt, bp = bank(2 + db % 2)
                for i in range(NTM):
                    fw.op(T, lambda e, pt=pt, i=i, db=db, S_=S_: e.matmul(pt[:, 0:512], lhsT=S_[:, i, :], rhs=xn2[:, i, db * 512:(db + 1) * 512],
                                                                       start=(i == 0), stop=(i == NTM - 1)),
                          reads=[b_xn2[i], bS], writes=[bp], signal=(i == NTM - 1))
                if db % 2 == 0:
                    fw.op(V, lambda e, pt=pt, db=db, xg=xg: e.tensor_copy(out=xg[:, db * 512:(db + 1) * 512], in_=pt[:, 0:512]), reads=[bp], writes=[bxgm])
                else:
                    fw.op(S, lambda e, pt=pt, db=db, xg=xg: e.activation(out=xg[:, db * 512:(db + 1) * 512], in_=pt[:, 0:512], func=AF.Identity), reads=[bp], writes=[bxgm])
            for half in range(2):
                pt, bp = bank16(2 + half)
                for u in range(8):
                    k = half * 8 + u
                    fw.op(T, lambda e, pt=pt, u=u, k=k, xg=xg: e.transpose(out=pt[:, u * 128:(u + 1) * 128], in_=xg[:, k * 128:(k + 1) * 128], identity=idb),
                          reads=[bxgm, b_idb], writes=[bp], signal=(u == 7))
                if half == 0:
                    fw.op(V, lambda e, pt=pt, xgT=xgT: e.tensor_copy(out=xgT[:, 0:8, :], in_=v3(pt[:, 0:1024], 8)), reads=[bp], writes=[bxg])
                else:
                    fw.op(S, lambda e, pt=pt, xgT=xgT: e.activation(out=xgT[:, 8:16, :], in_=v3(pt[:, 0:1024], 8), func=AF.Identity), reads=[bp], writes=[bxg])
            hgT, bhg0, bhg1 = PS[2]
            huT, bhu0, bhu1 = PS[3]
            for q in range(4):
                pieces = []
                for (wd_, acc, ba, bb) in ((w_eg_d, hgT, bhg0, bhg1), (w_eu_d, huT, bhu0, bhu1)):
                    wsl, bws = wring.next()
                    wv = v3(wsl, 4)
                    fw.dma(G, wv, wd_[ex].rearrange("(k p) c -> p k c", p=128)[:, 4 * q:4 * q + 4, :], dst=bws)
                    pieces.append((wv, bws, acc, ba, bb))
                for (wv, bws, acc, ba, bb) in pieces:
                    for kk in range(4):
                        k = 4 * q + kk
                        for half in range(2):
                            fw.op(T, lambda e, acc=acc, wv=wv, kk=kk, k=k, half=half, xgT=xgT: e.matmul(acc[:, half * 512:(half + 1) * 512], lhsT=xgT[:, k, :],
                                                                                                rhs=wv[:, kk, half * 512:(half + 1) * 512], start=(k == 0), stop=(k == 15)),
                                  reads=[bxg, bws], writes=[ba, bb], signal=(kk == 3 and half == 1))
            hs, bhs = hsr.next(); hb, bhb = hbr.next(); hT, bhT = hTr.next()
            for half, (ba, bb) in enumerate(((bhg0, bhu0), (bhg1, bhu1))):
                sl = slice(half * 512, (half + 1) * 512)
                fw.op(S, lambda e, sl=sl, hs=hs: e.activation(out=hs[:, sl], in_=hgT[:, sl], func=AF.Silu), reads=[ba], writes=[bhs])
                fw.op(V, lambda e, sl=sl, hs=hs, hb=hb: e.tensor_tensor(out=hb[:, sl], in0=hs[:, sl], in1=huT[:, sl], op=ALU.mult), reads=[bhs, bb], writes=[bhb])
            pt, bp = bank16(0)
            for k in range(8):
                fw.op(T, lambda e, pt=pt, k=k, hb=hb: e.transpose(out=pt[:, k * 128:(k + 1) * 128], in_=hb[:, k * 128:(k + 1) * 128], identity=idb),
                      reads=[bhb, b_idb], writes=[bp], signal=(k == 7))
            fw.op(V, lambda e, pt=pt, hT=hT: e.tensor_copy(out=hT, in_=v3(pt[:, 0:1024], 8)), reads=[bp], writes=[bhT])
            ydb = [bank(2), bank(3), bank(4), bank(5)]
            for q in range(4):
                wsl, bws = wring.next()
                wv = v3(wsl, 2)
                fw.dma(G, wv, w_ed_d[ex].rearrange("(k p) c -> p k c", p=128)[:, 2 * q:2 * q + 2, :], dst=bws)
                for kk in range(2):
                    k = 2 * q + kk
                    for db in range(4):
                        pt, bp = ydb[db]
                        fw.op(T, lambda e, pt=pt, wv=wv, kk=kk, k=k, db=db, hT=hT: e.matmul(pt[:, 0:512], lhsT=hT[:, k, :], rhs=wv[:, kk, db * 512:(db + 1) * 512],
                                                                                       start=(k == 0), stop=(k == 7)),
                              reads=[bhT, bws], writes=[bp], signal=(kk == 1 and db == 3))
            ye, bye = yer.next()
            for db in range(4):
                pt, bp = ydb[db]
                if db % 2 == 0:
                    fw.op(V, lambda e, pt=pt, db=db, ye=ye: e.tensor_copy(out=ye[:, db * 512:(db + 1) * 512], in_=pt[:, 0:512]), reads=[bp], writes=[bye])
                else:
                    fw.op(S, lambda e, pt=pt, db=db, ye=ye: e.activation(out=ye[:, db * 512:(db + 1) * 512], in_=pt[:, 0:512], func=AF.Identity), reads=[bp], writes=[bye])
            for i in range(NTM):
                for db in range(4):
                    pt, bp = bank((0, 1, 6, 7)[ci % 4]); ci += 1
                    fw.op(T, lambda e, pt=pt, i=i, db=db, SWT=SWT, ye=ye: e.matmul(pt[:, 0:512], lhsT=SWT[:, i, :], rhs=ye[:, db * 512:(db + 1) * 512], start=True, stop=True),
                          reads=[bSWT, bye], writes=[bp])
                    fw.op(V, lambda e, pt=pt, i=i, db=db: e.tensor_tensor(out=x2[:, i, db * 512:(db + 1) * 512], in0=x2[:, i, db * 512:(db + 1) * 512], in1=pt[:, 0:512], op=ALU.add),
                          reads=[bp, b_x2[i][db]], writes=[b_x2[i][db]])
        fw.pop()
        fw.barrier()

    if stage >= 6:
        fw.push()
        nlb, b_nlb = fw.alloc([128, D], F32, "nlb")
        fw.dma(SY, nlb, nl_d, dst=b_nlb)
        yor = fw.ring(2, [128, D], F32, "yo")
        nsr = fw.ring(2, [128, 4], F32, "nsr2")
        for i in range(NTM):
            yo, byo = yor.next(); ns, bns = nsr.next()
            rms_stats(x2[:, i, :], ns, bns, yo, byo, b_x2[i], 1e-6, D)
            fw.op(V, lambda e, yo=yo, ns=ns, i=i: e.scalar_tensor_tensor(out=yo, in0=x2[:, i, :], scalar=ns[:, 3:4], in1=nlb, op0=ALU.mult, op1=ALU.mult),
                  reads=b_x2[i] + [bns, b_nlb], writes=[byo])
            fw.dma(G, y_d[i * 128:(i + 1) * 128, :], yo, src=byo)
        fw.pop()
        fw.pop()
        fw.barrier()

    fw.emit()
    return nc, es


def _consts():
    t = np.arange(128)
    tri_p = (t[:, None] <= t[None, :]).astype(np.float32)
    same = (t[:, None] // 8 == t[None, :] // 8)
    tri_s = (tri_p * same).astype(np.float32)
    bones = np.stack([np.ones((128, 128), np.float32), same.astype(np.float32)])
    maskj = (t[:, None] // 8 == np.arange(16)[None, :]).astype(np.float32)
    onesj = np.ascontiguousarray(np.broadcast_to(maskj[:, :, None], (128, 16, 128))).astype(np.float32)
    return dict(ident=np.eye(128, dtype=np.float32), tri=np.stack([tri_p, tri_s]), bones=bones, onesj=onesj, maskj=maskj,
                iota=np.ascontiguousarray(np.broadcast_to(np.arange(128, dtype=np.float32)[None, :], (128, 128))),
                stri=(t[:, None] < t[None, :]).astype(np.float32))


def _bc(v, n=128):
    return np.ascontiguousarray(np.broadcast_to(np.asarray(v, np.float32).reshape(1, -1), (n, v.size)))


def make_in_maps(inp, cores=range(8)):
    f = lambda a: np.ascontiguousarray(np.asarray(a, dtype=np.float32))
    xpr, xs = f(inp["x_prompt"]), f(inp["x_sample"])
    shared = dict(
        w_in=f(inp["w_in"][0]), w_bo=f(inp["w_branch_out"][0]), w_out=f(inp["w_out"][0]),
        w_eg=f(inp["w_expert_gate"][0]), w_eu=f(inp["w_expert_up"][0]), w_ed=f(inp["w_expert_down"][0]),
        w_r=np.ascontiguousarray(np.concatenate([f(inp["w_router_coarse"][0]), f(inp["w_router_fine"][0])], axis=1)
                                 .reshape(16, 128, 36).transpose(1, 0, 2)),
        nm_bc=_bc(f(inp["norm_mixer"][0])), nf_bc=_bc(f(inp["norm_ffn"][0])), nl_bc=_bc(f(inp["norm_final"])),
        ssdn_bc=_bc(f(inp["ssd_norm"][0])), dtb_bc=_bc(f(inp["ssd_dt_bias"][0])), alog_bc=_bc(f(inp["ssd_a_log"][0])),
        dsk_bc=_bc(np.repeat(f(inp["ssd_d"][0]), 64)),
        cw_fm=np.ascontiguousarray(f(inp["ssd_conv_w"][0]).reshape(4, 48, 128).transpose(2, 1, 0)),
        cb_fm=np.ascontiguousarray(f(inp["ssd_conv_b"][0]).reshape(48, 128).T),
        scw_fm=np.ascontiguousarray(f(inp["sc_conv_w"][0]).reshape(3, 16, 128).transpose(2, 1, 0)),
        **_consts())
    maps = []
    for c in cores:
        s, h = c // 2, c % 2
        m = dict(shared)
        m["xm"] = np.concatenate([xpr[s, h * 1024:(h + 1) * 1024], xs[16 * c:16 * c + 16].reshape(128, D)], axis=0)
        m["xp"] = xpr[s, 0:1024]
        m["flag"] = np.full((128, 1), float(h), np.float32)
        m["st_ssm"] = f(inp["state_ssm"][0, 16 * c:16 * c + 16])
        m["st_conv"] = f(inp["state_ssd_conv"][0, 16 * c:16 * c + 16]).reshape(48, 6144)
        m["st_sc"] = f(inp["state_short_conv"][0, 16 * c:16 * c + 16]).reshape(32, 2048)
        maps.append(m)
    return maps


_CACHE = {}


def kernel(**inp):
    if "nc" not in _CACHE:
        _CACHE["nc"] = build_program()
    nc, _ = _CACHE["nc"]
    maps = make_in_maps(inp)
    res = run_bass_kernel_spmd(nc, maps, core_ids=list(range(8))).results
    y_prompt = np.zeros((4, 2048, D), np.float32)
    y_sample = np.zeros((128, 8, D), np.float32)
    p_ssm = np.zeros((1, 4, 64, 64, 128), np.float32)
    p_conv = np.zeros((1, 4, 3, 6144), np.float32)
    p_sc = np.zeros((1, 4, 2, 2048), np.float32)
    s_ssm = np.zeros((1, 128, 64, 64, 128), np.float32)
    s_conv = np.zeros((1, 128, 3, 6144), np.float32)
    s_sc = np.zeros((1, 128, 2, 2048), np.float32)
    for c in range(8):
        r = res[c]
        s, h = c // 2, c % 2
        y_prompt[s, h * 1024:(h + 1) * 1024] = r["y"][0:1024]
        y_sample[16 * c:16 * c + 16] = r["y"][1024:1152].reshape(16, 8, D)
        s_ssm[0, 16 * c:16 * c + 16] = r["o_ssm_s"]
        s_conv[0, 16 * c:16 * c + 16] = r["o_conv_s"].reshape(16, 3, 6144)
        s_sc[0, 16 * c:16 * c + 16] = r["o_sc_s"].reshape(16, 2, 2048)
        if h == 1:
            p_ssm[0, s] = r["o_ssm_p"]
            p_conv[0, s] = r["o_conv_p"]
            p_sc[0, s] = r["o_sc_p"]
    return (y_prompt, y_sample, p_ssm, p_conv, p_sc, s_ssm, s_conv, s_sc)
```

```python
import numpy as np
import concourse.bass as bass
import concourse.mybir as mybir
from concourse.bass_utils import run_bass_kernel_spmd

F32 = mybir.dt.float32
BF16 = mybir.dt.bfloat16
ALU = mybir.AluOpType
AF = mybir.ActivationFunctionType
AX = mybir.AxisListType
SAME_ENGINE_SYNC = True

D = 2048
TM, NTM = 1152, 9
TP, NTP = 1024, 8
NPROJ = 20544
C_GA, C_GB, C_Z, C_X, C_B, C_C, C_DT, C_SB, C_SC, C_SH = 0, 2048, 4096, 8192, 12288, 13312, 14336, 14400, 16448, 18496
NE, CAP = 32, 128


class Buf:
    __slots__ = ("name", "w", "r", "sem", "cnt")

    def __init__(self, name):
        self.name = name
        self.w = None
        self.r = []
        self.sem = None
        self.cnt = 0


class Eng:
    def __init__(self, name, sem):
        self.name = name
        self.sem = sem
        self.cnt = 0
        self.seen = {}
        self.prog = []
        self.pr = []
        self.pw = []


class FW:
    def __init__(self, nc, es):
        self.nc = nc
        self.es = es
        self.eng = {}
        for n in ("tensor", "vector", "scalar", "gpsimd", "sync"):
            self.eng[n] = Eng(n, self.new_sem("e_" + n))
        self.dma_sems = []
        self.semcnt = {}
        self.nbuf = 0
        self.big = es.enter_context(nc.sbuf_tensor("big", [128, 51200], F32))
        self.big16 = self.big.bitcast(BF16)
        self.off = 0
        self.marks = []
        self.free_sems = []

    def new_sem(self, name):
        return self.es.enter_context(self.nc.semaphore(name))

    def buf(self, name=None):
        self.nbuf += 1
        return Buf(name or f"b{self.nbuf}")

    def push(self):
        self.marks.append((self.off, []))

    def pop(self):
        self.off, bufs = self.marks.pop()
        for b in bufs:
            if b.sem is not None:
                self.free_sems.append(b.sem)
                b.sem = None

    def alloc(self, shape, dt, name=None):
        n = 1
        for s in shape[1:]:
            n *= s
        esz = 4 if dt == F32 else 2
        nbytes = (n * esz + 31) // 32 * 32
        assert self.off + nbytes <= 51200 * 4, f"SBUF overflow {name} {self.off} + {nbytes}"
        if dt == F32:
            o = self.off // 4
            ap = self.big[0:shape[0], o:o + n]
        else:
            o = self.off // 2
            ap = self.big16[0:shape[0], o:o + n]
        self.off += nbytes
        bufobj = self.buf(name)
        if self.marks:
            self.marks[-1][1].append(bufobj)
        if len(shape) == 3:
            ap = ap.rearrange("p (a b) -> p a b", a=shape[1])
        elif len(shape) == 4:
            ap = ap.rearrange("p (a b c) -> p a b c", a=shape[1], b=shape[2])
        return ap, bufobj

    def alias(self, off, shape, dt):
        n = 1
        for x in shape[1:]:
            n *= x
        if dt == F32:
            ap = self.big[0:shape[0], off // 4:off // 4 + n]
        else:
            ap = self.big16[0:shape[0], off // 2:off // 2 + n]
        if len(shape) == 3:
            ap = ap.rearrange("p (a b) -> p a b", a=shape[1])
        return ap

    def ring(self, n, shape, dt, name=None):
        return Ring([self.alloc(shape, dt, f"{name}{i}") for i in range(n)])

    def _need(self, E, tks):
        best = {}
        for (s, v) in tks:
            k = id(s)
            if k not in best or best[k][1] < v:
                best[k] = (s, v)
        for k, (s, v) in best.items():
            if (s is E.sem) and not SAME_ENGINE_SYNC:
                continue
            if E.seen.get(k, 0) >= v:
                continue
            E.seen[k] = v
            E.prog.append(lambda e, s=s, v=v: e.wait_ge(s, v))

    def _chk(self, en, reads, writes):
        for n2, E2 in self.eng.items():
            if n2 == en:
                continue
            for b in writes:
                assert all(b is not p for p in E2.pr) and all(b is not p for p in E2.pw), f"pending hazard {b.name} {n2}"
            for b in reads:
                assert all(b is not p for p in E2.pw), f"pending hazard {b.name} {n2}"

    def op(self, en, build, reads=(), writes=(), signal=True):
        E = self.eng[en]
        self._chk(en, reads, writes)
        tks = []
        for b in reads:
            if b.w:
                tks.append(b.w)
        for b in writes:
            if b.w:
                tks.append(b.w)
            tks.extend(b.r)
        self._need(E, tks)
        if signal:
            E.cnt += 1
            tk = (E.sem, E.cnt)
            sem = E.sem
            E.prog.append(lambda e, build=build, sem=sem: build(e).then_inc(sem, 1))
            rs = list(reads) + E.pr
            ws = list(writes) + E.pw
            E.pr, E.pw = [], []
            for b in ws:
                b.w = tk
                b.r = []
            for b in rs:
                if b.w is not tk:
                    b.r.append(tk)
        else:
            E.prog.append(lambda e, build=build: build(e))
            E.pr.extend(reads)
            E.pw.extend(writes)

    def dma(self, en, out, in_, src=None, dst=None, owner=None):
        E = self.eng[en]
        assert not E.pr and not E.pw
        self._chk(en, [src] if src is not None else [], [dst] if dst is not None else [])
        tks = []
        if src is not None and src.w:
            tks.append(src.w)
        if dst is not None:
            if dst.w:
                tks.append(dst.w)
            tks.extend(dst.r)
        self._need(E, tks)
        if owner is None:
            owner = dst if dst is not None else src
        if owner.sem is None:
            if self.free_sems:
                owner.sem = self.free_sems.pop()
            else:
                owner.sem = self.new_sem(f"d{len(self.semcnt)}")
                self.semcnt[id(owner.sem)] = [owner.sem, 0]
        ent = self.semcnt[id(owner.sem)]
        ent[1] += 16
        tk = (owner.sem, ent[1])
        sem = owner.sem
        E.prog.append(lambda e, out=out, in_=in_, sem=sem: e.dma_start(out=out, in_=in_).then_inc(sem, 16))
        if dst is not None:
            dst.w = tk
            dst.r = []
        if src is not None:
            src.r.append(tk)

    def barrier(self):
        tks = [(E.sem, E.cnt) for E in self.eng.values() if E.cnt > 0]
        tks += [(s_, c_) for (s_, c_) in self.semcnt.values()]
        for E in self.eng.values():
            assert not E.pr and not E.pw, E.name
            self._need(E, tks)

    def emit(self):
        self.barrier()
        with self.nc.Block() as block:
            for en in ("tensor", "vector", "scalar", "gpsimd", "sync"):
                prog = self.eng[en].prog

                def body(e, prog=prog):
                    for f in prog:
                        f(e)
                getattr(block, en)(body)


class Ring:
    def __init__(self, items):
        self.items = items
        self.i = 0

    def next(self):
        r = self.items[self.i % len(self.items)]
        self.i += 1
        return r


def build_program(stage=99, debug=False):
    import contextlib
    nc = bass.Bass("TRN2", target_bir_lowering=False)
    es = contextlib.ExitStack()
    fw = FW(nc, es)

    def din(name, shape, dt=F32):
        return nc.dram_tensor(name, list(shape), dt, kind="ExternalInput").ap()

    def dout(name, shape, dt=F32):
        return nc.dram_tensor(name, list(shape), dt, kind="ExternalOutput").ap()

    def dscr(name, shape, dt):
        return nc.dram_tensor(name, list(shape), dt, kind="Internal").ap()

    xm_d = din("xm", [TM, D]); xp_d = din("xp", [TP, D]); flag_d = din("flag", [128, 1])
    st_ssm_d = din("st_ssm", [16, 64, 64, 128]); st_conv_d = din("st_conv", [48, 6144]); st_sc_d = din("st_sc", [32, 2048])
    w_in_d = din("w_in", [D, NPROJ]); w_bo_d = din("w_bo", [6144, D]); w_out_d = din("w_out", [D, D])
    w_eg_d = din("w_eg", [NE, D, 1024]); w_eu_d = din("w_eu", [NE, D, 1024]); w_ed_d = din("w_ed", [NE, 1024, D])
    w_r_d = din("w_r", [128, 16, 36])
    nm_d = din("nm_bc", [128, D]); nf_d = din("nf_bc", [128, D]); nl_d = din("nl_bc", [128, D])
    ssdn_d = din("ssdn_bc", [128, 4096]); dtb_d = din("dtb_bc", [128, 64]); alog_d = din("alog_bc", [128, 64])
    dsk_d = din("dsk_bc", [128, 4096])
    cw_d = din("cw_fm", [128, 48, 4]); cb_d = din("cb_fm", [128, 48]); scw_d = din("scw_fm", [128, 16, 3])
    ident_d = din("ident", [128, 128]); tri_d = din("tri", [2, 128, 128]); bones_d = din("bones", [2, 128, 128])
    onesj_d = din("onesj", [128, 16, 128]); maskj_d = din("maskj", [128, 16]); iota_d = din("iota", [128, 128])
    stri_d = din("stri", [128, 128])

    y_d = dout("y", [TM, D])
    o_ssm_p = dout("o_ssm_p", [64, 64, 128]); o_conv_p = dout("o_conv_p", [3, 6144]); o_sc_p = dout("o_sc_p", [2, 2048])
    o_ssm_s = dout("o_ssm_s", [16, 64, 64, 128]); o_conv_s = dout("o_conv_s", [48, 6144]); o_sc_s = dout("o_sc_s", [32, 2048])

    xbcT = dscr("xbcT", [6144, TM], F32); scT = dscr("scT", [6144, TM], F32)
    xbcT_p = dscr("xbcT_p", [5120, TP], F32)
    gateT = dscr("gateT", [4096, TM], F32); zs_s = dscr("zs", [TM, 4096], F32)
    x32_tm = dscr("x32_tm", [TM, 4096], F32)
    x_tm = dscr("x_tm", [TM, 4096], BF16); b_tm = dscr("b_tm", [TM, 1024], BF16)
    bT_s = dscr("bT", [1024, TM], BF16); cT_s = dscr("cT", [1024, TM], BF16)
    x_tm_p = dscr("x_tm_p", [TP, 4096], BF16); b_tm_p = dscr("b_tm_p", [TP, 1024], BF16)
    yaT = dscr("yaT", [4096, TM], BF16); ybT = dscr("ybT", [2048, TM], BF16)

    V, S, T, G, SY = "vector", "scalar", "tensor", "gpsimd", "sync"

    idf, b_idf = fw.alloc([128, 128], F32, "idf")
    idb, b_idb = fw.alloc([128, 128], BF16, "idb")
    tri, b_tri = fw.alloc([128, 2, 128], F32, "tri")
    bones, b_bones = fw.alloc([128, 2, 128], F32, "bones")
    flag, b_flag = fw.alloc([128, 1], F32, "flag")
    cw, b_cw = fw.alloc([128, 48, 4], F32, "cw")
    cb, b_cb = fw.alloc([128, 48], F32, "cb")
    scw, b_scw = fw.alloc([128, 16, 3], F32, "scw")
    dtraw, b_dtraw = fw.alloc([128, NTM, 64], F32, "dtraw")
    dtraw_p, b_dtraw_p = fw.alloc([128, NTP, 64], F32, "dtraw_p")
    histx, b_histx = fw.alloc([128, 48, 3], F32, "histx")
    histsc, b_histsc = fw.alloc([128, 32, 3], F32, "histsc")
    xtail, b_xtail = fw.alloc([128, 16, 3], BF16, "xtail")
    dtb, b_dtb = fw.alloc([128, 64], F32, "dtb")
    Abc, b_Abc = fw.alloc([128, 64], F32, "Abc")
    ones64, b_ones64 = fw.alloc([64, 128], F32, "ones64")
    for (ap, b, d) in ((idf, b_idf, ident_d), (flag, b_flag, flag_d), (cw, b_cw, cw_d), (cb, b_cb, cb_d),
                       (scw, b_scw, scw_d), (dtb, b_dtb, dtb_d), (Abc, b_Abc, alog_d)):
        fw.dma(SY, ap, d, dst=b)
    fw.dma(SY, tri, tri_d.rearrange("a p q -> p a q"), dst=b_tri)
    fw.dma(SY, bones, bones_d.rearrange("a p q -> p a q"), dst=b_bones)
    fw.op(V, lambda e: e.tensor_copy(out=idb, in_=idf), reads=[b_idf], writes=[b_idb])
    fw.op(S, lambda e: e.activation(out=Abc, in_=Abc, func=AF.Exp), reads=[b_Abc], writes=[b_Abc])
    fw.op(V, lambda e: e.tensor_scalar(out=Abc, in0=Abc, scalar1=-1.0, scalar2=None, op0=ALU.mult), reads=[b_Abc], writes=[b_Abc])
    fw.op(V, lambda e: e.memset(ones64, 1.0), writes=[b_ones64])

    PS = []
    for i in range(4):
        t = es.enter_context(nc.psum_tensor(f"ps{i}", [128, 1024], F32))
        PS.append((t, fw.buf(f"ps{i}a"), fw.buf(f"ps{i}b")))

    def bank(i):
        t, ba, bb = PS[i // 2]
        return (t[:, 0:512], ba) if i % 2 == 0 else (t[:, 512:1024], bb)

    def bank16(i):
        t, ba, bb = PS[i // 2]
        t16 = t.bitcast(BF16)
        return (t16[:, 0:1024], ba) if i % 2 == 0 else (t16[:, 1024:2048], bb)

    def phase_norm(x_d, ntiles, wbc, b_wbc, xnT, b_xnT, Ttot):
        fw.push()
        xr = fw.ring(2, [128, D], F32, "xr")
        xnr = fw.ring(2, [128, D], BF16, "xnr")
        sc = fw.ring(2, [128, 4], F32, "nsc")
        for i in range(ntiles):
            xt, bx = xr.next(); xn, bxn = xnr.next(); s4, bs = sc.next()
            fw.dma(SY, xt, x_d[i * 128:(i + 1) * 128, :], dst=bx)
            fw.op(V, lambda e, s4=s4: e.memset(s4[:, 0:1], 0.0), writes=[bs])
            fw.op(S, lambda e, xn=xn, xt=xt, s4=s4: e.activation(out=xn, in_=xt, func=AF.Square, accum_out=s4[:, 0:1]),
                  reads=[bx], writes=[bxn, bs])
            fw.op(V, lambda e, s4=s4: e.tensor_scalar(out=s4[:, 1:2], in0=s4[:, 0:1], scalar1=1.0 / D, scalar2=1e-6,
                                                      op0=ALU.mult, op1=ALU.add), reads=[bs], writes=[bs])
            fw.op(S, lambda e, s4=s4: e.activation(out=s4[:, 2:3], in_=s4[:, 1:2], func=AF.Sqrt), reads=[bs], writes=[bs])
            fw.op(V, lambda e, s4=s4: e.reciprocal(out=s4[:, 3:4], in_=s4[:, 2:3]), reads=[bs], writes=[bs])
            fw.op(V, lambda e, xn=xn, xt=xt, s4=s4: e.scalar_tensor_tensor(out=xn, in0=xt, scalar=s4[:, 3:4], in1=wbc,
                                                                         op0=ALU.mult, op1=ALU.mult),
                  reads=[bx, bs, b_wbc], writes=[bxn])
            for half in range(2):
                pt, bp = bank16(2 * (i % 2) + half)
                for k in range(8):
                    kk = half * 8 + k
                    fw.op(T, lambda e, pt=pt, xn=xn, k=k, kk=kk: e.transpose(out=pt[:, k * 128:(k + 1) * 128],
                                                                           in_=xn[:, kk * 128:(kk + 1) * 128], identity=idb),
                          reads=[bxn, b_idb], writes=[bp], signal=(k == 7))
                dst = xnT[:, half * 8:(half + 1) * 8, i * 128:(i + 1) * 128]
                src = pt.rearrange("p (a b) -> p a b", a=8)
                if half == 0:
                    fw.op(S, lambda e, dst=dst, src=src: e.copy(out=dst, in_=src), reads=[bp], writes=[b_xnT])
                else:
                    fw.op(V, lambda e, dst=dst, src=src: e.tensor_copy(out=dst, in_=src), reads=[bp], writes=[b_xnT])
        fw.pop()

    def proj_fm(w_ap, nk, c0, ncols, xT, b_xT, tblocks, evac, wr, extra=None):
        pi = 0
        for cb0 in range(c0, c0 + ncols, 512):
            wt, bw = wr.next()
            fw.dma(G, wt[:, 0:nk, :], w_ap.rearrange("(k p) c -> p k c", p=128)[:, :, cb0:cb0 + 512], dst=bw)
            for j in range(4):
                ch = (cb0 - c0) // 128 + j
                for (t0, tn) in tblocks:
                    pt, bp = bank(pi % 8); pi += 1
                    for k in range(nk):
                        fw.op(T, lambda e, pt=pt, wt=wt, k=k, j=j, t0=t0, tn=tn: e.matmul(
                            pt[:, 0:tn], lhsT=wt[:, k, j * 128:(j + 1) * 128], rhs=xT[:, k, t0:t0 + tn],
                            start=(k == 0), stop=(k == nk - 1)), reads=[bw, b_xT], writes=[bp], signal=(k == nk - 1))
                    evac(ch, t0, tn, pt, bp)
                if extra is not None:
                    extra(ch, wt, bw, j)

    def proj_tm(w_ap, nk, c0, ncols, xT, b_xT, ntiles, evac, wr):
        wt, bw = wr.next()
        fw.dma(G, wt[:, 0:nk, 0:ncols], w_ap.rearrange("(k p) c -> p k c", p=128)[:, :, c0:c0 + ncols], dst=bw)
        for i in range(ntiles):
            pt, bp = bank(i % 8)
            for k in range(nk):
                fw.op(T, lambda e, pt=pt, wt=wt, k=k, i=i: e.matmul(
                    pt[:, 0:ncols], lhsT=xT[:, k, i * 128:(i + 1) * 128], rhs=wt[:, k, 0:ncols],
                    start=(k == 0), stop=(k == nk - 1)), reads=[bw, b_xT], writes=[bp], signal=(k == nk - 1))
            evac(i, pt, bp)

    fw.push()
    wr = fw.ring(2, [128, 16, 512], BF16, "wr")
    nbc, b_nbc = fw.alloc([128, D], F32, "nbc")
    fw.dma(SY, nbc, nm_d, dst=b_nbc)

    def make_store_evac(scr, Ttot, dt_st, func=None):
        stg = fw.ring(3, [128, Ttot], dt_st, "stg")
        cur = {}

        def evac(ch, t0, tn, pt, bp):
            if t0 == 0:
                cur["s"] = stg.next()
            st, bs = cur["s"]
            if func is not None:
                fw.op(S, lambda e: e.activation(out=st[:, t0:t0 + tn], in_=pt[:, 0:tn], func=func), reads=[bp], writes=[bs])
            elif (ch + t0 // 128) % 2 == 0:
                fw.op(V, lambda e: e.tensor_copy(out=st[:, t0:t0 + tn], in_=pt[:, 0:tn]), reads=[bp], writes=[bs])
            else:
                fw.op(S, lambda e: e.copy(out=st[:, t0:t0 + tn], in_=pt[:, 0:tn]), reads=[bp], writes=[bs])
            if t0 + tn == Ttot:
                fw.dma(SY, scr[ch * 128:(ch + 1) * 128, :], st, src=bs)
        return evac

    if stage >= 1:
        fw.push()
        xnTp, b_xnTp = fw.alloc([128, 16, TP], BF16, "xnTp")
        phase_norm(xp_d, NTP, nbc, b_nbc, xnTp, b_xnTp, TP)
        fw.op(V, lambda e: e.tensor_copy(out=xtail, in_=xnTp[:, :, TP - 3:TP]), reads=[b_xnTp], writes=[b_xtail])
        fw.push()
        ev = make_store_evac(xbcT_p, TP, F32)
        proj_fm(w_in_d, 16, C_X, 5120, xnTp, b_xnTp, [(0, 512), (512, 512)], ev, wr)

        def ev_dt_p(i, pt, bp):
            fw.op(V, lambda e: e.tensor_copy(out=dtraw_p[:, i, :], in_=pt[:, 0:64]), reads=[bp], writes=[b_dtraw_p])
        proj_tm(w_in_d, 16, C_DT, 64, xnTp, b_xnTp, NTP, ev_dt_p, wr)
        fw.pop()
        fw.pop()
        fw.barrier()

    def phase_conv(src_scr, nchunks, main):
        fw.push()
        L = 3 + 1024 + (176 if main else 0)
        upr = fw.ring(2, [128, L], F32, "up")
        accr = fw.ring(2, [128, TM if main else TP], F32, "acc")
        xcr = fw.ring(2, [128, TM if main else TP], BF16, "xc")
        ttr = fw.ring(2, [128, NTM, 128], BF16, "tt")
        segb = {}
        if main:
            t32r = fw.ring(2, [128, NTM, 128], F32, "t32")
            stc, b_stc = fw.alloc([48, 6144], F32, "stc")
            fw.dma(SY, stc, st_conv_d, dst=b_stc)
            cvo, b_cvo = fw.alloc([51, 6144], F32, "cvo")
            cst_r = fw.ring(2, [128, 51], F32, "cst")
        ntl = NTM if main else NTP
        xdst = x_tm if main else x_tm_p
        bdst = b_tm if main else b_tm_p
        for c in range(nchunks):
            up, bu = upr.next(); acc, ba = accr.next(); xc, bxc = xcr.next()
            fw.dma(SY, up[:, 3:1027], src_scr[c * 128:(c + 1) * 128, 0:1024], dst=bu)
            if main:
                upS = up[:, 1027:1203].rearrange("p (j t) -> p j t", t=11)
                fw.dma(SY, upS[:, :, 3:11], src_scr[c * 128:(c + 1) * 128, 1024:1152].rearrange("p (j t) -> p j t", t=8), dst=bu)
                fw.op(V, lambda e, up=up, c=c: e.tensor_scalar(out=up[:, 0:3], in0=histx[:, c, :], scalar1=flag[:, 0:1],
                                                              scalar2=None, op0=ALU.mult), reads=[b_histx, b_flag], writes=[bu])
                pt, bp = bank(0)
                fw.op(T, lambda e, pt=pt, c=c: e.transpose(out=pt[:, 0:48], in_=stc[0:48, c * 128:(c + 1) * 128], identity=idf[0:48, 0:48]),
                      reads=[b_stc, b_idf], writes=[bp])
                fw.op(V, lambda e, pt=pt, upS=upS: e.tensor_copy(out=upS[:, :, 0:3], in_=pt[:, 0:48].rearrange("p (j t) -> p j t", t=3)),
                      reads=[bp], writes=[bu])
            else:
                fw.op(V, lambda e, up=up: e.memset(up[:, 0:3], 0.0), writes=[bu])
            if id(ba) not in segb:
                segb[id(ba)] = [fw.buf(f"accseg{q}") for q in range(3)]
            sb3 = segb[id(ba)]
            segs = [(up[:, 0:515], acc[:, 0:512], lambda a, k: a[:, k:k + 512], sb3[0]),
                    (up[:, 512:1027], acc[:, 512:1024], lambda a, k: a[:, k:k + 512], sb3[1])]
            if main:
                segs.append((upS, acc[:, 1024:1152].rearrange("p (j t) -> p j t", t=8), lambda a, k: a[:, :, k:k + 8], sb3[2]))
            for k in range(4):
                for (src, dst, sl, bseg) in segs:
                    if k == 0:
                        fw.op(V, lambda e, src=src, dst=dst, sl=sl, c=c: e.tensor_scalar(
                            out=dst, in0=sl(src, 0), scalar1=cw[:, c, 0:1], scalar2=cb[:, c:c + 1], op0=ALU.mult, op1=ALU.add),
                            reads=[bu, b_cw, b_cb], writes=[bseg])
                    else:
                        fw.op(V, lambda e, src=src, dst=dst, sl=sl, c=c, k=k: e.scalar_tensor_tensor(
                            out=dst, in0=sl(src, k), scalar=cw[:, c, k:k + 1], in1=dst, op0=ALU.mult, op1=ALU.add),
                            reads=[bu, b_cw, bseg], writes=[bseg])
            ba_all = sb3[0:len(segs)]
            fw.op(S, lambda e, acc=acc: e.activation(out=acc, in_=acc, func=AF.Silu), reads=ba_all, writes=ba_all)
            fw.op(V, lambda e, xc=xc, acc=acc: e.tensor_copy(out=xc, in_=acc), reads=ba_all, writes=[bxc])
            if main and c < 32:
                t32, bt32 = t32r.next()
                for (b0, i0, i1) in ((4, 0, 4), (5, 4, 8), (6, 8, 9)):
                    pt, bp = bank(b0)
                    for i in range(i0, i1):
                        fw.op(T, lambda e, pt=pt, acc=acc, i=i, i0=i0: e.transpose(out=pt[:, (i - i0) * 128:(i - i0 + 1) * 128], in_=acc[:, i * 128:(i + 1) * 128], identity=idf),
                              reads=ba_all + [b_idf], writes=[bp], signal=(i == i1 - 1))
                    fw.op(V, lambda e, pt=pt, t32=t32, i0=i0, i1=i1: e.tensor_copy(out=t32[:, i0:i1, :], in_=pt[:, 0:(i1 - i0) * 128].rearrange("p (a b) -> p a b", b=128)),
                          reads=[bp], writes=[bt32])
                fw.dma(G, x32_tm.rearrange("(i p) c -> p i c", p=128)[:, :, c * 128:(c + 1) * 128], t32, src=bt32)
            if main:
                cst, bcs = cst_r.next()
                fw.op(V, lambda e, cst=cst, up=up: e.tensor_copy(out=cst[:, 0:3], in_=up[:, 1024:1027]), reads=[bu], writes=[bcs])
                fw.op(V, lambda e, cst=cst, upS=upS: e.tensor_copy(out=cst[:, 3:51].rearrange("p (j t) -> p j t", t=3), in_=upS[:, :, 8:11]),
                      reads=[bu], writes=[bcs])
                pt, bp = bank(1)
                fw.op(T, lambda e, pt=pt, cst=cst: e.transpose(out=pt[0:51, 0:128], in_=cst, identity=idf), reads=[bcs, b_idf], writes=[bp])
                fw.op(S, lambda e, pt=pt, c=c: e.copy(out=cvo[0:51, c * 128:(c + 1) * 128], in_=pt[0:51, 0:128]), reads=[bp], writes=[b_cvo])
            isx = c < 32
            isb = 32 <= c < 40
            if main and not isx:
                dsc = bT_s if isb else cT_s
                cc = c - 32 if isb else c - 40
                fw.dma(G, dsc[cc * 128:(cc + 1) * 128, :], xc, src=bxc)
            if isx or isb:
                tt, btt = ttr.next()
                for h in range(2):
                    n0 = h * 8
                    n1 = min(ntl, n0 + 8)
                    if n1 <= n0:
                        continue
                    pt, bp = bank16(2 + h)
                    for i in range(n0, n1):
                        fw.op(T, lambda e, pt=pt, xc=xc, i=i, n0=n0: e.transpose(out=pt[:, (i - n0) * 128:(i - n0 + 1) * 128],
                                                                              in_=xc[:, i * 128:(i + 1) * 128], identity=idb),
                              reads=[bxc, b_idb], writes=[bp], signal=(i == n1 - 1))
                    fw.op(S if h == 0 else V, (lambda e, pt=pt, tt=tt, n0=n0, n1=n1: (e.copy if False else e.tensor_copy)(
                        out=tt[:, n0:n1, :], in_=pt[:, 0:(n1 - n0) * 128].rearrange("p (a b) -> p a b", b=128))) if h == 1 else
                        (lambda e, pt=pt, tt=tt, n0=n0, n1=n1: e.copy(out=tt[:, n0:n1, :], in_=pt[:, 0:(n1 - n0) * 128].rearrange("p (a b) -> p a b", b=128))),
                        reads=[bp], writes=[btt])
                if isx:
                    fw.dma(G, xdst.rearrange("(i p) c -> p i c", p=128)[:, :, c * 128:(c + 1) * 128], tt[:, 0:ntl, :], src=btt)
                else:
                    cc = c - 32
                    fw.dma(G, bdst.rearrange("(i p) c -> p i c", p=128)[:, :, cc * 128:(cc + 1) * 128], tt[:, 0:ntl, :], src=btt)
        if main:
            fw.dma(G, o_conv_p, cvo[0:3, :], src=b_cvo)
            fw.dma(G, o_conv_s, cvo[3:51, :], src=b_cvo)
        fw.pop()
        fw.barrier()

    TB = [(0, 384), (384, 384), (768, 384)]
    if stage >= 3:
        xnT, b_xnT = fw.alloc([128, 16, TM], BF16, "xnT")
        phase_norm(xm_d, NTM, nbc, b_nbc, xnT, b_xnT, TM)

        def make_extra(hist, b_hist, ch_off):
            def extra(ch, wt, bw, j):
                pt, bp = bank(7)
                for k in range(16):
                    fw.op(T, lambda e, pt=pt, wt=wt, k=k, j=j: e.matmul(pt[:, 0:3], lhsT=wt[:, k, j * 128:(j + 1) * 128],
                                                                       rhs=xtail[:, k, 0:3], start=(k == 0), stop=(k == 15)),
                          reads=[bw, b_xtail], writes=[bp], signal=(k == 15))
                fw.op(V, lambda e, pt=pt, ch=ch: e.tensor_copy(out=hist[:, ch + ch_off, :], in_=pt[:, 0:3]), reads=[bp], writes=[b_hist])
            return extra

        fw.push()
        ev = make_store_evac(xbcT, TM, F32)
        proj_fm(w_in_d, 16, C_X, 6144, xnT, b_xnT, TB, ev, wr, extra=make_extra(histx, b_histx, 0))
        fw.pop()
        fw.push()
        ev = make_store_evac(scT, TM, F32)
        proj_fm(w_in_d, 16, C_SB, 2048, xnT, b_xnT, TB, ev, wr)
        ev2 = lambda ch, t0, tn, pt, bp: ev(ch + 16, t0, tn, pt, bp)
        proj_fm(w_in_d, 16, C_SC, 4096, xnT, b_xnT, TB, ev2, wr, extra=make_extra(histsc, b_histsc, 0))
        fw.pop()
        fw.push()
        ev = make_store_evac(gateT, TM, F32, func=AF.Sigmoid)
        proj_fm(w_in_d, 16, C_GA, 4096, xnT, b_xnT, TB, ev, wr)
        fw.pop()
        fw.push()
        zst = fw.ring(3, [128, 512], F32, "zst")
        for blk in range(8):
            def ev_z(i, pt, bp, blk=blk):
                st, bs = zst.next()
                fw.op(S, lambda e: e.activation(out=st, in_=pt[:, 0:512], func=AF.Silu), reads=[bp], writes=[bs])
                fw.dma(SY, zs_s[i * 128:(i + 1) * 128, blk * 512:(blk + 1) * 512], st, src=bs)
            proj_tm(w_in_d, 16, C_Z + blk * 512, 512, xnT, b_xnT, NTM, ev_z, wr)

        def ev_dt(i, pt, bp):
            fw.op(V, lambda e: e.tensor_copy(out=dtraw[:, i, :], in_=pt[:, 0:64]), reads=[bp], writes=[b_dtraw])
        proj_tm(w_in_d, 16, C_DT, 64, xnT, b_xnT, NTM, ev_dt, wr)
        fw.pop()
        fw.barrier()

    fw.pop()
    if stage >= 2:
        phase_conv(xbcT_p, 40, False)
    if stage >= 3:
        phase_conv(xbcT, 48, True)

    if stage >= 4:
        fw.push()
        L2 = 2 + 1024 + 160
        sts, b_sts = fw.alloc([32, 2048], F32, "sts")
        fw.dma(SY, sts, st_sc_d, dst=b_sts)
        sco, b_sco = fw.alloc([34, 2048], F32, "sco")
        inr = fw.ring(2, [128, 3, TM], F32, "scin")
        upr = fw.ring(2, [128, L2], F32, "up2")
        vr = fw.ring(2, [128, TM], F32, "scv")
        ybr = fw.ring(2, [128, TM], BF16, "yb")
        cs2r = fw.ring(2, [128, 34], F32, "cs2")
        segb2 = {}
        for c in range(16):
            it, bi = inr.next(); up, bu = upr.next(); v, bv = vr.next(); yb, byb = ybr.next(); cs2, bc2 = cs2r.next()
            for q in range(3):
                fw.dma(SY, it[:, q, :], scT[q * 2048 + c * 128:q * 2048 + (c + 1) * 128, :], dst=bi)
            upS = up[:, 1026:1186].rearrange("p (j t) -> p j t", t=10)
            fw.op(V, lambda e, up=up, it=it: e.tensor_tensor(out=up[:, 2:1026], in0=it[:, 1, 0:1024], in1=it[:, 2, 0:1024], op=ALU.mult),
                  reads=[bi], writes=[bu])
            fw.op(V, lambda e, upS=upS, it=it: e.tensor_tensor(out=upS[:, :, 2:10], in0=it[:, 1, 1024:1152].rearrange("p (j t) -> p j t", t=8),
                                                             in1=it[:, 2, 1024:1152].rearrange("p (j t) -> p j t", t=8), op=ALU.mult),
                  reads=[bi], writes=[bu])
            fw.op(V, lambda e, up=up, c=c: e.scalar_tensor_tensor(out=up[:, 0:2], in0=histsc[:, c, 1:3], scalar=flag[:, 0:1],
                                                                 in1=histsc[:, 16 + c, 1:3], op0=ALU.mult, op1=ALU.mult),
                  reads=[b_histsc, b_flag], writes=[bu])
            pt, bp = bank(c % 2)
            fw.op(T, lambda e, pt=pt, c=c: e.transpose(out=pt[:, 0:32], in_=sts[0:32, c * 128:(c + 1) * 128], identity=idf[0:32, 0:32]),
                  reads=[b_sts, b_idf], writes=[bp])
            fw.op(V, lambda e, pt=pt, upS=upS: e.tensor_copy(out=upS[:, :, 0:2], in_=pt[:, 0:32].rearrange("p (j t) -> p j t", t=2)),
                  reads=[bp], writes=[bu])
            if id(bv) not in segb2:
                segb2[id(bv)] = [fw.buf(f"vseg{q}") for q in range(3)]
            sb3 = segb2[id(bv)]
            segs = [(up[:, 0:514], v[:, 0:512], lambda a, k: a[:, k:k + 512], it[:, 0, 0:512], yb[:, 0:512], sb3[0]),
                    (up[:, 512:1026], v[:, 512:1024], lambda a, k: a[:, k:k + 512], it[:, 0, 512:1024], yb[:, 512:1024], sb3[1]),
                    (upS, v[:, 1024:1152].rearrange("p (j t) -> p j t", t=8), lambda a, k: a[:, :, k:k + 8],
                     it[:, 0, 1024:1152].rearrange("p (j t) -> p j t", t=8), yb[:, 1024:1152].rearrange("p (j t) -> p j t", t=8), sb3[2])]
            for k in range(3):
                for (src, dst, sl, bsrc, ydst, bseg) in segs:
                    if k == 0:
                        fw.op(V, lambda e, src=src, dst=dst, sl=sl, c=c: e.tensor_scalar(out=dst, in0=sl(src, 0), scalar1=scw[:, c, 0:1],
                                                                                      scalar2=None, op0=ALU.mult), reads=[bu, b_scw], writes=[bseg])
                    else:
                        fw.op(V, lambda e, src=src, dst=dst, sl=sl, c=c, k=k: e.scalar_tensor_tensor(
                            out=dst, in0=sl(src, k), scalar=scw[:, c, k:k + 1], in1=dst, op0=ALU.mult, op1=ALU.add),
                            reads=[bu, b_scw, bseg], writes=[bseg])
            for (src, dst, sl, bsrc, ydst, bseg) in segs:
                fw.op(V, lambda e, dst=dst, bsrc=bsrc, ydst=ydst: e.tensor_tensor(out=ydst, in0=dst, in1=bsrc, op=ALU.mult),
                      reads=[bseg, bi], writes=[byb])
            fw.dma(G, ybT[c * 128:(c + 1) * 128, :], yb, src=byb)
            fw.op(V, lambda e, cs2=cs2, up=up: e.tensor_copy(out=cs2[:, 0:2], in_=up[:, 1024:1026]), reads=[bu], writes=[bc2])
            fw.op(V, lambda e, cs2=cs2, upS=upS: e.tensor_copy(out=cs2[:, 2:34].rearrange("p (j t) -> p j t", t=2), in_=upS[:, :, 8:10]),
                  reads=[bu], writes=[bc2])
            pt, bp = bank(2 + c % 2)
            fw.op(T, lambda e, pt=pt, cs2=cs2: e.transpose(out=pt[0:34, 0:128], in_=cs2, identity=idf), reads=[bc2, b_idf], writes=[bp])
            fw.op(S, lambda e, pt=pt, c=c: e.copy(out=sco[0:34, c * 128:(c + 1) * 128], in_=pt[0:34, 0:128]), reads=[bp], writes=[b_sco])
        fw.dma(G, o_sc_p, sco[0:2, :], src=b_sco)
        fw.dma(G, o_sc_s, sco[2:34, :], src=b_sco)
        fw.pop()
        fw.barrier()

    def v3(ap, a):
        return ap.rearrange("p (a b) -> p a b", a=a)

    if stage >= 5:
        fw.push()
        ssdn, b_ssdn = fw.alloc([128, 4096], F32, "ssdn")
        fw.dma(SY, ssdn, ssdn_d, dst=b_ssdn)
        x32r = fw.ring(2, [128, 512], F32, "x32g")
        dskr = fw.ring(2, [128, 512], F32, "dskg")
        ztr = fw.ring(2, [128, 512], F32, "ztg")
        smr = fw.ring(2, [128, 12, 64], F32, "sm")
        csTr = fw.ring(2, [64, 128], F32, "csT")
        R4r = fw.ring(2, [64, 512], F32, "R4")
        t1r = fw.ring(8, [128, 128], F32, "t1")
        Er = fw.ring(8, [128, 128], F32, "E")
        LTr = fw.ring(8, [128, 128], BF16, "LT")
        CBr = fw.ring(2, [128, 128], F32, "CBm")
        ygr = fw.ring(2, [128, 512], F32, "yg")
        jkr = fw.ring(2, [128, 512], BF16, "jk")
        yar = fw.ring(2, [128, 512], BF16, "ya")
        ystr = fw.ring(2, [128, 4, 128], BF16, "yst")
        nscr = fw.ring(2, [128, 4], F32, "nsc2")

        def ssd_tile(i, kind, bufs, HT=None, HTb=None, bHT=None, bHTb=None, samp=None):
            pre = kind == "pre"
            do_y = not pre
            m = 1 if kind == "samp" else 0
            triM = tri[:, m, :]
            bonesM = bones[:, m, :]
            xsrc, bsrc = (x_tm_p, b_tm_p) if pre else (x_tm, b_tm)
            dtr, b_dtr = (dtraw_p, b_dtraw_p) if pre else (dtraw, b_dtraw)
            (xt, bxt), (bt, bbt), (bTt, bbT), (cTt, bcT), (xw, bxw) = bufs
            fw.dma(SY, xt, xsrc[i * 128:(i + 1) * 128, :], dst=bxt)
            fw.dma(SY, bt, bsrc[i * 128:(i + 1) * 128, :], dst=bbt)
            if do_y:
                fw.dma(SY, bTt, bT_s.rearrange("(g n) t -> n g t", n=128)[:, :, i * 128:(i + 1) * 128], dst=bbT)
                fw.dma(SY, cTt, cT_s.rearrange("(g n) t -> n g t", n=128)[:, :, i * 128:(i + 1) * 128], dst=bcT)
            sm, bsm = smr.next()
            v_, ab, l_, dt, a_, negcs, ecs, d1, wdt, dec = [sm[:, q, :] for q in range(10)]
            fw.op(V, lambda e: e.tensor_tensor(out=v_, in0=dtr[:, i, :], in1=dtb, op=ALU.add), reads=[b_dtr, b_dtb], writes=[bsm])
            fw.op(S, lambda e: e.activation(out=ab, in_=v_, func=AF.Abs), reads=[bsm], writes=[bsm])
            fw.op(S, lambda e: e.activation(out=ab, in_=ab, func=AF.Exp, scale=-1.0), reads=[bsm], writes=[bsm])
            fw.op(V, lambda e: e.tensor_scalar(out=ab, in0=ab, scalar1=1.0, scalar2=None, op0=ALU.add), reads=[bsm], writes=[bsm])
            fw.op(S, lambda e: e.activation(out=l_, in_=ab, func=AF.Ln), reads=[bsm], writes=[bsm])
            fw.op(V, lambda e: e.scalar_tensor_tensor(out=dt, in0=v_, scalar=0.0, in1=l_, op0=ALU.max, op1=ALU.add), reads=[bsm], writes=[bsm])
            fw.op(V, lambda e: e.tensor_tensor(out=a_, in0=dt, in1=Abc, op=ALU.mult), reads=[bsm, b_Abc], writes=[bsm])
            pt0, bp0 = bank(0)
            fw.op(T, lambda e: e.matmul(pt0[:, 0:64], lhsT=triM, rhs=a_, start=True, stop=True), reads=[b_tri, bsm], writes=[bp0], signal=False)
            fw.op(T, lambda e: e.matmul(pt0[:, 64:128], lhsT=bonesM, rhs=a_, start=True, stop=True), reads=[b_bones, bsm], writes=[bp0], signal=False)
            fw.op(T, lambda e: e.matmul(pt0[0:64, 128:256], lhsT=a_, rhs=triM, start=True, stop=True), reads=[b_tri, bsm], writes=[bp0])
            fw.op(V, lambda e: e.tensor_scalar(out=negcs, in0=pt0[:, 0:64], scalar1=-1.0, scalar2=None, op0=ALU.mult), reads=[bp0], writes=[bsm])
            fw.op(S, lambda e: e.activation(out=ecs, in_=pt0[:, 0:64], func=AF.Exp), reads=[bp0], writes=[bsm])
            fw.op(V, lambda e: e.tensor_tensor(out=d1, in0=pt0[:, 64:128], in1=negcs, op=ALU.add), reads=[bp0, bsm], writes=[bsm])
            fw.op(S, lambda e: e.activation(out=d1, in_=d1, func=AF.Exp), reads=[bsm], writes=[bsm])
            fw.op(V, lambda e: e.tensor_tensor(out=wdt, in0=d1, in1=dt, op=ALU.mult), reads=[bsm], writes=[bsm])
            fw.op(S, lambda e: e.activation(out=dec, in_=pt0[:, 64:128], func=AF.Exp), reads=[bp0], writes=[bsm])
            csT, bcsT = csTr.next()
            fw.op(S, lambda e: e.copy(out=csT, in_=pt0[0:64, 128:256]), reads=[bp0], writes=[bcsT])
            fw.op(V, lambda e: e.tensor_tensor(out=v3(xw, 64), in0=v3(xt, 64), in1=wdt.unsqueeze(2).broadcast_to([128, 64, 64]), op=ALU.mult),
                  reads=[bxt, bsm], writes=[bxw])
            if samp is not None:
                samp(i, a_, bsm, cTt, bcT, bt, bbt, xw, bxw)
            for g in range(8):
                if do_y:
                    ptb, bpb = bank(1)
                    fw.op(T, lambda e, g=g: e.matmul(ptb[:, 0:128], lhsT=bTt[:, g, :], rhs=cTt[:, g, :], start=True, stop=True),
                          reads=[bbT, bcT], writes=[bpb])
                    CBm, bCB = CBr.next()
                    fw.op(V, lambda e, CBm=CBm: e.tensor_tensor(out=CBm, in0=ptb[:, 0:128], in1=triM, op=ALU.mult), reads=[bpb, b_tri], writes=[bCB])
                    pty, bpy = bank(4)
                    hd = []
                    for half in range(2):
                        h0 = 8 * g + 4 * half
                        R4, bR4 = R4r.next()
                        fw.op(V, lambda e, R4=R4, h0=h0: e.tensor_tensor(out=v3(R4, 4), in0=idf[0:64, h0:h0 + 4].unsqueeze(2).broadcast_to([64, 4, 128]),
                                                                        in1=csT.unsqueeze(1).broadcast_to([64, 4, 128]), op=ALU.mult),
                              reads=[b_idf, bcsT], writes=[bR4])
                        ptc, bpc = bank(2 + half)
                        fw.op(T, lambda e, R4=R4, ptc=ptc: e.matmul(ptc[:, 0:512], lhsT=ones64, rhs=R4, start=True, stop=True),
                              reads=[b_ones64, bR4], writes=[bpc])
                        for hl in range(4):
                            h = h0 + hl
                            hg = 4 * half + hl
                            t1, bt1 = t1r.next(); E_, bE = Er.next()
                            fw.op(V, lambda e, t1=t1, ptc=ptc, hl=hl, h=h: e.tensor_scalar(out=t1, in0=ptc[:, hl * 128:(hl + 1) * 128], scalar1=negcs[:, h:h + 1],
                                                                                     scalar2=0.0, op0=ALU.add, op1=ALU.min), reads=[bpc, bsm], writes=[bt1])
                            fw.op(S, lambda e, t1=t1, E_=E_: e.activation(out=E_, in_=t1, func=AF.Exp), reads=[bt1], writes=[bE])
                            hd.append((h, hg, E_, bE))
                    for (h, hg, E_, bE) in hd:
                        LT, bLT = LTr.next()
                        fw.op(V, lambda e, E_=E_, LT=LT, CBm=CBm, h=h: e.scalar_tensor_tensor(out=LT, in0=E_, scalar=dt[:, h:h + 1], in1=CBm,
                                                                                       op0=ALU.mult, op1=ALU.mult), reads=[bE, bsm, bCB], writes=[bLT])
                        fw.op(T, lambda e, LT=LT, hg=hg, h=h: e.matmul(pty[:, hg * 64:(hg + 1) * 64], lhsT=LT, rhs=xt[:, h * 64:(h + 1) * 64], start=True, stop=True),
                              reads=[bLT, bxt], writes=[bpy])
                    pto, bpo = bank(5)
                    if kind == "main":
                        fw.op(T, lambda e, g=g: e.matmul(pto[:, 0:512], lhsT=cTt[:, g, :], rhs=HTb[:, g * 512:(g + 1) * 512], start=True, stop=True),
                              reads=[bcT, bHTb[g]], writes=[bpo])
                    else:
                        yoffT, byo = samp.yoffT
                        for u in range(4):
                            fw.op(T, lambda e, u=u, g=g: e.transpose(out=pto[:, u * 128:(u + 1) * 128], in_=yoffT[:, 4 * g + u, :], identity=idf),
                                  reads=[byo, b_idf], writes=[bpo], signal=(u == 3))
                    yg, byg = ygr.next()
                    fw.op(V, lambda e, yg=yg, g=g: e.tensor_tensor(out=v3(yg, 8), in0=v3(pto[:, 0:512], 8),
                                                                  in1=ecs[:, 8 * g:8 * g + 8].unsqueeze(2).broadcast_to([128, 8, 64]), op=ALU.mult),
                          reads=[bpo, bsm], writes=[byg])
                    fw.op(V, lambda e, yg=yg: e.tensor_tensor(out=yg, in0=yg, in1=pty[:, 0:512], op=ALU.add), reads=[byg, bpy], writes=[byg])
                    x32g, bx32 = x32r.next(); dskg, bdsk = dskr.next(); ztg, bztg = ztr.next()
                    fw.dma(SY, x32g, x32_tm[i * 128:(i + 1) * 128, g * 512:(g + 1) * 512], dst=bx32)
                    fw.dma(SY, dskg, dsk_d[:, g * 512:(g + 1) * 512], dst=bdsk)
                    fw.dma(SY, ztg, zs_s[i * 128:(i + 1) * 128, g * 512:(g + 1) * 512], dst=bztg)
                    fw.op(V, lambda e, x32g=x32g, dskg=dskg: e.tensor_tensor(out=x32g, in0=x32g, in1=dskg, op=ALU.mult), reads=[bx32, bdsk], writes=[bx32])
                    fw.op(V, lambda e, yg=yg, x32g=x32g: e.tensor_tensor(out=yg, in0=yg, in1=x32g, op=ALU.add), reads=[byg, bx32], writes=[byg])
                    fw.op(V, lambda e, yg=yg, ztg=ztg: e.tensor_tensor(out=yg, in0=yg, in1=ztg, op=ALU.mult), reads=[byg, bztg], writes=[byg])
                    ns, bns = nscr.next(); jk, bjk = jkr.next(); ya, bya = yar.next(); yst, byst = ystr.next()
                    fw.op(V, lambda e, ns=ns: e.memset(ns[:, 0:1], 0.0), writes=[bns])
                    fw.op(S, lambda e, jk=jk, yg=yg, ns=ns: e.activation(out=jk, in_=yg, func=AF.Square, accum_out=ns[:, 0:1]), reads=[byg], writes=[bjk, bns])
                    fw.op(V, lambda e, ns=ns: e.tensor_scalar(out=ns[:, 1:2], in0=ns[:, 0:1], scalar1=1.0 / 512, scalar2=1e-5, op0=ALU.mult, op1=ALU.add),
                          reads=[bns], writes=[bns])
                    fw.op(S, lambda e, ns=ns: e.activation(out=ns[:, 2:3], in_=ns[:, 1:2], func=AF.Sqrt), reads=[bns], writes=[bns])
                    fw.op(V, lambda e, ns=ns: e.reciprocal(out=ns[:, 3:4], in_=ns[:, 2:3]), reads=[bns], writes=[bns])
                    fw.op(V, lambda e, ns=ns, ya=ya, yg=yg, g=g: e.scalar_tensor_tensor(out=ya, in0=yg, scalar=ns[:, 3:4], in1=ssdn[:, g * 512:(g + 1) * 512],
                                                                                 op0=ALU.mult, op1=ALU.mult), reads=[byg, bns, b_ssdn], writes=[bya])
                    pt7, bp7 = bank16(7)
                    for u in range(4):
                        fw.op(T, lambda e, u=u, ya=ya: e.transpose(out=pt7[:, u * 128:(u + 1) * 128], in_=ya[:, u * 128:(u + 1) * 128], identity=idb),
                              reads=[bya, b_idb], writes=[bp7], signal=(u == 3))
                    fw.op(S, lambda e, yst=yst: e.copy(out=yst, in_=v3(pt7[:, 0:512], 4)), reads=[bp7], writes=[byst])
                    fw.dma(G, yaT[g * 512:(g + 1) * 512, i * 128:(i + 1) * 128].rearrange("(j p) t -> p j t", p=128), yst, src=byst)
                if kind != "samp":
                    pts, bps = bank(6)
                    fw.op(T, lambda e, g=g: e.matmul(pts[:, 0:512], lhsT=bt[:, g * 128:(g + 1) * 128], rhs=xw[:, g * 512:(g + 1) * 512], start=True, stop=True),
                          reads=[bbt, bxw], writes=[bps])
                    HTg = HT[:, g * 512:(g + 1) * 512]
                    fw.op(V, lambda e, HTg=HTg, g=g: e.tensor_tensor(out=v3(HTg, 8), in0=v3(HTg, 8),
                                                                    in1=dec[:, 8 * g:8 * g + 8].unsqueeze(2).broadcast_to([128, 8, 64]), op=ALU.mult),
                          reads=[bHT[g], bsm], writes=[bHT[g]])
                    fw.op(V, lambda e, HTg=HTg: e.tensor_tensor(out=HTg, in0=HTg, in1=pts[:, 0:512], op=ALU.add), reads=[bHT[g], bps], writes=[bHT[g]])
                    fw.op(S, lambda e, HTg=HTg, g=g: e.copy(out=HTb[:, g * 512:(g + 1) * 512], in_=HTg), reads=[bHT[g]], writes=[bHTb[g]])

        fw.push()
        HT, _ = fw.alloc([128, 4096], F32, "HT")
        HTb, _ = fw.alloc([128, 4096], BF16, "HTb")
        bHT = [fw.buf(f"HT{g}") for g in range(8)]
        bHTb = [fw.buf(f"HTb{g}") for g in range(8)]
        ldr = [fw.ring(2, sh, BF16, nm) for (sh, nm) in (([128, 4096], "xt"), ([128, 1024], "bt"), ([128, 8, 128], "bTt"), ([128, 8, 128], "cTt"))]
        xw1 = fw.alloc([128, 4096], BF16, "xw")
        hout, b_hout = fw.alloc([128, 32, 128], F32, "houtp")
        for g in range(8):
            fw.op(V, lambda e, g=g: e.memset(HT[:, g * 512:(g + 1) * 512], 0.0), writes=[bHT[g]])
            fw.op(V, lambda e, g=g: e.memset(HTb[:, g * 512:(g + 1) * 512], 0.0), writes=[bHTb[g]])
        for i in range(NTP if stage >= 5.1 else 1):
            ssd_tile(i, "pre", [r.next() for r in ldr] + [xw1], HT, HTb, bHT, bHTb)
        for g in range(8):
            HTg = HT[:, g * 512:(g + 1) * 512]
            fw.op(V, lambda e, HTg=HTg: e.tensor_scalar(out=HTg, in0=HTg, scalar1=flag[:, 0:1], scalar2=None, op0=ALU.mult), reads=[bHT[g], b_flag], writes=[bHT[g]])
            fw.op(S, lambda e, HTg=HTg, g=g: e.copy(out=HTb[:, g * 512:(g + 1) * 512], in_=HTg), reads=[bHT[g]], writes=[bHTb[g]])
        for i in range(8 if stage >= 5.3 else (1 if stage >= 5.2 else 0)):
            ssd_tile(i, "main", [r.next() for r in ldr] + [xw1], HT, HTb, bHT, bHTb)
        for q4 in range(8):
            ptx, bpx = bank(2 + q4 % 2)
            for u in range(4):
                hp = q4 * 4 + u
                fw.op(T, lambda e, ptx=ptx, u=u, hp=hp: e.transpose(out=ptx[:, u * 128:(u + 1) * 128], in_=HT[:, hp * 128:(hp + 1) * 128], identity=idf),
                      reads=[bHT[hp // 4], b_idf], writes=[bpx], signal=(u == 3))
            fw.op(S if q4 % 2 else V, (lambda e, ptx=ptx, q4=q4: e.copy(out=hout[:, q4 * 4:(q4 + 1) * 4, :], in_=v3(ptx[:, 0:512], 4))) if q4 % 2 else
                  (lambda e, ptx=ptx, q4=q4: e.tensor_copy(out=hout[:, q4 * 4:(q4 + 1) * 4, :], in_=v3(ptx[:, 0:512], 4))), reads=[bpx], writes=[b_hout])
        fw.dma(G, o_ssm_p.rearrange("h p n -> (h p) n").rearrange("(hp q) n -> q hp n", q=128), hout, src=b_hout)
        fw.pop()
        fw.barrier()

        fw.push()
        sbufs = [fw.alloc(sh, BF16, nm) for (sh, nm) in (([128, 4096], "sxt"), ([128, 1024], "sbt"), ([128, 8, 128], "sbTt"), ([128, 8, 128], "scTt"),
                                                         ([128, 4096], "sxw"))]
        onesj, b_onesj = fw.alloc([128, 16, 128], F32, "onesj")
        maskj, b_maskj = fw.alloc([128, 16], F32, "maskj")
        fw.dma(SY, onesj, onesj_d, dst=b_onesj)
        fw.dma(SY, maskj, maskj_d, dst=b_maskj)
        decs, b_decs = fw.alloc([128, 1024], F32, "decs")
        h0r = fw.ring(2, [128, 32, 128], F32, "h0f")
        h0T, b_h0T = fw.alloc([128, 4096], F32, "h0T")
        h0Tb, b_h0Tb = fw.alloc([128, 4096], BF16, "h0Tb")
        hnT, b_hnT = fw.alloc([128, 4096], F32, "hnT")
        yoffT, b_yoffT = fw.alloc([128, 32, 128], F32, "yoffT")
        bmjr = fw.ring(2, [128, 1024], BF16, "bmj")

        def samp(i, a_, bsm, cTt, bcT, bt, bbt, xw, bxw):
            if stage < 5.5:
                fw.op(V, lambda e: e.memset(yoffT, 0.0), writes=[b_yoffT])
                return
            PS0, ba0, bb0 = PS[0]
            for j in range(16):
                fw.op(T, lambda e, j=j: e.matmul(PS0[:, j * 64:(j + 1) * 64], lhsT=onesj[:, j, :], rhs=a_, start=True, stop=True),
                      reads=[b_onesj, bsm], writes=[ba0, bb0], signal=(j == 15))
            fw.op(S, lambda e: e.activation(out=decs[:, 0:512], in_=PS0[:, 0:512], func=AF.Exp), reads=[ba0], writes=[b_decs])
            fw.op(S, lambda e: e.activation(out=decs[:, 512:1024], in_=PS0[:, 512:1024], func=AF.Exp), reads=[bb0], writes=[b_decs])
            if stage < 5.52:
                fw.op(V, lambda e: e.memset(yoffT, 0.0), writes=[b_yoffT])
                return
            for j in range(16):
                h0f, bh = h0r.next()
                h0src = st_ssm_d[j].rearrange("h p n -> (h p) n").rearrange("(hp q) n -> q hp n", q=128)
                for q8 in range(4):
                    fw.dma(SY, h0f[:, q8 * 8:(q8 + 1) * 8, :], h0src[:, q8 * 8:(q8 + 1) * 8, :], dst=bh)
                if stage < 5.53:
                    if j == 0:
                        fw.op(V, lambda e: e.memset(yoffT, 0.0), writes=[b_yoffT])
                    continue
                for q4 in range(8):
                    ptx, bpx = bank(q4 % 4)
                    for u in range(4):
                        hp = q4 * 4 + u
                        fw.op(T, lambda e, ptx=ptx, u=u, hp=hp, h0f=h0f: e.transpose(out=ptx[:, u * 128:(u + 1) * 128], in_=h0f[:, hp, :], identity=idf),
                              reads=[bh, b_idf], writes=[bpx], signal=(u == 3))
                    fw.op(V, lambda e, ptx=ptx, q4=q4: e.tensor_copy(out=h0T[:, q4 * 512:(q4 + 1) * 512], in_=ptx[:, 0:512]), reads=[bpx], writes=[b_h0T])
                    fw.op(S, lambda e, q4=q4: e.activation(out=h0Tb[:, q4 * 512:(q4 + 1) * 512], in_=h0T[:, q4 * 512:(q4 + 1) * 512], func=AF.Identity), reads=[b_h0T], writes=[b_h0Tb])
                if stage < 5.6:
                    if j == 0:
                        fw.op(V, lambda e: e.memset(yoffT, 0.0), writes=[b_yoffT])
                    continue
                pto, bpo = bank(5)
                for hp in range(32):
                    fw.op(T, lambda e, hp=hp, j=j: e.matmul(pto[:, hp * 8:(hp + 1) * 8], lhsT=h0Tb[:, hp * 128:(hp + 1) * 128], rhs=cTt[:, hp // 4, 8 * j:8 * j + 8],
                                                          start=True, stop=True), reads=[b_h0Tb, bcT], writes=[bpo], signal=(hp == 31))
                fw.op(V, lambda e, j=j: e.tensor_copy(out=yoffT[:, :, 8 * j:8 * j + 8], in_=v3(pto[:, 0:256], 32)), reads=[bpo], writes=[b_yoffT])
                if stage < 5.7:
                    continue
                bmj, bbm = bmjr.next()
                fw.op(V, lambda e, bmj=bmj, j=j: e.tensor_scalar(out=bmj, in0=bt, scalar1=maskj[:, j:j + 1], scalar2=None, op0=ALU.mult), reads=[bbt, b_maskj], writes=[bbm])
                for g in range(8):
                    pts, bps = bank(6 + g % 2)
                    fw.op(T, lambda e, g=g, bmj=bmj, pts=pts: e.matmul(pts[:, 0:512], lhsT=bmj[:, g * 128:(g + 1) * 128], rhs=xw[:, g * 512:(g + 1) * 512], start=True, stop=True),
                          reads=[bbm, bxw], writes=[bps])
                    hng = hnT[:, g * 512:(g + 1) * 512]
                    fw.op(V, lambda e, hng=hng, g=g, j=j: e.tensor_tensor(out=v3(hng, 8), in0=v3(h0T[:, g * 512:(g + 1) * 512], 8),
                                                                        in1=decs[:, j * 64 + 8 * g:j * 64 + 8 * g + 8].unsqueeze(2).broadcast_to([128, 8, 64]), op=ALU.mult),
                          reads=[b_h0T, b_decs], writes=[b_hnT])
                    fw.op(V, lambda e, hng=hng, pts=pts: e.tensor_tensor(out=hng, in0=hng, in1=pts[:, 0:512], op=ALU.add), reads=[b_hnT, bps], writes=[b_hnT])
                ho, bho = h0r.next()
                for q4 in range(8):
                    ptx, bpx = bank(q4 % 4)
                    for u in range(4):
                        hp = q4 * 4 + u
                        fw.op(T, lambda e, ptx=ptx, u=u, hp=hp: e.transpose(out=ptx[:, u * 128:(u + 1) * 128], in_=hnT[:, hp * 128:(hp + 1) * 128], identity=idf),
                              reads=[b_hnT, b_idf], writes=[bpx], signal=(u == 3))
                    if q4 % 2:
                        fw.op(S, lambda e, ptx=ptx, q4=q4, ho=ho: e.copy(out=ho[:, q4 * 4:(q4 + 1) * 4, :], in_=v3(ptx[:, 0:512], 4)), reads=[bpx], writes=[bho])
                    else:
                        fw.op(V, lambda e, ptx=ptx, q4=q4, ho=ho: e.tensor_copy(out=ho[:, q4 * 4:(q4 + 1) * 4, :], in_=v3(ptx[:, 0:512], 4)), reads=[bpx], writes=[bho])
                fw.dma(G, o_ssm_s[j].rearrange("h p n -> (h p) n").rearrange("(hp q) n -> q hp n", q=128), ho, src=bho)
        samp.yoffT = (yoffT, b_yoffT)
        if stage >= 5.4:
            ssd_tile(8, "samp", sbufs, samp=samp)
        fw.pop()
        fw.pop()
        fw.barrier()

    def rms_stats(src, ns, bns, junk, bjunk, bsrc, eps, n):
        fw.op(V, lambda e: e.memset(ns[:, 0:1], 0.0), writes=[bns])
        fw.op(S, lambda e: e.activation(out=junk, in_=src, func=AF.Square, accum_out=ns[:, 0:1]), reads=(bsrc if isinstance(bsrc, list) else [bsrc]), writes=[bjunk, bns])
        fw.op(V, lambda e: e.tensor_scalar(out=ns[:, 1:2], in0=ns[:, 0:1], scalar1=1.0 / n, scalar2=eps, op0=ALU.mult, op1=ALU.add), reads=[bns], writes=[bns])
        fw.op(S, lambda e: e.activation(out=ns[:, 2:3], in_=ns[:, 1:2], func=AF.Sqrt), reads=[bns], writes=[bns])
        fw.op(V, lambda e: e.reciprocal(out=ns[:, 3:4], in_=ns[:, 2:3]), reads=[bns], writes=[bns])

    if stage >= 6:
        fw.push()
        offR1 = fw.off
        x2, _ = fw.alloc([128, NTM, D], F32, "x2")
        b_x2 = [[fw.buf(f"x2_{i}_{q}") for q in range(4)] for i in range(NTM)]
        yaTs = fw.alias(offR1, [128, 32, TM], BF16); b_yaTs = fw.buf("yaTs")
        offR2 = fw.off
        xn2, _ = fw.alloc([128, NTM, D], BF16, "xn2")
        b_xn2 = [fw.buf(f"xn2_{i}") for i in range(NTM)]
        ybTs = fw.alias(offR2, [128, 16, TM], BF16); b_ybTs = fw.buf("ybTs")
        offR3 = fw.off
        _r3, _ = fw.alloc([128, 20480], BF16, "R3")
        mT = fw.alias(offR3, [128, 16, TM], BF16)
        b_mT = [fw.buf(f"mT{d}") for d in range(16)]
        Mb_all, b_Mb = fw.alloc([128, NTM, 32], BF16, "Mb_all")
        rk_all, b_rk = fw.alloc([128, NTM, 32], F32, "rk_all")
        Wt_all, b_Wt = fw.alloc([128, NTM, 32], F32, "Wt_all")

        fw.push()
        wbr = fw.ring(2, [128, 48, 128], BF16, "wbo")
        gar = fw.ring(2, [128, 384], F32, "ga")
        gbr = fw.ring(2, [128, 384], F32, "gb")
        tmr = fw.ring(2, [128, 384], F32, "tmpm")
        tm2r = fw.ring(2, [128, 384], F32, "tmpm2")
        for k in range(32):
            fw.dma(SY, yaTs[:, k, :], yaT[k * 128:(k + 1) * 128, :], dst=b_yaTs)
        for k in range(16):
            fw.dma(SY, ybTs[:, k, :], ybT[k * 128:(k + 1) * 128, :], dst=b_ybTs)
        wbo_v = w_bo_d.rearrange("(k p) c -> p k c", p=128)
        pi = 0
        for d in range(16):
            wt, bw = wbr.next()
            fw.dma(G, wt, wbo_v[:, :, d * 128:(d + 1) * 128], dst=bw)
            for (t0, tn) in TB:
                ga, bga = gar.next(); gb, bgb = gbr.next()
                fw.dma(SY, ga, gateT[d * 128:(d + 1) * 128, t0:t0 + tn], dst=bga)
                fw.dma(SY, gb, gateT[2048 + d * 128:2048 + (d + 1) * 128, t0:t0 + tn], dst=bgb)
                pa, bpa = bank(pi % 8); pi += 1
                for k in range(32):
                    fw.op(T, lambda e, pa=pa, wt=wt, k=k, t0=t0, tn=tn: e.matmul(pa[:, 0:tn], lhsT=wt[:, k, :], rhs=yaTs[:, k, t0:t0 + tn], start=(k == 0), stop=(k == 31)),
                          reads=[bw, b_yaTs], writes=[bpa], signal=(k == 31))
                pb, bpb = bank(pi % 8); pi += 1
                for k in range(16):
                    fw.op(T, lambda e, pb=pb, wt=wt, k=k, t0=t0, tn=tn: e.matmul(pb[:, 0:tn], lhsT=wt[:, 32 + k, :], rhs=ybTs[:, k, t0:t0 + tn], start=(k == 0), stop=(k == 15)),
                          reads=[bw, b_ybTs], writes=[bpb], signal=(k == 15))
                tm, btm = tmr.next(); tm2, btm2 = tm2r.next()
                fw.op(V, lambda e, pa=pa, ga=ga, tm=tm, t0=t0, tn=tn: e.tensor_tensor(out=tm[:, 0:tn], in0=pa[:, 0:tn], in1=ga[:, 0:tn], op=ALU.mult),
                      reads=[bpa, bga], writes=[btm])
                fw.op(V, lambda e, pb=pb, gb=gb, tm2=tm2, t0=t0, tn=tn: e.tensor_tensor(out=tm2[:, 0:tn], in0=pb[:, 0:tn], in1=gb[:, 0:tn], op=ALU.mult),
                      reads=[bpb, bgb], writes=[btm2])
                fw.op(V, lambda e, tm=tm, tm2=tm2, d=d, t0=t0, tn=tn: e.tensor_tensor(out=mT[:, d, t0:t0 + tn], in0=tm[:, 0:tn], in1=tm2[:, 0:tn], op=ALU.add),
                      reads=[btm, btm2], writes=[b_mT[d]])
        fw.pop()
        fw.barrier()
        fw.push()
        wor = fw.ring(2, [128, 16, 512], BF16, "wo")
        xrr = fw.ring(2, [128, 512], F32, "xres")
        for dq in range(4):
            wo, bwo = wor.next()
            fw.dma(G, wo, w_out_d.rearrange("(k p) c -> p k c", p=128)[:, :, dq * 512:(dq + 1) * 512], dst=bwo)
            for i in range(NTM):
                xr, bxr = xrr.next()
                fw.dma(SY, xr, xm_d[i * 128:(i + 1) * 128, dq * 512:(dq + 1) * 512], dst=bxr)
                pt, bp = bank(pi % 8); pi += 1
                for k in range(16):
                    fw.op(T, lambda e, pt=pt, k=k, i=i, wo=wo: e.matmul(pt[:, 0:512], lhsT=mT[:, k, i * 128:(i + 1) * 128], rhs=wo[:, k, :],
                                                                     start=(k == 0), stop=(k == 15)), reads=[b_mT[k], bwo], writes=[bp], signal=(k == 15))
                c0 = dq * 512
                fw.op(V, lambda e, pt=pt, xr=xr, i=i, c0=c0: e.tensor_tensor(out=x2[:, i, c0:c0 + 512], in0=pt[:, 0:512], in1=xr, op=ALU.add),
                      reads=[bp, bxr], writes=[b_x2[i][dq]])
        fw.pop()
        fw.barrier()
        fw.push()
        nfb, b_nfb = fw.alloc([128, D], F32, "nfb")
        wr_sb, b_wr = fw.alloc([128, 16, 36], F32, "wr_sb")
        strb, b_strb = fw.alloc([128, 128], BF16, "strb")
        onesb, b_onesb = fw.alloc([128, 128], BF16, "onesb")
        fw.dma(SY, nfb, nf_d, dst=b_nfb)
        fw.dma(SY, wr_sb, w_r_d, dst=b_wr)
        fw.dma(G, strb, stri_d, dst=b_strb)
        fw.op(V, lambda e: e.memset(onesb, 1.0), writes=[b_onesb])
        xnf, b_xnf = fw.alloc([128, D], F32, "xnf")
        xfT, b_xfT = fw.alloc([128, 16, 128], F32, "xfT")
        rsr = fw.ring(2, [128, 160], F32, "rs")
        nsr = fw.ring(2, [128, 4], F32, "nsr")
        for i in range(NTM):
            ns, bns = nsr.next()
            rms_stats(x2[:, i, :], ns, bns, xn2[:, i, :], b_xn2[i], b_x2[i], 1e-6, D)
            fw.op(V, lambda e, ns=ns, i=i: e.scalar_tensor_tensor(out=xnf, in0=x2[:, i, :], scalar=ns[:, 3:4], in1=nfb, op0=ALU.mult, op1=ALU.mult),
                  reads=b_x2[i] + [bns, b_nfb], writes=[b_xnf])
            fw.op(V, lambda e, i=i: e.tensor_copy(out=xn2[:, i, :], in_=xnf), reads=[b_xnf], writes=[b_xn2[i]])
            for q in range(4):
                pt, bp = bank(pi % 8); pi += 1
                for u in range(4):
                    k = 4 * q + u
                    fw.op(T, lambda e, pt=pt, u=u, k=k: e.transpose(out=pt[:, u * 128:(u + 1) * 128], in_=xnf[:, k * 128:(k + 1) * 128], identity=idf),
                          reads=[b_xnf, b_idf], writes=[bp], signal=(u == 3))
                fw.op(V, lambda e, pt=pt, q=q: e.tensor_copy(out=xfT[:, 4 * q:4 * q + 4, :], in_=v3(pt[:, 0:512], 4)), reads=[bp], writes=[b_xfT])
            pt, bp = bank(pi % 8); pi += 1
            for k in range(16):
                fw.op(T, lambda e, pt=pt, k=k: e.matmul(pt[:, 0:36], lhsT=xfT[:, k, :], rhs=wr_sb[:, k, :], start=(k == 0), stop=(k == 15)),
                      reads=[b_xfT, b_wr], writes=[bp], signal=(k == 15))
            r, br = rsr.next()
            lg = r[:, 0:36]; mx = r[:, 36:37]; nmx = r[:, 37:38]; sg = r[:, 38:39]; pg = r[:, 39:40]; ohg = r[:, 40:44]; eg = r[:, 44:48]
            pen = r[:, 48:52]; m1 = r[:, 52:53]; m2 = r[:, 53:54]; dd = r[:, 54:55]; w1 = r[:, 55:56]; lfm = r[:, 56:88]; oh1 = r[:, 88:120]
            lf2 = r[:, 120:152]; w1p = r[:, 152:153]; w2p = r[:, 153:154]

            def vop(f, extra_r=(), extra_w=()):
                fw.op(V, f, reads=[br] + list(extra_r), writes=[br] + list(extra_w))
            fw.op(V, lambda e, pt=pt: e.tensor_copy(out=lg, in_=pt[:, 0:36]), reads=[bp], writes=[br])
            vop(lambda e: e.tensor_reduce(out=mx, in_=lg[:, 0:4], axis=AX.X, op=ALU.max))
            vop(lambda e: e.tensor_scalar(out=ohg, in0=lg[:, 0:4], scalar1=mx, scalar2=None, op0=ALU.is_equal))
            vop(lambda e: e.tensor_scalar(out=nmx, in0=mx, scalar1=-1.0, scalar2=None, op0=ALU.mult))
            fw.op(S, lambda e: e.activation(out=eg, in_=lg[:, 0:4], func=AF.Exp, bias=nmx, scale=1.0), reads=[br], writes=[br])
            vop(lambda e: e.reduce_sum(out=sg, in_=eg, axis=AX.X))
            vop(lambda e: e.reciprocal(out=pg, in_=sg))
            vop(lambda e: e.tensor_scalar(out=pen, in0=ohg, scalar1=-1.0, scalar2=1e30, op0=ALU.add, op1=ALU.mult))
            vop(lambda e: e.tensor_tensor(out=v3(lfm, 4), in0=v3(lg[:, 4:36], 4), in1=pen.unsqueeze(2).broadcast_to([128, 4, 8]), op=ALU.add))
            vop(lambda e: e.tensor_reduce(out=m1, in_=lfm, axis=AX.X, op=ALU.max))
            vop(lambda e: e.tensor_scalar(out=oh1, in0=lfm, scalar1=m1, scalar2=None, op0=ALU.is_equal))
            vop(lambda e: e.scalar_tensor_tensor(out=lf2, in0=oh1, scalar=-1e30, in1=lfm, op0=ALU.mult, op1=ALU.add))
            vop(lambda e: e.tensor_reduce(out=m2, in_=lf2, axis=AX.X, op=ALU.max))
            vop(lambda e: e.tensor_scalar(out=lfm, in0=lf2, scalar1=m2, scalar2=None, op0=ALU.is_equal))
            vop(lambda e: e.tensor_tensor(out=dd, in0=m2, in1=m1, op=ALU.subtract))
            fw.op(S, lambda e: e.activation(out=dd, in_=dd, func=AF.Exp), reads=[br], writes=[br])
            vop(lambda e: e.tensor_scalar(out=dd, in0=dd, scalar1=1.0, scalar2=None, op0=ALU.add))
            vop(lambda e: e.reciprocal(out=w1, in_=dd))
            vop(lambda e: e.tensor_tensor(out=w1p, in0=w1, in1=pg, op=ALU.mult))
            vop(lambda e: e.tensor_tensor(out=w2p, in0=pg, in1=w1p, op=ALU.subtract))
            vop(lambda e, i=i: e.tensor_scalar(out=Wt_all[:, i, :], in0=oh1, scalar1=w1p, scalar2=None, op0=ALU.mult), extra_w=[b_Wt])
            vop(lambda e, i=i: e.scalar_tensor_tensor(out=Wt_all[:, i, :], in0=lfm, scalar=w2p, in1=Wt_all[:, i, :], op0=ALU.mult, op1=ALU.add), extra_r=[b_Wt], extra_w=[b_Wt])
            vop(lambda e, i=i: e.tensor_tensor(out=Mb_all[:, i, :], in0=oh1, in1=lfm, op=ALU.add), extra_w=[b_Mb])
            pt2, bp2 = bank(pi % 8); pi += 1
            for ip in range(i):
                fw.op(T, lambda e, pt2=pt2, ip=ip: e.matmul(pt2[:, 0:32], lhsT=onesb, rhs=Mb_all[:, ip, :], start=(ip == 0), stop=False),
                      reads=[b_onesb, b_Mb], writes=[bp2], signal=False)
            fw.op(T, lambda e, pt2=pt2, i=i: e.matmul(pt2[:, 0:32], lhsT=strb, rhs=Mb_all[:, i, :], start=(i == 0), stop=True), reads=[b_strb, b_Mb], writes=[bp2])
            fw.op(V, lambda e, pt2=pt2, i=i: e.scalar_tensor_tensor(out=rk_all[:, i, :], in0=pt2[:, 0:32], scalar=1.0, in1=Mb_all[:, i, :], op0=ALU.add, op1=ALU.mult),
                  reads=[bp2, b_Mb], writes=[b_rk])
            fw.op(V, lambda e, i=i: e.tensor_scalar(out=rk_all[:, i, :], in0=rk_all[:, i, :], scalar1=-1.0, scalar2=None, op0=ALU.add), reads=[b_rk], writes=[b_rk])
        if debug:
            dbg_rk = dout("dbg_rk", [128, NTM * 32]); dbg_wt = dout("dbg_wt", [128, NTM * 32])
            fw.dma(SY, dbg_rk, rk_all.rearrange("p a b -> p (a b)"), src=b_rk)
            fw.dma(SY, dbg_wt, Wt_all.rearrange("p a b -> p (a b)"), src=b_Wt)
        fw.pop()
        fw.barrier()

    if stage >= 7:
        fw.push()
        iot, b_iot = fw.alloc([128, 128], F32, "iot")
        fw.dma(SY, iot, iota_d, dst=b_iot)
        Sr = fw.ring(2, [128, NTM, 128], BF16, "Ssel")
        SWr = fw.ring(1, [128, NTM, 128], BF16, "SWsel")
        SWTr = fw.ring(2, [128, NTM, 128], BF16, "SWT")
        xgr = fw.ring(1, [128, 16, 128], BF16, "xgT")
        xgmr = fw.ring(1, [128, 2048], BF16, "xgm")
        hsr = fw.ring(1, [128, 1024], F32, "hs")
        hbr = fw.ring(1, [128, 1024], BF16, "hb")
        hTr = fw.ring(2, [128, 8, 128], BF16, "hT")
        yer = fw.ring(1, [128, 2048], BF16, "yexp")
        wslots = [(fw.alias(offR3 + q * 8192, [128, 4096], BF16), fw.buf(f"wslot{q}")) for q in range(5)]
        wring = Ring(wslots)
        NEXP = NE if stage >= 7.5 else 2
        ci = 0
        for ex in range(NEXP):
            S_, bS = Sr.next(); SW, bSW = SWr.next(); SWT, bSWT = SWTr.next()
            for i in range(NTM):
                fw.op(V, lambda e, S_=S_, i=i, ex=ex: e.tensor_scalar(out=S_[:, i, :], in0=iot, scalar1=rk_all[:, i, ex:ex + 1], scalar2=None, op0=ALU.is_equal),
                      reads=[b_iot, b_rk], writes=[bS])
                fw.op(V, lambda e, SW=SW, i=i, ex=ex: e.tensor_scalar(out=SW[:, i, :], in0=iot, scalar1=rk_all[:, i, ex:ex + 1], scalar2=Wt_all[:, i, ex:ex + 1],
                                                                     op0=ALU.is_equal, op1=ALU.mult), reads=[b_iot, b_rk, b_Wt], writes=[bSW])
            for (i0, i1, bk) in ((0, 8, 0), (8, 9, 1)):
                pt, bp = bank16(bk)
                for i in range(i0, i1):
                    fw.op(T, lambda e, pt=pt, i=i, i0=i0, SW=SW: e.transpose(out=pt[:, (i - i0) * 128:(i - i0 + 1) * 128], in_=SW[:, i, :], identity=idb),
                          reads=[bSW, b_idb], writes=[bp], signal=(i == i1 - 1))
                fw.op(V, lambda e, pt=pt, i0=i0, i1=i1, SWT=SWT: e.tensor_copy(out=SWT[:, i0:i1, :], in_=v3(pt[:, 0:(i1 - i0) * 128], i1 - i0)), reads=[bp], writes=[bSWT])
            xgT, bxg = xgr.next()
            xg, bxgm = xgmr.next()
            for db in range(4):
                pt, bp = bank(2 + db % 2)
                for i in range(NTM):
                    fw.op(T, lambda e, pt=pt, i=i, db=db, S_=S_: e.matmul(pt[:, 0:512], lhsT=S_[:, i, :], rhs=xn2[:, i, db * 512:(db + 1) * 512],
                                                                       start=(i == 0), stop=(i == NTM - 1)),
                          reads=[b_xn2[i], bS], writes=[bp], signal=(i == NTM - 1))
                if db % 2 == 0:
                    fw.op(V, lambda e, pt=pt, db=db, xg=xg: e.tensor_copy(out=xg[:, db * 512:(db + 1) * 512], in_=pt[:, 0:512]), reads=[bp], writes=[bxgm])
                else:
                    fw.op(S, lambda e, pt=pt, db=db, xg=xg: e.activation(out=xg[:, db * 512:(db + 1) * 512], in_=pt[:, 0:512], func=AF.Identity), reads=[bp], writes=[bxgm])
            for half in range(2):
                pt, bp = bank16(2 + half)
                for u in range(8):
                    k = half * 8 + u
                    fw.op(T, lambda e, pt=pt, u=u, k=k, xg=xg: e.transpose(out=pt[:, u * 128:(u + 1) * 128], in_=xg[:, k * 128:(k + 1) * 128], identity=idb),
                          reads=[bxgm, b_idb], writes=[bp], signal=(u == 7))
                if half == 0:
                    fw.op(V, lambda e, pt=pt, xgT=xgT: e.tensor_copy(out=xgT[:, 0:8, :], in_=v3(pt[:, 0:1024], 8)), reads=[bp], writes=[bxg])
                else:
                    fw.op(S, lambda e, pt=pt, xgT=xgT: e.activation(out=xgT[:, 8:16, :], in_=v3(pt[:, 0:1024], 8), func=AF.Identity), reads=[bp], writes=[bxg])
            hgT, bhg0, bhg1 = PS[2]
            huT, bhu0, bhu1 = PS[3]
            for q in range(4):
                pieces = []
                for (wd_, acc, ba, bb) in ((w_eg_d, hgT, bhg0, bhg1), (w_eu_d, huT, bhu0, bhu1)):
                    wsl, bws = wring.next()
                    wv = v3(wsl, 4)
                    fw.dma(G, wv, wd_[ex].rearrange("(k p) c -> p k c", p=128)[:, 4 * q:4 * q + 4, :], dst=bws)
                    pieces.append((wv, bws, acc, ba, bb))
                for (wv, bws, acc, ba, bb) in pieces:
                    for kk in range(4):
                        k = 4 * q + kk
                        for half in range(2):
                            fw.op(T, lambda e, acc=acc, wv=wv, kk=kk, k=k, half=half, xgT=xgT: e.matmul(acc[:, half * 512:(half + 1) * 512], lhsT=xgT[:, k, :],
                                                                                                rhs=wv[:, kk, half * 512:(half + 1) * 512], start=(k == 0), stop=(k == 15)),
                                  reads=[bxg, bws], writes=[ba, bb], signal=(kk == 3 and half == 1))
            hs, bhs = hsr.next(); hb, bhb = hbr.next(); hT, bhT = hTr.next()
            for half, (ba, bb) in enumerate(((bhg0, bhu0), (bhg1, bhu1))):
                sl = slice(half * 512, (half + 1) * 512)
                fw.op(S, lambda e, sl=sl, hs=hs: e.activation(out=hs[:, sl], in_=hgT[:, sl], func=AF.Silu), reads=[ba], writes=[bhs])
                fw.op(V, lambda e, sl=sl, hs=hs, hb=hb: e.tensor_tensor(out=hb[:, sl], in0=hs[:, sl], in1=huT[:, sl], op=ALU.mult), reads=[bhs, bb], writes=[bhb])
            pt, bp = bank16(0)
            for k in range(8):
                fw.op(T, lambda e, pt=pt, k=k, hb=hb: e.transpose(out=pt[:, k * 128:(k + 1) * 128], in_=hb[:, k * 128:(k + 1) * 128], identity=idb),
                      reads=[bhb, b_idb], writes=[bp], signal=(k == 7))
            fw.op(V, lambda e, pt=pt, hT=hT: e.tensor_copy(out=hT, in_=v3(pt[:, 0:1024], 8)), reads=[bp], writes=[bhT])
            ydb = [bank(2), bank(3), bank(4), bank(5)]
            for q in range(4):
                wsl, bws = wring.next()
                wv = v3(wsl, 2)
                fw.dma(G, wv, w_ed_d[ex].rearrange("(k p) c -> p k c", p=128)[:, 2 * q:2 * q + 2, :], dst=bws)
                for kk in range(2):
                    k = 2 * q + kk
                    for db in range(4):
                        pt, bp = ydb[db]
                        fw.op(T, lambda e, pt=pt, wv=wv, kk=kk, k=k, db=db, hT=hT: e.matmul(pt[:, 0:512], lhsT=hT[:, k, :], rhs=wv[:, kk, db * 512:(db + 1) * 512],
                                                                                       start=(k == 0), stop=(k == 7)),
                              reads=[bhT, bws], writes=[bp], signal=(kk == 1 and db == 3))
            ye, bye = yer.next()
            for db in range(4):
                pt, bp = ydb[db]
                if db % 2 == 0:
                    fw.op(V, lambda e, pt=pt, db=db, ye=ye: e.tensor_copy(out=ye[:, db * 512:(db + 1) * 512], in_=pt[:, 0:512]), reads=[bp], writes=[bye])
                else:
                    fw.op(S, lambda e, pt=pt, db=db, ye=ye: e.activation(out=ye[:, db * 512:(db + 1) * 512], in_=pt[:, 0:512], func=AF.Identity), reads=[bp], writes=[bye])
            for i in range(NTM):
                for db in range(4):
                    pt, bp = bank((0, 1, 6, 7)[ci % 4]); ci += 1
                    fw.op(T, lambda e, pt=pt, i=i, db=db, SWT=SWT, ye=ye: e.matmul(pt[:, 0:512], lhsT=SWT[:, i, :], rhs=ye[:, db * 512:(db + 1) * 512], start=True, stop=True),
                          reads=[bSWT, bye], writes=[bp])
                    fw.op(V, lambda e, pt=pt, i=i, db=db: e.tensor_tensor(out=x2[:, i, db * 512:(db + 1) * 512], in0=x2[:, i, db * 512:(db + 1) * 512], in1=pt[:, 0:512], op=ALU.add),
                          reads=[bp, b_x2[i][db]], writes=[b_x2[i][db]])
        fw.pop()
        fw.barrier()

    if stage >= 6:
        fw.push()
        nlb, b_nlb = fw.alloc([128, D], F32, "nlb")
        fw.dma(SY, nlb, nl_d, dst=b_nlb)
        yor = fw.ring(2, [128, D], F32, "yo")
        nsr = fw.ring(2, [128, 4], F32, "nsr2")
        for i in range(NTM):
            yo, byo = yor.next(); ns, bns = nsr.next()
            rms_stats(x2[:, i, :], ns, bns, yo, byo, b_x2[i], 1e-6, D)
            fw.op(V, lambda e, yo=yo, ns=ns, i=i: e.scalar_tensor_tensor(out=yo, in0=x2[:, i, :], scalar=ns[:, 3:4], in1=nlb, op0=ALU.mult, op1=ALU.mult),
                  reads=b_x2[i] + [bns, b_nlb], writes=[byo])
            fw.dma(G, y_d[i * 128:(i + 1) * 128, :], yo, src=byo)
        fw.pop()
        fw.pop()
        fw.barrier()

    fw.emit()
    return nc, es


def _consts():
    t = np.arange(128)
    tri_p = (t[:, None] <= t[None, :]).astype(np.float32)
    same = (t[:, None] // 8 == t[None, :] // 8)
    tri_s = (tri_p * same).astype(np.float32)
    bones = np.stack([np.ones((128, 128), np.float32), same.astype(np.float32)])
    maskj = (t[:, None] // 8 == np.arange(16)[None, :]).astype(np.float32)
    onesj = np.ascontiguousarray(np.broadcast_to(maskj[:, :, None], (128, 16, 128))).astype(np.float32)
    return dict(ident=np.eye(128, dtype=np.float32), tri=np.stack([tri_p, tri_s]), bones=bones, onesj=onesj, maskj=maskj,
                iota=np.ascontiguousarray(np.broadcast_to(np.arange(128, dtype=np.float32)[None, :], (128, 128))),
                stri=(t[:, None] < t[None, :]).astype(np.float32))


def _bc(v, n=128):
    return np.ascontiguousarray(np.broadcast_to(np.asarray(v, np.float32).reshape(1, -1), (n, v.size)))


def make_in_maps(inp, cores=range(8)):
    f = lambda a: np.ascontiguousarray(np.asarray(a, dtype=np.float32))
    xpr, xs = f(inp["x_prompt"]), f(inp["x_sample"])
    shared = dict(
        w_in=f(inp["w_in"][0]), w_bo=f(inp["w_branch_out"][0]), w_out=f(inp["w_out"][0]),
        w_eg=f(inp["w_expert_gate"][0]), w_eu=f(inp["w_expert_up"][0]), w_ed=f(inp["w_expert_down"][0]),
        w_r=np.ascontiguousarray(np.concatenate([f(inp["w_router_coarse"][0]), f(inp["w_router_fine"][0])], axis=1)
                                 .reshape(16, 128, 36).transpose(1, 0, 2)),
        nm_bc=_bc(f(inp["norm_mixer"][0])), nf_bc=_bc(f(inp["norm_ffn"][0])), nl_bc=_bc(f(inp["norm_final"])),
        ssdn_bc=_bc(f(inp["ssd_norm"][0])), dtb_bc=_bc(f(inp["ssd_dt_bias"][0])), alog_bc=_bc(f(inp["ssd_a_log"][0])),
        dsk_bc=_bc(np.repeat(f(inp["ssd_d"][0]), 64)),
        cw_fm=np.ascontiguousarray(f(inp["ssd_conv_w"][0]).reshape(4, 48, 128).transpose(2, 1, 0)),
        cb_fm=np.ascontiguousarray(f(inp["ssd_conv_b"][0]).reshape(48, 128).T),
        scw_fm=np.ascontiguousarray(f(inp["sc_conv_w"][0]).reshape(3, 16, 128).transpose(2, 1, 0)),
        **_consts())
    maps = []
    for c in cores:
        s, h = c // 2, c % 2
        m = dict(shared)
        m["xm"] = np.concatenate([xpr[s, h * 1024:(h + 1) * 1024], xs[16 * c:16 * c + 16].reshape(128, D)], axis=0)
        m["xp"] = xpr[s, 0:1024]
        m["flag"] = np.full((128, 1), float(h), np.float32)
        m["st_ssm"] = f(inp["state_ssm"][0, 16 * c:16 * c + 16])
        m["st_conv"] = f(inp["state_ssd_conv"][0, 16 * c:16 * c + 16]).reshape(48, 6144)
        m["st_sc"] = f(inp["state_short_conv"][0, 16 * c:16 * c + 16]).reshape(32, 2048)
        maps.append(m)
    return maps


_CACHE = {}


def kernel(**inp):
    if "nc" not in _CACHE:
        _CACHE["nc"] = build_program()
    nc, _ = _CACHE["nc"]
    maps = make_in_maps(inp)
    res = run_bass_kernel_spmd(nc, maps, core_ids=list(range(8))).results
    y_prompt = np.zeros((4, 2048, D), np.float32)
    y_sample = np.zeros((128, 8, D), np.float32)
    p_ssm = np.zeros((1, 4, 64, 64, 128), np.float32)
    p_conv = np.zeros((1, 4, 3, 6144), np.float32)
    p_sc = np.zeros((1, 4, 2, 2048), np.float32)
    s_ssm = np.zeros((1, 128, 64, 64, 128), np.float32)
    s_conv = np.zeros((1, 128, 3, 6144), np.float32)
    s_sc = np.zeros((1, 128, 2, 2048), np.float32)
    for c in range(8):
        r = res[c]
        s, h = c // 2, c % 2
        y_prompt[s, h * 1024:(h + 1) * 1024] = r["y"][0:1024]
        y_sample[16 * c:16 * c + 16] = r["y"][1024:1152].reshape(16, 8, D)
        s_ssm[0, 16 * c:16 * c + 16] = r["o_ssm_s"]
        s_conv[0, 16 * c:16 * c + 16] = r["o_conv_s"].reshape(16, 3, 6144)
        s_sc[0, 16 * c:16 * c + 16] = r["o_sc_s"].reshape(16, 2, 2048)
        if h == 1:
            p_ssm[0, s] = r["o_ssm_p"]
            p_conv[0, s] = r["o_conv_p"]
            p_sc[0, s] = r["o_sc_p"]
    return (y_prompt, y_sample, p_ssm, p_conv, p_sc, s_ssm, s_conv, s_sc)
```

```python
import numpy as np
import concourse.bass as bass
import concourse.mybir as mybir
from concourse.bass_utils import run_bass_kernel_spmd

F32 = mybir.dt.float32
BF16 = mybir.dt.bfloat16
ALU = mybir.AluOpType
AF = mybir.ActivationFunctionType
AX = mybir.AxisListType
SAME_ENGINE_SYNC = True

D = 2048
TM, NTM = 1152, 9
TP, NTP = 1024, 8
NPROJ = 20544
C_GA, C_GB, C_Z, C_X, C_B, C_C, C_DT, C_SB, C_SC, C_SH = 0, 2048, 4096, 8192, 12288, 13312, 14336, 14400, 16448, 18496
NE, CAP = 32, 128


class Buf:
    __slots__ = ("name", "w", "r", "sem", "cnt")

    def __init__(self, name):
        self.name = name
        self.w = None
        self.r = []
        self.sem = None
        self.cnt = 0


class Eng:
    def __init__(self, name, sem):
        self.name = name
        self.sem = sem
        self.cnt = 0
        self.seen = {}
        self.prog = []
        self.pr = []
        self.pw = []


class FW:
    def __init__(self, nc, es):
        self.nc = nc
        self.es = es
        self.eng = {}
        for n in ("tensor", "vector", "scalar", "gpsimd", "sync"):
            self.eng[n] = Eng(n, self.new_sem("e_" + n))
        self.dma_sems = []
        self.semcnt = {}
        self.nbuf = 0
        self.big = es.enter_context(nc.sbuf_tensor("big", [128, 51200], F32))
        self.big16 = self.big.bitcast(BF16)
        self.off = 0
        self.marks = []
        self.free_sems = []

    def new_sem(self, name):
        return self.es.enter_context(self.nc.semaphore(name))

    def buf(self, name=None):
        self.nbuf += 1
        return Buf(name or f"b{self.nbuf}")

    def push(self):
        self.marks.append((self.off, []))

    def pop(self):
        self.off, bufs = self.marks.pop()
        for b in bufs:
            if b.sem is not None:
                self.free_sems.append(b.sem)
                b.sem = None

    def alloc(self, shape, dt, name=None):
        n = 1
        for s in shape[1:]:
            n *= s
        esz = 4 if dt == F32 else 2
        nbytes = (n * esz + 31) // 32 * 32
        assert self.off + nbytes <= 51200 * 4, f"SBUF overflow {name} {self.off} + {nbytes}"
        if dt == F32:
            o = self.off // 4
            ap = self.big[0:shape[0], o:o + n]
        else:
            o = self.off // 2
            ap = self.big16[0:shape[0], o:o + n]
        self.off += nbytes
        bufobj = self.buf(name)
        if self.marks:
            self.marks[-1][1].append(bufobj)
        if len(shape) == 3:
            ap = ap.rearrange("p (a b) -> p a b", a=shape[1])
        elif len(shape) == 4:
            ap = ap.rearrange("p (a b c) -> p a b c", a=shape[1], b=shape[2])
        return ap, bufobj

    def alias(self, off, shape, dt):
        n = 1
        for x in shape[1:]:
            n *= x
        if dt == F32:
            ap = self.big[0:shape[0], off // 4:off // 4 + n]
        else:
            ap = self.big16[0:shape[0], off // 2:off // 2 + n]
        if len(shape) == 3:
            ap = ap.rearrange("p (a b) -> p a b", a=shape[1])
        return ap

    def ring(self, n, shape, dt, name=None):
        return Ring([self.alloc(shape, dt, f"{name}{i}") for i in range(n)])

    def _need(self, E, tks):
        best = {}
        for (s, v) in tks:
            k = id(s)
            if k not in best or best[k][1] < v:
                best[k] = (s, v)
        for k, (s, v) in best.items():
            if (s is E.sem) and not SAME_ENGINE_SYNC:
                continue
            if E.seen.get(k, 0) >= v:
                continue
            E.seen[k] = v
            E.prog.append(lambda e, s=s, v=v: e.wait_ge(s, v))

    def _chk(self, en, reads, writes):
        for n2, E2 in self.eng.items():
            if n2 == en:
                continue
            for b in writes:
                assert all(b is not p for p in E2.pr) and all(b is not p for p in E2.pw), f"pending hazard {b.name} {n2}"
            for b in reads:
                assert all(b is not p for p in E2.pw), f"pending hazard {b.name} {n2}"

    def op(self, en, build, reads=(), writes=(), signal=True):
        E = self.eng[en]
        self._chk(en, reads, writes)
        tks = []
        for b in reads:
            if b.w:
                tks.append(b.w)
        for b in writes:
            if b.w:
                tks.append(b.w)
            tks.extend(b.r)
        self._need(E, tks)
        if signal:
            E.cnt += 1
            tk = (E.sem, E.cnt)
            sem = E.sem
            E.prog.append(lambda e, build=build, sem=sem: build(e).then_inc(sem, 1))
            rs = list(reads) + E.pr
            ws = list(writes) + E.pw
            E.pr, E.pw = [], []
            for b in ws:
                b.w = tk
                b.r = []
            for b in rs:
                if b.w is not tk:
                    b.r.append(tk)
        else:
            E.prog.append(lambda e, build=build: build(e))
            E.pr.extend(reads)
            E.pw.extend(writes)

    def dma(self, en, out, in_, src=None, dst=None, owner=None):
        E = self.eng[en]
        assert not E.pr and not E.pw
        self._chk(en, [src] if src is not None else [], [dst] if dst is not None else [])
        tks = []
        if src is not None and src.w:
            tks.append(src.w)
        if dst is not None:
            if dst.w:
                tks.append(dst.w)
            tks.extend(dst.r)
        self._need(E, tks)
        if owner is None:
            owner = dst if dst is not None else src
        if owner.sem is None:
            if self.free_sems:
                owner.sem = self.free_sems.pop()
            else:
                owner.sem = self.new_sem(f"d{len(self.semcnt)}")
                self.semcnt[id(owner.sem)] = [owner.sem, 0]
        ent = self.semcnt[id(owner.sem)]
        ent[1] += 16
        tk = (owner.sem, ent[1])
        sem = owner.sem
        E.prog.append(lambda e, out=out, in_=in_, sem=sem: e.dma_start(out=out, in_=in_).then_inc(sem, 16))
        if dst is not None:
            dst.w = tk
            dst.r = []
        if src is not None:
            src.r.append(tk)

    def barrier(self):
        tks = [(E.sem, E.cnt) for E in self.eng.values() if E.cnt > 0]
        tks += [(s_, c_) for (s_, c_) in self.semcnt.values()]
        for E in self.eng.values():
            assert not E.pr and not E.pw, E.name
            self._need(E, tks)

    def emit(self):
        self.barrier()
        with self.nc.Block() as block:
            for en in ("tensor", "vector", "scalar", "gpsimd", "sync"):
                prog = self.eng[en].prog

                def body(e, prog=prog):
                    for f in prog:
                        f(e)
                getattr(block, en)(body)


class Ring:
    def __init__(self, items):
        self.items = items
        self.i = 0

    def next(self):
        r = self.items[self.i % len(self.items)]
        self.i += 1
        return r


def build_program(stage=99, debug=False):
    import contextlib
    nc = bass.Bass("TRN2", target_bir_lowering=False)
    es = contextlib.ExitStack()
    fw = FW(nc, es)

    def din(name, shape, dt=F32):
        return nc.dram_tensor(name, list(shape), dt, kind="ExternalInput").ap()

    def dout(name, shape, dt=F32):
        return nc.dram_tensor(name, list(shape), dt, kind="ExternalOutput").ap()

    def dscr(name, shape, dt):
        return nc.dram_tensor(name, list(shape), dt, kind="Internal").ap()

    xm_d = din("xm", [TM, D]); xp_d = din("xp", [TP, D]); flag_d = din("flag", [128, 1])
    st_ssm_d = din("st_ssm", [16, 64, 64, 128]); st_conv_d = din("st_conv", [48, 6144]); st_sc_d = din("st_sc", [32, 2048])
    w_in_d = din("w_in", [D, NPROJ]); w_bo_d = din("w_bo", [6144, D]); w_out_d = din("w_out", [D, D])
    w_eg_d = din("w_eg", [NE, D, 1024]); w_eu_d = din("w_eu", [NE, D, 1024]); w_ed_d = din("w_ed", [NE, 1024, D])
    w_r_d = din("w_r", [128, 16, 36])
    nm_d = din("nm_bc", [128, D]); nf_d = din("nf_bc", [128, D]); nl_d = din("nl_bc", [128, D])
    ssdn_d = din("ssdn_bc", [128, 4096]); dtb_d = din("dtb_bc", [128, 64]); alog_d = din("alog_bc", [128, 64])
    dsk_d = din("dsk_bc", [128, 4096])
    cw_d = din("cw_fm", [128, 48, 4]); cb_d = din("cb_fm", [128, 48]); scw_d = din("scw_fm", [128, 16, 3])
    ident_d = din("ident", [128, 128]); tri_d = din("tri", [2, 128, 128]); bones_d = din("bones", [2, 128, 128])
    onesj_d = din("onesj", [128, 16, 128]); maskj_d = din("maskj", [128, 16]); iota_d = din("iota", [128, 128])
    stri_d = din("stri", [128, 128])

    y_d = dout("y", [TM, D])
    o_ssm_p = dout("o_ssm_p", [64, 64, 128]); o_conv_p = dout("o_conv_p", [3, 6144]); o_sc_p = dout("o_sc_p", [2, 2048])
    o_ssm_s = dout("o_ssm_s", [16, 64, 64, 128]); o_conv_s = dout("o_conv_s", [48, 6144]); o_sc_s = dout("o_sc_s", [32, 2048])

    xbcT = dscr("xbcT", [6144, TM], F32); scT = dscr("scT", [6144, TM], F32)
    xbcT_p = dscr("xbcT_p", [5120, TP], F32)
    gateT = dscr("gateT", [4096, TM], F32); zs_s = dscr("zs", [TM, 4096], F32)
    x32_tm = dscr("x32_tm", [TM, 4096], F32)
    x_tm = dscr("x_tm", [TM, 4096], BF16); b_tm = dscr("b_tm", [TM, 1024], BF16)
    bT_s = dscr("bT", [1024, TM], BF16); cT_s = dscr("cT", [1024, TM], BF16)
    x_tm_p = dscr("x_tm_p", [TP, 4096], BF16); b_tm_p = dscr("b_tm_p", [TP, 1024], BF16)
    yaT = dscr("yaT", [4096, TM], BF16); ybT = dscr("ybT", [2048, TM], BF16)

    V, S, T, G, SY = "vector", "scalar", "tensor", "gpsimd", "sync"

    idf, b_idf = fw.alloc([128, 128], F32, "idf")
    idb, b_idb = fw.alloc([128, 128], BF16, "idb")
    tri, b_tri = fw.alloc([128, 2, 128], F32, "tri")
    bones, b_bones = fw.alloc([128, 2, 128], F32, "bones")
    flag, b_flag = fw.alloc([128, 1], F32, "flag")
    cw, b_cw = fw.alloc([128, 48, 4], F32, "cw")
    cb, b_cb = fw.alloc([128, 48], F32, "cb")
    scw, b_scw = fw.alloc([128, 16, 3], F32, "scw")
    dtraw, b_dtraw = fw.alloc([128, NTM, 64], F32, "dtraw")
    dtraw_p, b_dtraw_p = fw.alloc([128, NTP, 64], F32, "dtraw_p")
    histx, b_histx = fw.alloc([128, 48, 3], F32, "histx")
    histsc, b_histsc = fw.alloc([128, 32, 3], F32, "histsc")
    xtail, b_xtail = fw.alloc([128, 16, 3], BF16, "xtail")
    dtb, b_dtb = fw.alloc([128, 64], F32, "dtb")
    Abc, b_Abc = fw.alloc([128, 64], F32, "Abc")
    ones64, b_ones64 = fw.alloc([64, 128], F32, "ones64")
    for (ap, b, d) in ((idf, b_idf, ident_d), (flag, b_flag, flag_d), (cw, b_cw, cw_d), (cb, b_cb, cb_d),
                       (scw, b_scw, scw_d), (dtb, b_dtb, dtb_d), (Abc, b_Abc, alog_d)):
        fw.dma(SY, ap, d, dst=b)
    fw.dma(SY, tri, tri_d.rearrange("a p q -> p a q"), dst=b_tri)
    fw.dma(SY, bones, bones_d.rearrange("a p q -> p a q"), dst=b_bones)
    fw.op(V, lambda e: e.tensor_copy(out=idb, in_=idf), reads=[b_idf], writes=[b_idb])
    fw.op(S, lambda e: e.activation(out=Abc, in_=Abc, func=AF.Exp), reads=[b_Abc], writes=[b_Abc])
    fw.op(V, lambda e: e.tensor_scalar(out=Abc, in0=Abc, scalar1=-1.0, scalar2=None, op0=ALU.mult), reads=[b_Abc], writes=[b_Abc])
    fw.op(V, lambda e: e.memset(ones64, 1.0), writes=[b_ones64])

    PS = []
    for i in range(4):
        t = es.enter_context(nc.psum_tensor(f"ps{i}", [128, 1024], F32))
        PS.append((t, fw.buf(f"ps{i}a"), fw.buf(f"ps{i}b")))

    def bank(i):
        t, ba, bb = PS[i // 2]
        return (t[:, 0:512], ba) if i % 2 == 0 else (t[:, 512:1024], bb)

    def bank16(i):
        t, ba, bb = PS[i // 2]
        t16 = t.bitcast(BF16)
        return (t16[:, 0:1024], ba) if i % 2 == 0 else (t16[:, 1024:2048], bb)

    def phase_norm(x_d, ntiles, wbc, b_wbc, xnT, b_xnT, Ttot):
        fw.push()
        xr = fw.ring(2, [128, D], F32, "xr")
        xnr = fw.ring(2, [128, D], BF16, "xnr")
        sc = fw.ring(2, [128, 4], F32, "nsc")
        for i in range(ntiles):
            xt, bx = xr.next(); xn, bxn = xnr.next(); s4, bs = sc.next()
            fw.dma(SY, xt, x_d[i * 128:(i + 1) * 128, :], dst=bx)
            fw.op(V, lambda e, s4=s4: e.memset(s4[:, 0:1], 0.0), writes=[bs])
            fw.op(S, lambda e, xn=xn, xt=xt, s4=s4: e.activation(out=xn, in_=xt, func=AF.Square, accum_out=s4[:, 0:1]),
                  reads=[bx], writes=[bxn, bs])
            fw.op(V, lambda e, s4=s4: e.tensor_scalar(out=s4[:, 1:2], in0=s4[:, 0:1], scalar1=1.0 / D, scalar2=1e-6,
                                                      op0=ALU.mult, op1=ALU.add), reads=[bs], writes=[bs])
            fw.op(S, lambda e, s4=s4: e.activation(out=s4[:, 2:3], in_=s4[:, 1:2], func=AF.Sqrt), reads=[bs], writes=[bs])
            fw.op(V, lambda e, s4=s4: e.reciprocal(out=s4[:, 3:4], in_=s4[:, 2:3]), reads=[bs], writes=[bs])
            fw.op(V, lambda e, xn=xn, xt=xt, s4=s4: e.scalar_tensor_tensor(out=xn, in0=xt, scalar=s4[:, 3:4], in1=wbc,
                                                                         op0=ALU.mult, op1=ALU.mult),
                  reads=[bx, bs, b_wbc], writes=[bxn])
            for half in range(2):
                pt, bp = bank16(2 * (i % 2) + half)
                for k in range(8):
                    kk = half * 8 + k
                    fw.op(T, lambda e, pt=pt, xn=xn, k=k, kk=kk: e.transpose(out=pt[:, k * 128:(k + 1) * 128],
                                                                           in_=xn[:, kk * 128:(kk + 1) * 128], identity=idb),
                          reads=[bxn, b_idb], writes=[bp], signal=(k == 7))
                dst = xnT[:, half * 8:(half + 1) * 8, i * 128:(i + 1) * 128]
                src = pt.rearrange("p (a b) -> p a b", a=8)
                if half == 0:
                    fw.op(S, lambda e, dst=dst, src=src: e.copy(out=dst, in_=src), reads=[bp], writes=[b_xnT])
                else:
                    fw.op(V, lambda e, dst=dst, src=src: e.tensor_copy(out=dst, in_=src), reads=[bp], writes=[b_xnT])
        fw.pop()

    def proj_fm(w_ap, nk, c0, ncols, xT, b_xT, tblocks, evac, wr, extra=None):
        pi = 0
        for cb0 in range(c0, c0 + ncols, 512):
            wt, bw = wr.next()
            fw.dma(G, wt[:, 0:nk, :], w_ap.rearrange("(k p) c -> p k c", p=128)[:, :, cb0:cb0 + 512], dst=bw)
            for j in range(4):
                ch = (cb0 - c0) // 128 + j
                for (t0, tn) in tblocks:
                    pt, bp = bank(pi % 8); pi += 1
                    for k in range(nk):
                        fw.op(T, lambda e, pt=pt, wt=wt, k=k, j=j, t0=t0, tn=tn: e.matmul(
                            pt[:, 0:tn], lhsT=wt[:, k, j * 128:(j + 1) * 128], rhs=xT[:, k, t0:t0 + tn],
                            start=(k == 0), stop=(k == nk - 1)), reads=[bw, b_xT], writes=[bp], signal=(k == nk - 1))
                    evac(ch, t0, tn, pt, bp)
                if extra is not None:
                    extra(ch, wt, bw, j)

    def proj_tm(w_ap, nk, c0, ncols, xT, b_xT, ntiles, evac, wr):
        wt, bw = wr.next()
        fw.dma(G, wt[:, 0:nk, 0:ncols], w_ap.rearrange("(k p) c -> p k c", p=128)[:, :, c0:c0 + ncols], dst=bw)
        for i in range(ntiles):
            pt, bp = bank(i % 8)
            for k in range(nk):
                fw.op(T, lambda e, pt=pt, wt=wt, k=k, i=i: e.matmul(
                    pt[:, 0:ncols], lhsT=xT[:, k, i * 128:(i + 1) * 128], rhs=wt[:, k, 0:ncols],
                    start=(k == 0), stop=(k == nk - 1)), reads=[bw, b_xT], writes=[bp], signal=(k == nk - 1))
            evac(i, pt, bp)

    fw.push()
    wr = fw.ring(2, [128, 16, 512], BF16, "wr")
    nbc, b_nbc = fw.alloc([128, D], F32, "nbc")
    fw.dma(SY, nbc, nm_d, dst=b_nbc)

    def make_store_evac(scr, Ttot, dt_st, func=None):
        stg = fw.ring(3, [128, Ttot], dt_st, "stg")
        cur = {}

        def evac(ch, t0, tn, pt, bp):
            if t0 == 0:
                cur["s"] = stg.next()
            st, bs = cur["s"]
            if func is not None:
                fw.op(S, lambda e: e.activation(out=st[:, t0:t0 + tn], in_=pt[:, 0:tn], func=func), reads=[bp], writes=[bs])
            elif (ch + t0 // 128) % 2 == 0:
                fw.op(V, lambda e: e.tensor_copy(out=st[:, t0:t0 + tn], in_=pt[:, 0:tn]), reads=[bp], writes=[bs])
            else:
                fw.op(S, lambda e: e.copy(out=st[:, t0:t0 + tn], in_=pt[:, 0:tn]), reads=[bp], writes=[bs])
            if t0 + tn == Ttot:
                fw.dma(SY, scr[ch * 128:(ch + 1) * 128, :], st, src=bs)
        return evac

    if stage >= 1:
        fw.push()
        xnTp, b_xnTp = fw.alloc([128, 16, TP], BF16, "xnTp")
        phase_norm(xp_d, NTP, nbc, b_nbc, xnTp, b_xnTp, TP)
        fw.op(V, lambda e: e.tensor_copy(out=xtail, in_=xnTp[:, :, TP - 3:TP]), reads=[b_xnTp], writes=[b_xtail])
        fw.push()
        ev = make_store_evac(xbcT_p, TP, F32)
        proj_fm(w_in_d, 16, C_X, 5120, xnTp, b_xnTp, [(0, 512), (512, 512)], ev, wr)

        def ev_dt_p(i, pt, bp):
            fw.op(V, lambda e: e.tensor_copy(out=dtraw_p[:, i, :], in_=pt[:, 0:64]), reads=[bp], writes=[b_dtraw_p])
        proj_tm(w_in_d, 16, C_DT, 64, xnTp, b_xnTp, NTP, ev_dt_p, wr)
        fw.pop()
        fw.pop()
        fw.barrier()

    def phase_conv(src_scr, nchunks, main):
        fw.push()
        L = 3 + 1024 + (176 if main else 0)
        upr = fw.ring(4, [128, L], F32, "up")
        accr = fw.ring(3, [128, TM if main else TP], F32, "acc")
        xcr = fw.ring(2, [128, TM if main else TP], BF16, "xc")
        ttr = fw.ring(2, [128, NTM, 128], BF16, "tt")
        segb = {}
        if main:
            t32r = fw.ring(2, [128, NTM, 128], F32, "t32")
            stc, b_stc = fw.alloc([48, 6144], F32, "stc")
            fw.dma(SY, stc, st_conv_d, dst=b_stc)
            cvo, b_cvo = fw.alloc([51, 6144], F32, "cvo")
            cst_r = fw.ring(2, [128, 51], F32, "cst")
        ntl = NTM if main else NTP
        xdst = x_tm if main else x_tm_p
        bdst = b_tm if main else b_tm_p
        for c in range(nchunks):
            up, bu = upr.next(); acc, ba = accr.next(); xc, bxc = xcr.next()
            fw.dma(SY, up[:, 3:1027], src_scr[c * 128:(c + 1) * 128, 0:1024], dst=bu)
            if main:
                upS = up[:, 1027:1203].rearrange("p (j t) -> p j t", t=11)
                fw.dma(SY, upS[:, :, 3:11], src_scr[c * 128:(c + 1) * 128, 1024:1152].rearrange("p (j t) -> p j t", t=8), dst=bu)
                fw.op(V, lambda e, up=up, c=c: e.tensor_scalar(out=up[:, 0:3], in0=histx[:, c, :], scalar1=flag[:, 0:1],
                                                              scalar2=None, op0=ALU.mult), reads=[b_histx, b_flag], writes=[bu])
                pt, bp = bank(0)
                fw.op(T, lambda e, pt=pt, c=c: e.transpose(out=pt[:, 0:48], in_=stc[0:48, c * 128:(c + 1) * 128], identity=idf[0:48, 0:48]),
                      reads=[b_stc, b_idf], writes=[bp])
                fw.op(V, lambda e, pt=pt, upS=upS: e.tensor_copy(out=upS[:, :, 0:3], in_=pt[:, 0:48].rearrange("p (j t) -> p j t", t=3)),
                      reads=[bp], writes=[bu])
            else:
                fw.op(V, lambda e, up=up: e.memset(up[:, 0:3], 0.0), writes=[bu])
            if id(ba) not in segb:
                segb[id(ba)] = [fw.buf(f"accseg{q}") for q in range(3)]
            sb3 = segb[id(ba)]
            segs = [(up[:, 0:515], acc[:, 0:512], lambda a, k: a[:, k:k + 512], sb3[0]),
                    (up[:, 512:1027], acc[:, 512:1024], lambda a, k: a[:, k:k + 512], sb3[1])]
            if main:
                segs.append((upS, acc[:, 1024:1152].rearrange("p (j t) -> p j t", t=8), lambda a, k: a[:, :, k:k + 8], sb3[2]))
            for k in range(4):
                for (src, dst, sl, bseg) in segs:
                    if k == 0:
                        fw.op(V, lambda e, src=src, dst=dst, sl=sl, c=c: e.tensor_scalar(
                            out=dst, in0=sl(src, 0), scalar1=cw[:, c, 0:1], scalar2=cb[:, c:c + 1], op0=ALU.mult, op1=ALU.add),
                            reads=[bu, b_cw, b_cb], writes=[bseg])
                    else:
                        fw.op(V, lambda e, src=src, dst=dst, sl=sl, c=c, k=k: e.scalar_tensor_tensor(
                            out=dst, in0=sl(src, k), scalar=cw[:, c, k:k + 1], in1=dst, op0=ALU.mult, op1=ALU.add),
                            reads=[bu, b_cw, bseg], writes=[bseg])
            ba_all = sb3[0:len(segs)]
            fw.op(S, lambda e, acc=acc: e.activation(out=acc, in_=acc, func=AF.Silu), reads=ba_all, writes=ba_all)
            fw.op(V, lambda e, xc=xc, acc=acc: e.tensor_copy(out=xc, in_=acc), reads=ba_all, writes=[bxc])
            if main and c < 32:
                t32, bt32 = t32r.next()
                for (b0, i0, i1) in ((4, 0, 4), (5, 4, 8), (6, 8, 9)):
                    pt, bp = bank(b0)
                    for i in range(i0, i1):
                        fw.op(T, lambda e, pt=pt, acc=acc, i=i, i0=i0: e.transpose(out=pt[:, (i - i0) * 128:(i - i0 + 1) * 128], in_=acc[:, i * 128:(i + 1) * 128], identity=idf),
                              reads=ba_all + [b_idf], writes=[bp], signal=(i == i1 - 1))
                    fw.op(V, lambda e, pt=pt, t32=t32, i0=i0, i1=i1: e.tensor_copy(out=t32[:, i0:i1, :], in_=pt[:, 0:(i1 - i0) * 128].rearrange("p (a b) -> p a b", b=128)),
                          reads=[bp], writes=[bt32])
                fw.dma(G, x32_tm.rearrange("(i p) c -> p i c", p=128)[:, :, c * 128:(c + 1) * 128], t32, src=bt32)
            if main:
                cst, bcs = cst_r.next()
                fw.op(V, lambda e, cst=cst, up=up: e.tensor_copy(out=cst[:, 0:3], in_=up[:, 1024:1027]), reads=[bu], writes=[bcs])
                fw.op(V, lambda e, cst=cst, upS=upS: e.tensor_copy(out=cst[:, 3:51].rearrange("p (j t) -> p j t", t=3), in_=upS[:, :, 8:11]),
                      reads=[bu], writes=[bcs])
                pt, bp = bank(1)
                fw.op(T, lambda e, pt=pt, cst=cst: e.transpose(out=pt[0:51, 0:128], in_=cst, identity=idf), reads=[bcs, b_idf], writes=[bp])
                fw.op(S, lambda e, pt=pt, c=c: e.copy(out=cvo[0:51, c * 128:(c + 1) * 128], in_=pt[0:51, 0:128]), reads=[bp], writes=[b_cvo])
            isx = c < 32
            isb = 32 <= c < 40
            if main and not isx:
                dsc = bT_s if isb else cT_s
                cc = c - 32 if isb else c - 40
                fw.dma(G, dsc[cc * 128:(cc + 1) * 128, :], xc, src=bxc)
            if isx or isb:
                tt, btt = ttr.next()
                for h in range(2):
                    n0 = h * 8
                    n1 = min(ntl, n0 + 8)
                    if n1 <= n0:
                        continue
                    pt, bp = bank16(2 + h)
                    for i in range(n0, n1):
                        fw.op(T, lambda e, pt=pt, xc=xc, i=i, n0=n0: e.transpose(out=pt[:, (i - n0) * 128:(i - n0 + 1) * 128],
                                                                              in_=xc[:, i * 128:(i + 1) * 128], identity=idb),
                              reads=[bxc, b_idb], writes=[bp], signal=(i == n1 - 1))
                    fw.op(S if h == 0 else V, (lambda e, pt=pt, tt=tt, n0=n0, n1=n1: (e.copy if False else e.tensor_copy)(
                        out=tt[:, n0:n1, :], in_=pt[:, 0:(n1 - n0) * 128].rearrange("p (a b) -> p a b", b=128))) if h == 1 else
                        (lambda e, pt=pt, tt=tt, n0=n0, n1=n1: e.copy(out=tt[:, n0:n1, :], in_=pt[:, 0:(n1 - n0) * 128].rearrange("p (a b) -> p a b", b=128))),
                        reads=[bp], writes=[btt])
                if isx:
                    fw.dma(G, xdst.rearrange("(i p) c -> p i c", p=128)[:, :, c * 128:(c + 1) * 128], tt[:, 0:ntl, :], src=btt)
                else:
                    cc = c - 32
                    fw.dma(G, bdst.rearrange("(i p) c -> p i c", p=128)[:, :, cc * 128:(cc + 1) * 128], tt[:, 0:ntl, :], src=btt)
        if main:
            fw.dma(G, o_conv_p, cvo[0:3, :], src=b_cvo)
            fw.dma(G, o_conv_s, cvo[3:51, :], src=b_cvo)
        fw.pop()
        fw.barrier()

    TB = [(0, 384), (384, 384), (768, 384)]
    if stage >= 3:
        xnT, b_xnT = fw.alloc([128, 16, TM], BF16, "xnT")
        phase_norm(xm_d, NTM, nbc, b_nbc, xnT, b_xnT, TM)

        def make_extra(hist, b_hist, ch_off):
            def extra(ch, wt, bw, j):
                pt, bp = bank(7)
                for k in range(16):
                    fw.op(T, lambda e, pt=pt, wt=wt, k=k, j=j: e.matmul(pt[:, 0:3], lhsT=wt[:, k, j * 128:(j + 1) * 128],
                                                                       rhs=xtail[:, k, 0:3], start=(k == 0), stop=(k == 15)),
                          reads=[bw, b_xtail], writes=[bp], signal=(k == 15))
                fw.op(V, lambda e, pt=pt, ch=ch: e.tensor_copy(out=hist[:, ch + ch_off, :], in_=pt[:, 0:3]), reads=[bp], writes=[b_hist])
            return extra

        fw.push()
        ev = make_store_evac(xbcT, TM, F32)
        proj_fm(w_in_d, 16, C_X, 6144, xnT, b_xnT, TB, ev, wr, extra=make_extra(histx, b_histx, 0))
        fw.pop()
        fw.push()
        ev = make_store_evac(scT, TM, F32)
        proj_fm(w_in_d, 16, C_SB, 2048, xnT, b_xnT, TB, ev, wr)
        ev2 = lambda ch, t0, tn, pt, bp: ev(ch + 16, t0, tn, pt, bp)
        proj_fm(w_in_d, 16, C_SC, 4096, xnT, b_xnT, TB, ev2, wr, extra=make_extra(histsc, b_histsc, 0))
        fw.pop()
        fw.push()
        ev = make_store_evac(gateT, TM, F32, func=AF.Sigmoid)
        proj_fm(w_in_d, 16, C_GA, 4096, xnT, b_xnT, TB, ev, wr)
        fw.pop()
        fw.push()
        zst = fw.ring(3, [128, 512], F32, "zst")
        for blk in range(8):
            def ev_z(i, pt, bp, blk=blk):
                st, bs = zst.next()
                fw.op(S, lambda e: e.activation(out=st, in_=pt[:, 0:512], func=AF.Silu), reads=[bp], writes=[bs])
                fw.dma(SY, zs_s[i * 128:(i + 1) * 128, blk * 512:(blk + 1) * 512], st, src=bs)
            proj_tm(w_in_d, 16, C_Z + blk * 512, 512, xnT, b_xnT, NTM, ev_z, wr)

        def ev_dt(i, pt, bp):
            fw.op(V, lambda e: e.tensor_copy(out=dtraw[:, i, :], in_=pt[:, 0:64]), reads=[bp], writes=[b_dtraw])
        proj_tm(w_in_d, 16, C_DT, 64, xnT, b_xnT, NTM, ev_dt, wr)
        fw.pop()
        fw.barrier()

    fw.pop()
    if stage >= 2:
        phase_conv(xbcT_p, 40, False)
    if stage >= 3:
        phase_conv(xbcT, 48, True)

    if stage >= 4:
        fw.push()
        L2 = 2 + 1024 + 160
        sts, b_sts = fw.alloc([32, 2048], F32, "sts")
        fw.dma(SY, sts, st_sc_d, dst=b_sts)
        sco, b_sco = fw.alloc([34, 2048], F32, "sco")
        inr = fw.ring(3, [128, 3, TM], F32, "scin")
        upr = fw.ring(3, [128, L2], F32, "up2")
        vr = fw.ring(2, [128, TM], F32, "scv")
        ybr = fw.ring(2, [128, TM], BF16, "yb")
        cs2r = fw.ring(2, [128, 34], F32, "cs2")
        segb2 = {}
        for c in range(16):
            it, bi = inr.next(); up, bu = upr.next(); v, bv = vr.next(); yb, byb = ybr.next(); cs2, bc2 = cs2r.next()
            for q in range(3):
                fw.dma(SY, it[:, q, :], scT[q * 2048 + c * 128:q * 2048 + (c + 1) * 128, :], dst=bi)
            upS = up[:, 1026:1186].rearrange("p (j t) -> p j t", t=10)
            fw.op(V, lambda e, up=up, it=it: e.tensor_tensor(out=up[:, 2:1026], in0=it[:, 1, 0:1024], in1=it[:, 2, 0:1024], op=ALU.mult),
                  reads=[bi], writes=[bu])
            fw.op(V, lambda e, upS=upS, it=it: e.tensor_tensor(out=upS[:, :, 2:10], in0=it[:, 1, 1024:1152].rearrange("p (j t) -> p j t", t=8),
                                                             in1=it[:, 2, 1024:1152].rearrange("p (j t) -> p j t", t=8), op=ALU.mult),
                  reads=[bi], writes=[bu])
            fw.op(V, lambda e, up=up, c=c: e.scalar_tensor_tensor(out=up[:, 0:2], in0=histsc[:, c, 1:3], scalar=flag[:, 0:1],
                                                                 in1=histsc[:, 16 + c, 1:3], op0=ALU.mult, op1=ALU.mult),
                  reads=[b_histsc, b_flag], writes=[bu])
            pt, bp = bank(c % 2)
            fw.op(T, lambda e, pt=pt, c=c: e.transpose(out=pt[:, 0:32], in_=sts[0:32, c * 128:(c + 1) * 128], identity=idf[0:32, 0:32]),
                  reads=[b_sts, b_idf], writes=[bp])
            fw.op(V, lambda e, pt=pt, upS=upS: e.tensor_copy(out=upS[:, :, 0:2], in_=pt[:, 0:32].rearrange("p (j t) -> p j t", t=2)),
                  reads=[bp], writes=[bu])
            if id(bv) not in segb2:
                segb2[id(bv)] = [fw.buf(f"vseg{q}") for q in range(3)]
            sb3 = segb2[id(bv)]
            segs = [(up[:, 0:514], v[:, 0:512], lambda a, k: a[:, k:k + 512], it[:, 0, 0:512], yb[:, 0:512], sb3[0]),
                    (up[:, 512:1026], v[:, 512:1024], lambda a, k: a[:, k:k + 512], it[:, 0, 512:1024], yb[:, 512:1024], sb3[1]),
                    (upS, v[:, 1024:1152].rearrange("p (j t) -> p j t", t=8), lambda a, k: a[:, :, k:k + 8],
                     it[:, 0, 1024:1152].rearrange("p (j t) -> p j t", t=8), yb[:, 1024:1152].rearrange("p (j t) -> p j t", t=8), sb3[2])]
            for k in range(3):
                for (src, dst, sl, bsrc, ydst, bseg) in segs:
                    if k == 0:
                        fw.op(V, lambda e, src=src, dst=dst, sl=sl, c=c: e.tensor_scalar(out=dst, in0=sl(src, 0), scalar1=scw[:, c, 0:1],
                                                                                      scalar2=None, op0=ALU.mult), reads=[bu, b_scw], writes=[bseg])
                    else:
                        fw.op(V, lambda e, src=src, dst=dst, sl=sl, c=c, k=k: e.scalar_tensor_tensor(
                            out=dst, in0=sl(src, k), scalar=scw[:, c, k:k + 1], in1=dst, op0=ALU.mult, op1=ALU.add),
                            reads=[bu, b_scw, bseg], writes=[bseg])
            for (src, dst, sl, bsrc, ydst, bseg) in segs:
                fw.op(V, lambda e, dst=dst, bsrc=bsrc, ydst=ydst: e.tensor_tensor(out=ydst, in0=dst, in1=bsrc, op=ALU.mult),
                      reads=[bseg, bi], writes=[byb])
            fw.dma(G, ybT[c * 128:(c + 1) * 128, :], yb, src=byb)
            fw.op(V, lambda e, cs2=cs2, up=up: e.tensor_copy(out=cs2[:, 0:2], in_=up[:, 1024:1026]), reads=[bu], writes=[bc2])
            fw.op(V, lambda e, cs2=cs2, upS=upS: e.tensor_copy(out=cs2[:, 2:34].rearrange("p (j t) -> p j t", t=2), in_=upS[:, :, 8:10]),
                  reads=[bu], writes=[bc2])
            pt, bp = bank(2 + c % 2)
            fw.op(T, lambda e, pt=pt, cs2=cs2: e.transpose(out=pt[0:34, 0:128], in_=cs2, identity=idf), reads=[bc2, b_idf], writes=[bp])
            fw.op(S, lambda e, pt=pt, c=c: e.copy(out=sco[0:34, c * 128:(c + 1) * 128], in_=pt[0:34, 0:128]), reads=[bp], writes=[b_sco])
        fw.dma(G, o_sc_p, sco[0:2, :], src=b_sco)
        fw.dma(G, o_sc_s, sco[2:34, :], src=b_sco)
        fw.pop()
        fw.barrier()

    def v3(ap, a):
        return ap.rearrange("p (a b) -> p a b", a=a)

    if stage >= 5:
        fw.push()
        ssdn, b_ssdn = fw.alloc([128, 4096], F32, "ssdn")
        fw.dma(SY, ssdn, ssdn_d, dst=b_ssdn)
        x32r = fw.ring(2, [128, 512], F32, "x32g")
        dskr = fw.ring(2, [128, 512], F32, "dskg")
        ztr = fw.ring(2, [128, 512], F32, "ztg")
        smr = fw.ring(2, [128, 12, 64], F32, "sm")
        csTr = fw.ring(2, [64, 128], F32, "csT")
        R4r = fw.ring(2, [64, 512], F32, "R4")
        t1r = fw.ring(8, [128, 128], F32, "t1")
        Er = fw.ring(8, [128, 128], F32, "E")
        LTr = fw.ring(8, [128, 128], BF16, "LT")
        CBr = fw.ring(2, [128, 128], F32, "CBm")
        ygr = fw.ring(2, [128, 512], F32, "yg")
        jkr = fw.ring(2, [128, 512], BF16, "jk")
        yar = fw.ring(2, [128, 512], BF16, "ya")
        ystr = fw.ring(2, [128, 4, 128], BF16, "yst")
        nscr = fw.ring(2, [128, 4], F32, "nsc2")

        def ssd_tile(i, kind, bufs, HT=None, HTb=None, bHT=None, bHTb=None, samp=None):
            pre = kind == "pre"
            do_y = not pre
            m = 1 if kind == "samp" else 0
            triM = tri[:, m, :]
            bonesM = bones[:, m, :]
            xsrc, bsrc = (x_tm_p, b_tm_p) if pre else (x_tm, b_tm)
            dtr, b_dtr = (dtraw_p, b_dtraw_p) if pre else (dtraw, b_dtraw)
            (xt, bxt), (bt, bbt), (bTt, bbT), (cTt, bcT), (xw, bxw) = bufs
            fw.dma(SY, xt, xsrc[i * 128:(i + 1) * 128, :], dst=bxt)
            fw.dma(SY, bt, bsrc[i * 128:(i + 1) * 128, :], dst=bbt)
            if do_y:
                fw.dma(SY, bTt, bT_s.rearrange("(g n) t -> n g t", n=128)[:, :, i * 128:(i + 1) * 128], dst=bbT)
                fw.dma(SY, cTt, cT_s.rearrange("(g n) t -> n g t", n=128)[:, :, i * 128:(i + 1) * 128], dst=bcT)
            sm, bsm = smr.next()
            v_, ab, l_, dt, a_, negcs, ecs, d1, wdt, dec = [sm[:, q, :] for q in range(10)]
            fw.op(V, lambda e: e.tensor_tensor(out=v_, in0=dtr[:, i, :], in1=dtb, op=ALU.add), reads=[b_dtr, b_dtb], writes=[bsm])
            fw.op(S, lambda e: e.activation(out=ab, in_=v_, func=AF.Abs), reads=[bsm], writes=[bsm])
            fw.op(S, lambda e: e.activation(out=ab, in_=ab, func=AF.Exp, scale=-1.0), reads=[bsm], writes=[bsm])
            fw.op(V, lambda e: e.tensor_scalar(out=ab, in0=ab, scalar1=1.0, scalar2=None, op0=ALU.add), reads=[bsm], writes=[bsm])
            fw.op(S, lambda e: e.activation(out=l_, in_=ab, func=AF.Ln), reads=[bsm], writes=[bsm])
            fw.op(V, lambda e: e.scalar_tensor_tensor(out=dt, in0=v_, scalar=0.0, in1=l_, op0=ALU.max, op1=ALU.add), reads=[bsm], writes=[bsm])
            fw.op(V, lambda e: e.tensor_tensor(out=a_, in0=dt, in1=Abc, op=ALU.mult), reads=[bsm, b_Abc], writes=[bsm])
            pt0, bp0 = bank(0)
            fw.op(T, lambda e: e.matmul(pt0[:, 0:64], lhsT=triM, rhs=a_, start=True, stop=True), reads=[b_tri, bsm], writes=[bp0], signal=False)
            fw.op(T, lambda e: e.matmul(pt0[:, 64:128], lhsT=bonesM, rhs=a_, start=True, stop=True), reads=[b_bones, bsm], writes=[bp0], signal=False)
            fw.op(T, lambda e: e.matmul(pt0[0:64, 128:256], lhsT=a_, rhs=triM, start=True, stop=True), reads=[b_tri, bsm], writes=[bp0])
            fw.op(V, lambda e: e.tensor_scalar(out=negcs, in0=pt0[:, 0:64], scalar1=-1.0, scalar2=None, op0=ALU.mult), reads=[bp0], writes=[bsm])
            fw.op(S, lambda e: e.activation(out=ecs, in_=pt0[:, 0:64], func=AF.Exp), reads=[bp0], writes=[bsm])
            fw.op(V, lambda e: e.tensor_tensor(out=d1, in0=pt0[:, 64:128], in1=negcs, op=ALU.add), reads=[bp0, bsm], writes=[bsm])
            fw.op(S, lambda e: e.activation(out=d1, in_=d1, func=AF.Exp), reads=[bsm], writes=[bsm])
            fw.op(V, lambda e: e.tensor_tensor(out=wdt, in0=d1, in1=dt, op=ALU.mult), reads=[bsm], writes=[bsm])
            fw.op(S, lambda e: e.activation(out=dec, in_=pt0[:, 64:128], func=AF.Exp), reads=[bp0], writes=[bsm])
            csT, bcsT = csTr.next()
            fw.op(S, lambda e: e.copy(out=csT, in_=pt0[0:64, 128:256]), reads=[bp0], writes=[bcsT])
            fw.op(V, lambda e: e.tensor_tensor(out=v3(xw, 64), in0=v3(xt, 64), in1=wdt.unsqueeze(2).broadcast_to([128, 64, 64]), op=ALU.mult),
                  reads=[bxt, bsm], writes=[bxw])
            if samp is not None:
                samp(i, a_, bsm, cTt, bcT, bt, bbt, xw, bxw)
            for g in range(8):
                if do_y:
                    ptb, bpb = bank(1)
                    fw.op(T, lambda e, g=g: e.matmul(ptb[:, 0:128], lhsT=bTt[:, g, :], rhs=cTt[:, g, :], start=True, stop=True),
                          reads=[bbT, bcT], writes=[bpb])
                    CBm, bCB = CBr.next()
                    fw.op(V, lambda e, CBm=CBm: e.tensor_tensor(out=CBm, in0=ptb[:, 0:128], in1=triM, op=ALU.mult), reads=[bpb, b_tri], writes=[bCB])
                    pty, bpy = bank(4)
                    hd = []
                    for half in range(2):
                        h0 = 8 * g + 4 * half
                        R4, bR4 = R4r.next()
                        fw.op(V, lambda e, R4=R4, h0=h0: e.tensor_tensor(out=v3(R4, 4), in0=idf[0:64, h0:h0 + 4].unsqueeze(2).broadcast_to([64, 4, 128]),
                                                                        in1=csT.unsqueeze(1).broadcast_to([64, 4, 128]), op=ALU.mult),
                              reads=[b_idf, bcsT], writes=[bR4])
                        ptc, bpc = bank(2 + half)
                        fw.op(T, lambda e, R4=R4, ptc=ptc: e.matmul(ptc[:, 0:512], lhsT=ones64, rhs=R4, start=True, stop=True),
                              reads=[b_ones64, bR4], writes=[bpc])
                        for hl in range(4):
                            h = h0 + hl
                            hg = 4 * half + hl
                            t1, bt1 = t1r.next(); E_, bE = Er.next()
                            fw.op(V, lambda e, t1=t1, ptc=ptc, hl=hl, h=h: e.tensor_scalar(out=t1, in0=ptc[:, hl * 128:(hl + 1) * 128], scalar1=negcs[:, h:h + 1],
                                                                                     scalar2=0.0, op0=ALU.add, op1=ALU.min), reads=[bpc, bsm], writes=[bt1])
                            fw.op(S, lambda e, t1=t1, E_=E_: e.activation(out=E_, in_=t1, func=AF.Exp), reads=[bt1], writes=[bE])
                            hd.append((h, hg, E_, bE))
                    for (h, hg, E_, bE) in hd:
                        LT, bLT = LTr.next()
                        fw.op(V, lambda e, E_=E_, LT=LT, CBm=CBm, h=h: e.scalar_tensor_tensor(out=LT, in0=E_, scalar=dt[:, h:h + 1], in1=CBm,
                                                                                       op0=ALU.mult, op1=ALU.mult), reads=[bE, bsm, bCB], writes=[bLT])
                        fw.op(T, lambda e, LT=LT, hg=hg, h=h: e.matmul(pty[:, hg * 64:(hg + 1) * 64], lhsT=LT, rhs=xt[:, h * 64:(h + 1) * 64], start=True, stop=True),
                              reads=[bLT, bxt], writes=[bpy])
                    pto, bpo = bank(5)
                    if kind == "main":
                        fw.op(T, lambda e, g=g: e.matmul(pto[:, 0:512], lhsT=cTt[:, g, :], rhs=HTb[:, g * 512:(g + 1) * 512], start=True, stop=True),
                              reads=[bcT, bHTb[g]], writes=[bpo])
                    else:
                        yoffT, byo = samp.yoffT
                        for u in range(4):
                            fw.op(T, lambda e, u=u, g=g: e.transpose(out=pto[:, u * 128:(u + 1) * 128], in_=yoffT[:, 4 * g + u, :], identity=idf),
                                  reads=[byo, b_idf], writes=[bpo], signal=(u == 3))
                    yg, byg = ygr.next()
                    fw.op(V, lambda e, yg=yg, g=g: e.tensor_tensor(out=v3(yg, 8), in0=v3(pto[:, 0:512], 8),
                                                                  in1=ecs[:, 8 * g:8 * g + 8].unsqueeze(2).broadcast_to([128, 8, 64]), op=ALU.mult),
                          reads=[bpo, bsm], writes=[byg])
                    fw.op(V, lambda e, yg=yg: e.tensor_tensor(out=yg, in0=yg, in1=pty[:, 0:512], op=ALU.add), reads=[byg, bpy], writes=[byg])
                    x32g, bx32 = x32r.next(); dskg, bdsk = dskr.next(); ztg, bztg = ztr.next()
                    fw.dma(SY, x32g, x32_tm[i * 128:(i + 1) * 128, g * 512:(g + 1) * 512], dst=bx32)
                    fw.dma(SY, dskg, dsk_d[:, g * 512:(g + 1) * 512], dst=bdsk)
                    fw.dma(SY, ztg, zs_s[i * 128:(i + 1) * 128, g * 512:(g + 1) * 512], dst=bztg)
                    fw.op(V, lambda e, x32g=x32g, dskg=dskg: e.tensor_tensor(out=x32g, in0=x32g, in1=dskg, op=ALU.mult), reads=[bx32, bdsk], writes=[bx32])
                    fw.op(V, lambda e, yg=yg, x32g=x32g: e.tensor_tensor(out=yg, in0=yg, in1=x32g, op=ALU.add), reads=[byg, bx32], writes=[byg])
                    fw.op(V, lambda e, yg=yg, ztg=ztg: e.tensor_tensor(out=yg, in0=yg, in1=ztg, op=ALU.mult), reads=[byg, bztg], writes=[byg])
                    ns, bns = nscr.next(); jk, bjk = jkr.next(); ya, bya = yar.next(); yst, byst = ystr.next()
                    fw.op(V, lambda e, ns=ns: e.memset(ns[:, 0:1], 0.0), writes=[bns])
                    fw.op(S, lambda e, jk=jk, yg=yg, ns=ns: e.activation(out=jk, in_=yg, func=AF.Square, accum_out=ns[:, 0:1]), reads=[byg], writes=[bjk, bns])
                    fw.op(V, lambda e, ns=ns: e.tensor_scalar(out=ns[:, 1:2], in0=ns[:, 0:1], scalar1=1.0 / 512, scalar2=1e-5, op0=ALU.mult, op1=ALU.add),
                          reads=[bns], writes=[bns])
                    fw.op(S, lambda e, ns=ns: e.activation(out=ns[:, 2:3], in_=ns[:, 1:2], func=AF.Sqrt), reads=[bns], writes=[bns])
                    fw.op(V, lambda e, ns=ns: e.reciprocal(out=ns[:, 3:4], in_=ns[:, 2:3]), reads=[bns], writes=[bns])
                    fw.op(V, lambda e, ns=ns, ya=ya, yg=yg, g=g: e.scalar_tensor_tensor(out=ya, in0=yg, scalar=ns[:, 3:4], in1=ssdn[:, g * 512:(g + 1) * 512],
                                                                                 op0=ALU.mult, op1=ALU.mult), reads=[byg, bns, b_ssdn], writes=[bya])
                    pt7, bp7 = bank16(7)
                    for u in range(4):
                        fw.op(T, lambda e, u=u, ya=ya: e.transpose(out=pt7[:, u * 128:(u + 1) * 128], in_=ya[:, u * 128:(u + 1) * 128], identity=idb),
                              reads=[bya, b_idb], writes=[bp7], signal=(u == 3))
                    fw.op(S, lambda e, yst=yst: e.copy(out=yst, in_=v3(pt7[:, 0:512], 4)), reads=[bp7], writes=[byst])
                    fw.dma(G, yaT[g * 512:(g + 1) * 512, i * 128:(i + 1) * 128].rearrange("(j p) t -> p j t", p=128), yst, src=byst)
                if kind != "samp":
                    pts, bps = bank(6)
                    fw.op(T, lambda e, g=g: e.matmul(pts[:, 0:512], lhsT=bt[:, g * 128:(g + 1) * 128], rhs=xw[:, g * 512:(g + 1) * 512], start=True, stop=True),
                          reads=[bbt, bxw], writes=[bps])
                    HTg = HT[:, g * 512:(g + 1) * 512]
                    fw.op(V, lambda e, HTg=HTg, g=g: e.tensor_tensor(out=v3(HTg, 8), in0=v3(HTg, 8),
                                                                    in1=dec[:, 8 * g:8 * g + 8].unsqueeze(2).broadcast_to([128, 8, 64]), op=ALU.mult),
                          reads=[bHT[g], bsm], writes=[bHT[g]])
                    fw.op(V, lambda e, HTg=HTg: e.tensor_tensor(out=HTg, in0=HTg, in1=pts[:, 0:512], op=ALU.add), reads=[bHT[g], bps], writes=[bHT[g]])
                    fw.op(S, lambda e, HTg=HTg, g=g: e.copy(out=HTb[:, g * 512:(g + 1) * 512], in_=HTg), reads=[bHT[g]], writes=[bHTb[g]])

        fw.push()
        HT, _ = fw.alloc([128, 4096], F32, "HT")
        HTb, _ = fw.alloc([128, 4096], BF16, "HTb")
        bHT = [fw.buf(f"HT{g}") for g in range(8)]
        bHTb = [fw.buf(f"HTb{g}") for g in range(8)]
        ldr = [fw.ring(2, sh, BF16, nm) for (sh, nm) in (([128, 4096], "xt"), ([128, 1024], "bt"), ([128, 8, 128], "bTt"), ([128, 8, 128], "cTt"))]
        xw1 = fw.alloc([128, 4096], BF16, "xw")
        hout, b_hout = fw.alloc([128, 32, 128], F32, "houtp")
        for g in range(8):
            fw.op(V, lambda e, g=g: e.memset(HT[:, g * 512:(g + 1) * 512], 0.0), writes=[bHT[g]])
            fw.op(V, lambda e, g=g: e.memset(HTb[:, g * 512:(g + 1) * 512], 0.0), writes=[bHTb[g]])
        for i in range(NTP if stage >= 5.1 else 1):
            ssd_tile(i, "pre", [r.next() for r in ldr] + [xw1], HT, HTb, bHT, bHTb)
        for g in range(8):
            HTg = HT[:, g * 512:(g + 1) * 512]
            fw.op(V, lambda e, HTg=HTg: e.tensor_scalar(out=HTg, in0=HTg, scalar1=flag[:, 0:1], scalar2=None, op0=ALU.mult), reads=[bHT[g], b_flag], writes=[bHT[g]])
            fw.op(S, lambda e, HTg=HTg, g=g: e.copy(out=HTb[:, g * 512:(g + 1) * 512], in_=HTg), reads=[bHT[g]], writes=[bHTb[g]])
        for i in range(8 if stage >= 5.3 else (1 if stage >= 5.2 else 0)):
            ssd_tile(i, "main", [r.next() for r in ldr] + [xw1], HT, HTb, bHT, bHTb)
        for q4 in range(8):
            ptx, bpx = bank(2 + q4 % 2)
            for u in range(4):
                hp = q4 * 4 + u
                fw.op(T, lambda e, ptx=ptx, u=u, hp=hp: e.transpose(out=ptx[:, u * 128:(u + 1) * 128], in_=HT[:, hp * 128:(hp + 1) * 128], identity=idf),
                      reads=[bHT[hp // 4], b_idf], writes=[bpx], signal=(u == 3))
            fw.op(S if q4 % 2 else V, (lambda e, ptx=ptx, q4=q4: e.copy(out=hout[:, q4 * 4:(q4 + 1) * 4, :], in_=v3(ptx[:, 0:512], 4))) if q4 % 2 else
                  (lambda e, ptx=ptx, q4=q4: e.tensor_copy(out=hout[:, q4 * 4:(q4 + 1) * 4, :], in_=v3(ptx[:, 0:512], 4))), reads=[bpx], writes=[b_hout])
        fw.dma(G, o_ssm_p.rearrange("h p n -> (h p) n").rearrange("(hp q) n -> q hp n", q=128), hout, src=b_hout)
        fw.pop()
        fw.barrier()

        fw.push()
        sbufs = [fw.alloc(sh, BF16, nm) for (sh, nm) in (([128, 4096], "sxt"), ([128, 1024], "sbt"), ([128, 8, 128], "sbTt"), ([128, 8, 128], "scTt"),
                                                         ([128, 4096], "sxw"))]
        onesj, b_onesj = fw.alloc([128, 16, 128], F32, "onesj")
        maskj, b_maskj = fw.alloc([128, 16], F32, "maskj")
        fw.dma(SY, onesj, onesj_d, dst=b_onesj)
        fw.dma(SY, maskj, maskj_d, dst=b_maskj)
        decs, b_decs = fw.alloc([128, 1024], F32, "decs")
        h0r = fw.ring(2, [128, 32, 128], F32, "h0f")
        h0T, b_h0T = fw.alloc([128, 4096], F32, "h0T")
        h0Tb, b_h0Tb = fw.alloc([128, 4096], BF16, "h0Tb")
        hnT, b_hnT = fw.alloc([128, 4096], F32, "hnT")
        yoffT, b_yoffT = fw.alloc([128, 32, 128], F32, "yoffT")
        bmjr = fw.ring(2, [128, 1024], BF16, "bmj")

        def samp(i, a_, bsm, cTt, bcT, bt, bbt, xw, bxw):
            if stage < 5.5:
                fw.op(V, lambda e: e.memset(yoffT, 0.0), writes=[b_yoffT])
                return
            PS0, ba0, bb0 = PS[0]
            for j in range(16):
                fw.op(T, lambda e, j=j: e.matmul(PS0[:, j * 64:(j + 1) * 64], lhsT=onesj[:, j, :], rhs=a_, start=True, stop=True),
                      reads=[b_onesj, bsm], writes=[ba0, bb0], signal=(j == 15))
            fw.op(S, lambda e: e.activation(out=decs[:, 0:512], in_=PS0[:, 0:512], func=AF.Exp), reads=[ba0], writes=[b_decs])
            fw.op(S, lambda e: e.activation(out=decs[:, 512:1024], in_=PS0[:, 512:1024], func=AF.Exp), reads=[bb0], writes=[b_decs])
            if stage < 5.52:
                fw.op(V, lambda e: e.memset(yoffT, 0.0), writes=[b_yoffT])
                return
            for j in range(16):
                h0f, bh = h0r.next()
                h0src = st_ssm_d[j].rearrange("h p n -> (h p) n").rearrange("(hp q) n -> q hp n", q=128)
                for q8 in range(4):
                    fw.dma(SY, h0f[:, q8 * 8:(q8 + 1) * 8, :], h0src[:, q8 * 8:(q8 + 1) * 8, :], dst=bh)
                if stage < 5.53:
                    if j == 0:
                        fw.op(V, lambda e: e.memset(yoffT, 0.0), writes=[b_yoffT])
                    continue
                for q4 in range(8):
                    ptx, bpx = bank(q4 % 4)
                    for u in range(4):
                        hp = q4 * 4 + u
                        fw.op(T, lambda e, ptx=ptx, u=u, hp=hp, h0f=h0f: e.transpose(out=ptx[:, u * 128:(u + 1) * 128], in_=h0f[:, hp, :], identity=idf),
                              reads=[bh, b_idf], writes=[bpx], signal=(u == 3))
                    fw.op(V, lambda e, ptx=ptx, q4=q4: e.tensor_copy(out=h0T[:, q4 * 512:(q4 + 1) * 512], in_=ptx[:, 0:512]), reads=[bpx], writes=[b_h0T])
                    fw.op(S, lambda e, q4=q4: e.activation(out=h0Tb[:, q4 * 512:(q4 + 1) * 512], in_=h0T[:, q4 * 512:(q4 + 1) * 512], func=AF.Identity), reads=[b_h0T], writes=[b_h0Tb])
                if stage < 5.6:
                    if j == 0:
                        fw.op(V, lambda e: e.memset(yoffT, 0.0), writes=[b_yoffT])
                    continue
                pto, bpo = bank(5)
                for hp in range(32):
                    fw.op(T, lambda e, hp=hp, j=j: e.matmul(pto[:, hp * 8:(hp + 1) * 8], lhsT=h0Tb[:, hp * 128:(hp + 1) * 128], rhs=cTt[:, hp // 4, 8 * j:8 * j + 8],
                                                          start=True, stop=True), reads=[b_h0Tb, bcT], writes=[bpo], signal=(hp == 31))
                fw.op(V, lambda e, j=j: e.tensor_copy(out=yoffT[:, :, 8 * j:8 * j + 8], in_=v3(pto[:, 0:256], 32)), reads=[bpo], writes=[b_yoffT])
                if stage < 5.7:
                    continue
                bmj, bbm = bmjr.next()
                fw.op(V, lambda e, bmj=bmj, j=j: e.tensor_scalar(out=bmj, in0=bt, scalar1=maskj[:, j:j + 1], scalar2=None, op0=ALU.mult), reads=[bbt, b_maskj], writes=[bbm])
                for g in range(8):
                    pts, bps = bank(6 + g % 2)
                    fw.op(T, lambda e, g=g, bmj=bmj, pts=pts: e.matmul(pts[:, 0:512], lhsT=bmj[:, g * 128:(g + 1) * 128], rhs=xw[:, g * 512:(g + 1) * 512], start=True, stop=True),
                          reads=[bbm, bxw], writes=[bps])
                    hng = hnT[:, g * 512:(g + 1) * 512]
                    fw.op(V, lambda e, hng=hng, g=g, j=j: e.tensor_tensor(out=v3(hng, 8), in0=v3(h0T[:, g * 512:(g + 1) * 512], 8),
                                                                        in1=decs[:, j * 64 + 8 * g:j * 64 + 8 * g + 8].unsqueeze(2).broadcast_to([128, 8, 64]), op=ALU.mult),
                          reads=[b_h0T, b_decs], writes=[b_hnT])
                    fw.op(V, lambda e, hng=hng, pts=pts: e.tensor_tensor(out=hng, in0=hng, in1=pts[:, 0:512], op=ALU.add), reads=[b_hnT, bps], writes=[b_hnT])
                ho, bho = h0r.next()
                for q4 in range(8):
                    ptx, bpx = bank(q4 % 4)
                    for u in range(4):
                        hp = q4 * 4 + u
                        fw.op(T, lambda e, ptx=ptx, u=u, hp=hp: e.transpose(out=ptx[:, u * 128:(u + 1) * 128], in_=hnT[:, hp * 128:(hp + 1) * 128], identity=idf),
                              reads=[b_hnT, b_idf], writes=[bpx], signal=(u == 3))
                    if q4 % 2:
                        fw.op(S, lambda e, ptx=ptx, q4=q4, ho=ho: e.copy(out=ho[:, q4 * 4:(q4 + 1) * 4, :], in_=v3(ptx[:, 0:512], 4)), reads=[bpx], writes=[bho])
                    else:
                        fw.op(V, lambda e, ptx=ptx, q4=q4, ho=ho: e.tensor_copy(out=ho[:, q4 * 4:(q4 + 1) * 4, :], in_=v3(ptx[:, 0:512], 4)), reads=[bpx], writes=[bho])
                fw.dma(G, o_ssm_s[j].rearrange("h p n -> (h p) n").rearrange("(hp q) n -> q hp n", q=128), ho, src=bho)
        samp.yoffT = (yoffT, b_yoffT)
        if stage >= 5.4:
            ssd_tile(8, "samp", sbufs, samp=samp)
        fw.pop()
        fw.pop()
        fw.barrier()

    def rms_stats(src, ns, bns, junk, bjunk, bsrc, eps, n):
        fw.op(V, lambda e: e.memset(ns[:, 0:1], 0.0), writes=[bns])
        fw.op(S, lambda e: e.activation(out=junk, in_=src, func=AF.Square, accum_out=ns[:, 0:1]), reads=(bsrc if isinstance(bsrc, list) else [bsrc]), writes=[bjunk, bns])
        fw.op(V, lambda e: e.tensor_scalar(out=ns[:, 1:2], in0=ns[:, 0:1], scalar1=1.0 / n, scalar2=eps, op0=ALU.mult, op1=ALU.add), reads=[bns], writes=[bns])
        fw.op(S, lambda e: e.activation(out=ns[:, 2:3], in_=ns[:, 1:2], func=AF.Sqrt), reads=[bns], writes=[bns])
        fw.op(V, lambda e: e.reciprocal(out=ns[:, 3:4], in_=ns[:, 2:3]), reads=[bns], writes=[bns])

    if stage >= 6:
        fw.push()
        offR1 = fw.off
        x2, _ = fw.alloc([128, NTM, D], F32, "x2")
        b_x2 = [[fw.buf(f"x2_{i}_{q}") for q in range(4)] for i in range(NTM)]
        yaTs = fw.alias(offR1, [128, 32, TM], BF16); b_yaTs = fw.buf("yaTs")
        offR2 = fw.off
        xn2, _ = fw.alloc([128, NTM, D], BF16, "xn2")
        b_xn2 = [fw.buf(f"xn2_{i}") for i in range(NTM)]
        ybTs = fw.alias(offR2, [128, 16, TM], BF16); b_ybTs = fw.buf("ybTs")
        offR3 = fw.off
        _r3, _ = fw.alloc([128, 20480], BF16, "R3")
        mT = fw.alias(offR3, [128, 16, TM], BF16)
        b_mT = [fw.buf(f"mT{d}") for d in range(16)]
        Mb_all, b_Mb = fw.alloc([128, NTM, 32], BF16, "Mb_all")
        rk_all, b_rk = fw.alloc([128, NTM, 32], F32, "rk_all")
        Wt_all, b_Wt = fw.alloc([128, NTM, 32], F32, "Wt_all")

        fw.push()
        wbr = fw.ring(2, [128, 48, 128], BF16, "wbo")
        gar = fw.ring(2, [128, 384], F32, "ga")
        gbr = fw.ring(2, [128, 384], F32, "gb")
        tmr = fw.ring(2, [128, 384], F32, "tmpm")
        tm2r = fw.ring(2, [128, 384], F32, "tmpm2")
        for k in range(32):
            fw.dma(SY, yaTs[:, k, :], yaT[k * 128:(k + 1) * 128, :], dst=b_yaTs)
        for k in range(16):
            fw.dma(SY, ybTs[:, k, :], ybT[k * 128:(k + 1) * 128, :], dst=b_ybTs)
        wbo_v = w_bo_d.rearrange("(k p) c -> p k c", p=128)
        pi = 0
        for d in range(16):
            wt, bw = wbr.next()
            fw.dma(G, wt, wbo_v[:, :, d * 128:(d + 1) * 128], dst=bw)
            for (t0, tn) in TB:
                ga, bga = gar.next(); gb, bgb = gbr.next()
                fw.dma(SY, ga, gateT[d * 128:(d + 1) * 128, t0:t0 + tn], dst=bga)
                fw.dma(SY, gb, gateT[2048 + d * 128:2048 + (d + 1) * 128, t0:t0 + tn], dst=bgb)
                pa, bpa = bank(pi % 8); pi += 1
                for k in range(32):
                    fw.op(T, lambda e, pa=pa, wt=wt, k=k, t0=t0, tn=tn: e.matmul(pa[:, 0:tn], lhsT=wt[:, k, :], rhs=yaTs[:, k, t0:t0 + tn], start=(k == 0), stop=(k == 31)),
                          reads=[bw, b_yaTs], writes=[bpa], signal=(k == 31))
                pb, bpb = bank(pi % 8); pi += 1
                for k in range(16):
                    fw.op(T, lambda e, pb=pb, wt=wt, k=k, t0=t0, tn=tn: e.matmul(pb[:, 0:tn], lhsT=wt[:, 32 + k, :], rhs=ybTs[:, k, t0:t0 + tn], start=(k == 0), stop=(k == 15)),
                          reads=[bw, b_ybTs], writes=[bpb], signal=(k == 15))
                tm, btm = tmr.next(); tm2, btm2 = tm2r.next()
                fw.op(V, lambda e, pa=pa, ga=ga, tm=tm, t0=t0, tn=tn: e.tensor_tensor(out=tm[:, 0:tn], in0=pa[:, 0:tn], in1=ga[:, 0:tn], op=ALU.mult),
                      reads=[bpa, bga], writes=[btm])
                fw.op(V, lambda e, pb=pb, gb=gb, tm2=tm2, t0=t0, tn=tn: e.tensor_tensor(out=tm2[:, 0:tn], in0=pb[:, 0:tn], in1=gb[:, 0:tn], op=ALU.mult),
                      reads=[bpb, bgb], writes=[btm2])
                fw.op(V, lambda e, tm=tm, tm2=tm2, d=d, t0=t0, tn=tn: e.tensor_tensor(out=mT[:, d, t0:t0 + tn], in0=tm[:, 0:tn], in1=tm2[:, 0:tn], op=ALU.add),
                      reads=[btm, btm2], writes=[b_mT[d]])
        fw.pop()
        fw.barrier()
        fw.push()
        wor = fw.ring(2, [128, 16, 512], BF16, "wo")
        xrr = fw.ring(2, [128, 512], F32, "xres")
        for dq in range(4):
            wo, bwo = wor.next()
            fw.dma(G, wo, w_out_d.rearrange("(k p) c -> p k c", p=128)[:, :, dq * 512:(dq + 1) * 512], dst=bwo)
            for i in range(NTM):
                xr, bxr = xrr.next()
                fw.dma(SY, xr, xm_d[i * 128:(i + 1) * 128, dq * 512:(dq + 1) * 512], dst=bxr)
                pt, bp = bank(pi % 8); pi += 1
                for k in range(16):
                    fw.op(T, lambda e, pt=pt, k=k, i=i, wo=wo: e.matmul(pt[:, 0:512], lhsT=mT[:, k, i * 128:(i + 1) * 128], rhs=wo[:, k, :],
                                                                     start=(k == 0), stop=(k == 15)), reads=[b_mT[k], bwo], writes=[bp], signal=(k == 15))
                c0 = dq * 512
                fw.op(V, lambda e, pt=pt, xr=xr, i=i, c0=c0: e.tensor_tensor(out=x2[:, i, c0:c0 + 512], in0=pt[:, 0:512], in1=xr, op=ALU.add),
                      reads=[bp, bxr], writes=[b_x2[i][dq]])
        fw.pop()
        fw.barrier()
        fw.push()
        nfb, b_nfb = fw.alloc([128, D], F32, "nfb")
        wr_sb, b_wr = fw.alloc([128, 16, 36], F32, "wr_sb")
        strb, b_strb = fw.alloc([128, 128], BF16, "strb")
        onesb, b_onesb = fw.alloc([128, 128], BF16, "onesb")
        fw.dma(SY, nfb, nf_d, dst=b_nfb)
        fw.dma(SY, wr_sb, w_r_d, dst=b_wr)
        fw.dma(G, strb, stri_d, dst=b_strb)
        fw.op(V, lambda e: e.memset(onesb, 1.0), writes=[b_onesb])
        xnf, b_xnf = fw.alloc([128, D], F32, "xnf")
        xfT, b_xfT = fw.alloc([128, 16, 128], F32, "xfT")
        rsr = fw.ring(2, [128, 160], F32, "rs")
        nsr = fw.ring(2, [128, 4], F32, "nsr")
        for i in range(NTM):
            ns, bns = nsr.next()
            rms_stats(x2[:, i, :], ns, bns, xn2[:, i, :], b_xn2[i], b_x2[i], 1e-6, D)
            fw.op(V, lambda e, ns=ns, i=i: e.scalar_tensor_tensor(out=xnf, in0=x2[:, i, :], scalar=ns[:, 3:4], in1=nfb, op0=ALU.mult, op1=ALU.mult),
                  reads=b_x2[i] + [bns, b_nfb], writes=[b_xnf])
            fw.op(V, lambda e, i=i: e.tensor_copy(out=xn2[:, i, :], in_=xnf), reads=[b_xnf], writes=[b_xn2[i]])
            for q in range(4):
                pt, bp = bank(pi % 8); pi += 1
                for u in range(4):
                    k = 4 * q + u
                    fw.op(T, lambda e, pt=pt, u=u, k=k: e.transpose(out=pt[:, u * 128:(u + 1) * 128], in_=xnf[:, k * 128:(k + 1) * 128], identity=idf),
                          reads=[b_xnf, b_idf], writes=[bp], signal=(u == 3))
                fw.op(V, lambda e, pt=pt, q=q: e.tensor_copy(out=xfT[:, 4 * q:4 * q + 4, :], in_=v3(pt[:, 0:512], 4)), reads=[bp], writes=[b_xfT])
            pt, bp = bank(pi % 8); pi += 1
            for k in range(16):
                fw.op(T, lambda e, pt=pt, k=k: e.matmul(pt[:, 0:36], lhsT=xfT[:, k, :], rhs=wr_sb[:, k, :], start=(k == 0), stop=(k == 15)),
                      reads=[b_xfT, b_wr], writes=[bp], signal=(k == 15))
            r, br = rsr.next()
            lg = r[:, 0:36]; mx = r[:, 36:37]; nmx = r[:, 37:38]; sg = r[:, 38:39]; pg = r[:, 39:40]; ohg = r[:, 40:44]; eg = r[:, 44:48]
            pen = r[:, 48:52]; m1 = r[:, 52:53]; m2 = r[:, 53:54]; dd = r[:, 54:55]; w1 = r[:, 55:56]; lfm = r[:, 56:88]; oh1 = r[:, 88:120]
            lf2 = r[:, 120:152]; w1p = r[:, 152:153]; w2p = r[:, 153:154]

            def vop(f, extra_r=(), extra_w=()):
                fw.op(V, f, reads=[br] + list(extra_r), writes=[br] + list(extra_w))
            fw.op(V, lambda e, pt=pt: e.tensor_copy(out=lg, in_=pt[:, 0:36]), reads=[bp], writes=[br])
            vop(lambda e: e.tensor_reduce(out=mx, in_=lg[:, 0:4], axis=AX.X, op=ALU.max))
            vop(lambda e: e.tensor_scalar(out=ohg, in0=lg[:, 0:4], scalar1=mx, scalar2=None, op0=ALU.is_equal))
            vop(lambda e: e.tensor_scalar(out=nmx, in0=mx, scalar1=-1.0, scalar2=None, op0=ALU.mult))
            fw.op(S, lambda e: e.activation(out=eg, in_=lg[:, 0:4], func=AF.Exp, bias=nmx, scale=1.0), reads=[br], writes=[br])
            vop(lambda e: e.reduce_sum(out=sg, in_=eg, axis=AX.X))
            vop(lambda e: e.reciprocal(out=pg, in_=sg))
            vop(lambda e: e.tensor_scalar(out=pen, in0=ohg, scalar1=-1.0, scalar2=1e30, op0=ALU.add, op1=ALU.mult))
            vop(lambda e: e.tensor_tensor(out=v3(lfm, 4), in0=v3(lg[:, 4:36], 4), in1=pen.unsqueeze(2).broadcast_to([128, 4, 8]), op=ALU.add))
            vop(lambda e: e.tensor_reduce(out=m1, in_=lfm, axis=AX.X, op=ALU.max))
            vop(lambda e: e.tensor_scalar(out=oh1, in0=lfm, scalar1=m1, scalar2=None, op0=ALU.is_equal))
            vop(lambda e: e.scalar_tensor_tensor(out=lf2, in0=oh1, scalar=-1e30, in1=lfm, op0=ALU.mult, op1=ALU.add))
            vop(lambda e: e.tensor_reduce(out=m2, in_=lf2, axis=AX.X, op=ALU.max))
            vop(lambda e: e.tensor_scalar(out=lfm, in0=lf2, scalar1=m2, scalar2=None, op0=ALU.is_equal))
            vop(lambda e: e.tensor_tensor(out=dd, in0=m2, in1=m1, op=ALU.subtract))
            fw.op(S, lambda e: e.activation(out=dd, in_=dd, func=AF.Exp), reads=[br], writes=[br])
            vop(lambda e: e.tensor_scalar(out=dd, in0=dd, scalar1=1.0, scalar2=None, op0=ALU.add))
            vop(lambda e: e.reciprocal(out=w1, in_=dd))
            vop(lambda e: e.tensor_tensor(out=w1p, in0=w1, in1=pg, op=ALU.mult))
            vop(lambda e: e.tensor_tensor(out=w2p, in0=pg, in1=w1p, op=ALU.subtract))
            vop(lambda e, i=i: e.tensor_scalar(out=Wt_all[:, i, :], in0=oh1, scalar1=w1p, scalar2=None, op0=ALU.mult), extra_w=[b_Wt])
            vop(lambda e, i=i: e.scalar_tensor_tensor(out=Wt_all[:, i, :], in0=lfm, scalar=w2p, in1=Wt_all[:, i, :], op0=ALU.mult, op1=ALU.add), extra_r=[b_Wt], extra_w=[b_Wt])
            vop(lambda e, i=i: e.tensor_tensor(out=Mb_all[:, i, :], in0=oh1, in1=lfm, op=ALU.add), extra_w=[b_Mb])
            pt2, bp2 = bank(pi % 8); pi += 1
            for ip in range(i):
                fw.op(T, lambda e, pt2=pt2, ip=ip: e.matmul(pt2[:, 0:32], lhsT=onesb, rhs=Mb_all[:, ip, :], start=(ip == 0), stop=False),
                      reads=[b_onesb, b_Mb], writes=[bp2], signal=False)
            fw.op(T, lambda e, pt2=pt2, i=i: e.matmul(pt2[:, 0:32], lhsT=strb, rhs=Mb_all[:, i, :], start=(i == 0), stop=True), reads=[b_strb, b_Mb], writes=[bp2])
            fw.op(V, lambda e, pt2=pt2, i=i: e.scalar_tensor_tensor(out=rk_all[:, i, :], in0=pt2[:, 0:32], scalar=1.0, in1=Mb_all[:, i, :], op0=ALU.add, op1=ALU.mult),
                  reads=[bp2, b_Mb], writes=[b_rk])
            fw.op(V, lambda e, i=i: e.tensor_scalar(out=rk_all[:, i, :], in0=rk_all[:, i, :], scalar1=-1.0, scalar2=None, op0=ALU.add), reads=[b_rk], writes=[b_rk])
        if debug:
            dbg_rk = dout("dbg_rk", [128, NTM * 32]); dbg_wt = dout("dbg_wt", [128, NTM * 32])
            fw.dma(SY, dbg_rk, rk_all.rearrange("p a b -> p (a b)"), src=b_rk)
            fw.dma(SY, dbg_wt, Wt_all.rearrange("p a b -> p (a b)"), src=b_Wt)
        fw.pop()
        fw.barrier()

    if stage >= 7:
        fw.push()
        iot, b_iot = fw.alloc([128, 128], F32, "iot")
        fw.dma(SY, iot, iota_d, dst=b_iot)
        Sr = fw.ring(2, [128, NTM, 128], BF16, "Ssel")
        SWr = fw.ring(1, [128, NTM, 128], BF16, "SWsel")
        SWTr = fw.ring(2, [128, NTM, 128], BF16, "SWT")
        xgr = fw.ring(1, [128, 16, 128], BF16, "xgT")
        xgmr = fw.ring(1, [128, 2048], BF16, "xgm")
        hsr = fw.ring(1, [128, 1024], F32, "hs")
        hbr = fw.ring(1, [128, 1024], BF16, "hb")
        hTr = fw.ring(2, [128, 8, 128], BF16, "hT")
        yer = fw.ring(1, [128, 2048], BF16, "yexp")
        wslots = [(fw.alias(offR3 + q * 8192, [128, 4096], BF16), fw.buf(f"wslot{q}")) for q in range(5)]
        wring = Ring(wslots)
        NEXP = NE if stage >= 7.5 else 2
        ci = 0
        for ex in range(NEXP):
            S_, bS = Sr.next(); SW, bSW = SWr.next(); SWT, bSWT = SWTr.next()
            for i in range(NTM):
                fw.op(V, lambda e, S_=S_, i=i, ex=ex: e.tensor_scalar(out=S_[:, i, :], in0=iot, scalar1=rk_all[:, i, ex:ex + 1], scalar2=None, op0=ALU.is_equal),
                      reads=[b_iot, b_rk], writes=[bS])
                fw.op(V, lambda e, SW=SW, i=i, ex=ex: e.tensor_scalar(out=SW[:, i, :], in0=iot, scalar1=rk_all[:, i, ex:ex + 1], scalar2=Wt_all[:, i, ex:ex + 1],
                                                                     op0=ALU.is_equal, op1=ALU.mult), reads=[b_iot, b_rk, b_Wt], writes=[bSW])
            for (i0, i1, bk) in ((0, 8, 0), (8, 9, 1)):
                pt, bp = bank16(bk)
                for i in range(i0, i1):
                    fw.op(T, lambda e, pt=pt, i=i, i0=i0, SW=SW: e.transpose(out=pt[:, (i - i0) * 128:(i - i0 + 1) * 128], in_=SW[:, i, :], identity=idb),
                          reads=[bSW, b_idb], writes=[bp], signal=(i == i1 - 1))
                fw.op(V, lambda e, pt=pt, i0=i0, i1=i1, SWT=SWT: e.tensor_copy(out=SWT[:, i0:i1, :], in_=v3(pt[:, 0:(i1 - i0) * 128], i1 - i0)), reads=[bp], writes=[bSWT])
            xgT, bxg = xgr.next()
            xg, bxgm = xgmr.next()
            for db in range(4):
                pt, bp = bank(2 + db % 2)
                for i in range(NTM):
                    fw.op(T, lambda e, pt=pt, i=i, db=db, S_=S_: e.matmul(pt[:, 0:512], lhsT=S_[:, i, :], rhs=xn2[:, i, db * 512:(db + 1) * 512],
                                                                       start=(i == 0), stop=(i == NTM - 1)),
                          reads=[b_xn2[i], bS], writes=[bp], signal=(i == NTM - 1))
                if db % 2 == 0:
                    fw.op(V, lambda e, pt=pt, db=db, xg=xg: e.tensor_copy(out=xg[:, db * 512:(db + 1) * 512], in_=pt[:, 0:512]), reads=[bp], writes=[bxgm])
                else:
                    fw.op(S, lambda e, pt=pt, db=db, xg=xg: e.activation(out=xg[:, db * 512:(db + 1) * 512], in_=pt[:, 0:512], func=AF.Identity), reads=[bp], writes=[bxgm])
            for half in range(2):
                pt, bp = bank16(2 + half)
                for u in range(8):
                    k = half * 8 + u
                    fw.op(T, lambda e, pt=pt, u=u, k=k, xg=xg: e.transpose(out=pt[:, u * 128:(u + 1) * 128], in_=xg[:, k * 128:(k + 1) * 128], identity=idb),
                          reads=[bxgm, b_idb], writes=[bp], signal=(u == 7))
                if half == 0:
                    fw.op(V, lambda e, pt=pt, xgT=xgT: e.tensor_copy(out=xgT[:, 0:8, :], in_=v3(pt[:, 0:1024], 8)), reads=[bp], writes=[bxg])
                else:
                    fw.op(S, lambda e, pt=pt, xgT=xgT: e.activation(out=xgT[:, 8:16, :], in_=v3(pt[:, 0:1024], 8), func=AF.Identity), reads=[bp], writes=[bxg])
            hgT, bhg0, bhg1 = PS[2]
            huT, bhu0, bhu1 = PS[3]
            for q in range(4):
                pieces = []
                for (wd_, acc, ba, bb) in ((w_eg_d, hgT, bhg0, bhg1), (w_eu_d, huT, bhu0, bhu1)):
                    wsl, bws = wring.next()
                    wv = v3(wsl, 4)
                    fw.dma(G, wv, wd_[ex].rearrange("(k p) c -> p k c", p=128)[:, 4 * q:4 * q + 4, :], dst=bws)
                    pieces.append((wv, bws, acc, ba, bb))
                for (wv, bws, acc, ba, bb) in pieces:
                    for kk in range(4):
                        k = 4 * q + kk
                        for half in range(2):
                            fw.op(T, lambda e, acc=acc, wv=wv, kk=kk, k=k, half=half, xgT=xgT: e.matmul(acc[:, half * 512:(half + 1) * 512], lhsT=xgT[:, k, :],
                                                                                                rhs=wv[:, kk, half * 512:(half + 1) * 512], start=(k == 0), stop=(k == 15)),
                                  reads=[bxg, bws], writes=[ba, bb], signal=(kk == 3 and half == 1))
            hs, bhs = hsr.next(); hb, bhb = hbr.next(); hT, bhT = hTr.next()
            for half, (ba, bb) in enumerate(((bhg0, bhu0), (bhg1, bhu1))):
                sl = slice(half * 512, (half + 1) * 512)
                fw.op(S, lambda e, sl=sl, hs=hs: e.activation(out=hs[:, sl], in_=hgT[:, sl], func=AF.Silu), reads=[ba], writes=[bhs])
                fw.op(V, lambda e, sl=sl, hs=hs, hb=hb: e.tensor_tensor(out=hb[:, sl], in0=hs[:, sl], in1=huT[:, sl], op=ALU.mult), reads=[bhs, bb], writes=[bhb])
            pt, bp = bank16(0)
            for k in range(8):
                fw.op(T, lambda e, pt=pt, k=k, hb=hb: e.transpose(out=pt[:, k * 128:(k + 1) * 128], in_=hb[:, k * 128:(k + 1) * 128], identity=idb),
                      reads=[bhb, b_idb], writes=[bp], signal=(k == 7))
            fw.op(V, lambda e, pt=pt, hT=hT: e.tensor_copy(out=hT, in_=v3(pt[:, 0:1024], 8)), reads=[bp], writes=[bhT])
            ydb = [bank(2), bank(3), bank(4), bank(5)]
            for q in range(4):
                wsl, bws = wring.next()
                wv = v3(wsl, 2)
                fw.dma(G, wv, w_ed_d[ex].rearrange("(k p) c -> p k c", p=128)[:, 2 * q:2 * q + 2, :], dst=bws)
                for kk in range(2):
                    k = 2 * q + kk
                    for db in range(4):
                        pt, bp = ydb[db]
                        fw.op(T, lambda e, pt=pt, wv=wv, kk=kk, k=k, db=db, hT=hT: e.matmul(pt[:, 0:512], lhsT=hT[:, k, :], rhs=wv[:, kk, db * 512:(db + 1) * 512],
                                                                                       start=(k == 0), stop=(k == 7)),
                              reads=[bhT, bws], writes=[bp], signal=(kk == 1 and db == 3))
            ye, bye = yer.next()
            for db in range(4):
                pt, bp = ydb[db]
                if db % 2 == 0:
                    fw.op(V, lambda e, pt=pt, db=db, ye=ye: e.tensor_copy(out=ye[:, db * 512:(db + 1) * 512], in_=pt[:, 0:512]), reads=[bp], writes=[bye])
                else:
                    fw.op(S, lambda e, pt=pt, db=db, ye=ye: e.activation(out=ye[:, db * 512:(db + 1) * 512], in_=pt[:, 0:512], func=AF.Identity), reads=[bp], writes=[bye])
            for i in range(NTM):
                for db in range(4):
                    pt, bp = bank((0, 1, 6, 7)[ci % 4]); ci += 1
                    fw.op(T, lambda e, pt=pt, i=i, db=db, SWT=SWT, ye=ye: e.matmul(pt[:, 0:512], lhsT=SWT[:, i, :], rhs=ye[:, db * 512:(db + 1) * 512], start=True, stop=True),
                          reads=[bSWT, bye], writes=[bp])
                    fw.op(V, lambda e, pt=pt, i=i, db=db: e.tensor_tensor(out=x2[:, i, db * 512:(db + 1) * 512], in0=x2[:, i, db * 512:(db + 1) * 512], in1=pt[:, 0:512], op=ALU.add),
                          reads=[bp, b_x2[i][db]], writes=[b_x2[i][db]])
        fw.pop()
        fw.barrier()

    if stage >= 6:
        fw.push()
        nlb, b_nlb = fw.alloc([128, D], F32, "nlb")
        fw.dma(SY, nlb, nl_d, dst=b_nlb)
        yor = fw.ring(2, [128, D], F32, "yo")
        nsr = fw.ring(2, [128, 4], F32, "nsr2")
        for i in range(NTM):
            yo, byo = yor.next(); ns, bns = nsr.next()
            rms_stats(x2[:, i, :], ns, bns, yo, byo, b_x2[i], 1e-6, D)
            fw.op(V, lambda e, yo=yo, ns=ns, i=i: e.scalar_tensor_tensor(out=yo, in0=x2[:, i, :], scalar=ns[:, 3:4], in1=nlb, op0=ALU.mult, op1=ALU.mult),
                  reads=b_x2[i] + [bns, b_nlb], writes=[byo])
            fw.dma(G, y_d[i * 128:(i + 1) * 128, :], yo, src=byo)
        fw.pop()
        fw.pop()
        fw.barrier()

    fw.emit()
    return nc, es


def _consts():
    t = np.arange(128)
    tri_p = (t[:, None] <= t[None, :]).astype(np.float32)
    same = (t[:, None] // 8 == t[None, :] // 8)
    tri_s = (tri_p * same).astype(np.float32)
    bones = np.stack([np.ones((128, 128), np.float32), same.astype(np.float32)])
    maskj = (t[:, None] // 8 == np.arange(16)[None, :]).astype(np.float32)
    onesj = np.ascontiguousarray(np.broadcast_to(maskj[:, :, None], (128, 16, 128))).astype(np.float32)
    return dict(ident=np.eye(128, dtype=np.float32), tri=np.stack([tri_p, tri_s]), bones=bones, onesj=onesj, maskj=maskj,
                iota=np.ascontiguousarray(np.broadcast_to(np.arange(128, dtype=np.float32)[None, :], (128, 128))),
                stri=(t[:, None] < t[None, :]).astype(np.float32))


def _bc(v, n=128):
    return np.ascontiguousarray(np.broadcast_to(np.asarray(v, np.float32).reshape(1, -1), (n, v.size)))


def make_in_maps(inp, cores=range(8)):
    f = lambda a: np.ascontiguousarray(np.asarray(a, dtype=np.float32))
    xpr, xs = f(inp["x_prompt"]), f(inp["x_sample"])
    shared = dict(
        w_in=f(inp["w_in"][0]), w_bo=f(inp["w_branch_out"][0]), w_out=f(inp["w_out"][0]),
        w_eg=f(inp["w_expert_gate"][0]), w_eu=f(inp["w_expert_up"][0]), w_ed=f(inp["w_expert_down"][0]),
        w_r=np.ascontiguousarray(np.concatenate([f(inp["w_router_coarse"][0]), f(inp["w_router_fine"][0])], axis=1)
                                 .reshape(16, 128, 36).transpose(1, 0, 2)),
        nm_bc=_bc(f(inp["norm_mixer"][0])), nf_bc=_bc(f(inp["norm_ffn"][0])), nl_bc=_bc(f(inp["norm_final"])),
        ssdn_bc=_bc(f(inp["ssd_norm"][0])), dtb_bc=_bc(f(inp["ssd_dt_bias"][0])), alog_bc=_bc(f(inp["ssd_a_log"][0])),
        dsk_bc=_bc(np.repeat(f(inp["ssd_d"][0]), 64)),
        cw_fm=np.ascontiguousarray(f(inp["ssd_conv_w"][0]).reshape(4, 48, 128).transpose(2, 1, 0)),
        cb_fm=np.ascontiguousarray(f(inp["ssd_conv_b"][0]).reshape(48, 128).T),
        scw_fm=np.ascontiguousarray(f(inp["sc_conv_w"][0]).reshape(3, 16, 128).transpose(2, 1, 0)),
        **_consts())
    maps = []
    for c in cores:
        s, h = c // 2, c % 2
        m = dict(shared)
        m["xm"] = np.concatenate([xpr[s, h * 1024:(h + 1) * 1024], xs[16 * c:16 * c + 16].reshape(128, D)], axis=0)
        m["xp"] = xpr[s, 0:1024]
        m["flag"] = np.full((128, 1), float(h), np.float32)
        m["st_ssm"] = f(inp["state_ssm"][0, 16 * c:16 * c + 16])
        m["st_conv"] = f(inp["state_ssd_conv"][0, 16 * c:16 * c + 16]).reshape(48, 6144)
        m["st_sc"] = f(inp["state_short_conv"][0, 16 * c:16 * c + 16]).reshape(32, 2048)
        maps.append(m)
    return maps


_CACHE = {}


def kernel(**inp):
    if "nc" not in _CACHE:
        _CACHE["nc"] = build_program()
    nc, _ = _CACHE["nc"]
    maps = make_in_maps(inp)
    res = run_bass_kernel_spmd(nc, maps, core_ids=list(range(8))).results
    y_prompt = np.zeros((4, 2048, D), np.float32)
    y_sample = np.zeros((128, 8, D), np.float32)
    p_ssm = np.zeros((1, 4, 64, 64, 128), np.float32)
    p_conv = np.zeros((1, 4, 3, 6144), np.float32)
    p_sc = np.zeros((1, 4, 2, 2048), np.float32)
    s_ssm = np.zeros((1, 128, 64, 64, 128), np.float32)
    s_conv = np.zeros((1, 128, 3, 6144), np.float32)
    s_sc = np.zeros((1, 128, 2, 2048), np.float32)
    for c in range(8):
        r = res[c]
        s, h = c // 2, c % 2
        y_prompt[s, h * 1024:(h + 1) * 1024] = r["y"][0:1024]
        y_sample[16 * c:16 * c + 16] = r["y"][1024:1152].reshape(16, 8, D)
        s_ssm[0, 16 * c:16 * c + 16] = r["o_ssm_s"]
        s_conv[0, 16 * c:16 * c + 16] = r["o_conv_s"].reshape(16, 3, 6144)
        s_sc[0, 16 * c:16 * c + 16] = r["o_sc_s"].reshape(16, 2, 2048)
        if h == 1:
            p_ssm[0, s] = r["o_ssm_p"]
            p_conv[0, s] = r["o_conv_p"]
            p_sc[0, s] = r["o_sc_p"]
    return (y_prompt, y_sample, p_ssm, p_conv, p_sc, s_ssm, s_conv, s_sc)
```
